# Optimizing a Trainium2 kernel written in Bass

```python
import math
import jax, jax.numpy as jnp
from jax import lax
import numpy as np

D_MODEL = 2048
BATCH = 8
SEQ = 4096
DEPTH = 2

CTX_LEN = 256
GRID_W = 64

HEAD_DIM = 128
D_GDN = 3 * D_MODEL // 8
D_HYENA = D_MODEL // 4
D_MLSTM = 3 * D_MODEL // 8
D_MIX = D_GDN + D_HYENA + D_MLSTM
GDN_HEADS = D_GDN // HEAD_DIM
MLSTM_HEADS = D_MLSTM // HEAD_DIM
GDN_CONV = 5
HYENA_ORDER = 2
HYENA_CONV = 3
FILTER_EMB = 33
FILTER_BANDS = (FILTER_EMB - 1) // 2
FILTER_HIDDEN = 64
DECAY_TARGET = 1e-2
FAST_DECAY_PCT = 0.3
SLOW_DECAY_PCT = 1.5
MIN_DECAY = math.log(DECAY_TARGET) / SLOW_DECAY_PCT
MAX_DECAY = math.log(DECAY_TARGET) / FAST_DECAY_PCT
CHUNK = 64
NORM_EPS = 1e-6
IN_SPLITS = (3 * D_GDN, D_GDN, 4 * GDN_HEADS, 3 * D_HYENA, D_HYENA, 3 * D_MLSTM, D_MLSTM, D_MLSTM, 4 * MLSTM_HEADS)
D_IN = sum(IN_SPLITS)

kernel_name = "hymba_gdn_hyena_mlstm_flow_block"


def rms_norm(x, w):
    xf = x.astype(jnp.float32)
    y = xf * lax.rsqrt(jnp.mean(xf * xf, axis=-1, keepdims=True) + NORM_EPS)
    return (y * w.astype(jnp.float32)).astype(x.dtype)


def adaln(cond, w, b):
    m = jax.nn.silu(cond) @ w + b
    return jnp.split(m, 3, axis=-1)


def split_cols(u):
    idx = np.cumsum(IN_SPLITS)[:-1].tolist()
    return jnp.split(u, idx, axis=-1)


def to_heads(t, n_heads):
    b, l, _ = t.shape
    return t.reshape(b, l, n_heads, HEAD_DIM).transpose(0, 2, 1, 3)


def from_heads(t):
    b, h, l, d = t.shape
    return t.transpose(0, 2, 1, 3).reshape(b, l, h * d)


def flip_seq(t):
    return jnp.flip(t, axis=2)


def l2norm(t):
    return t * lax.rsqrt(jnp.sum(t * t, axis=-1, keepdims=True) + NORM_EPS)


def head_out(o, w, z):
    o = o * lax.rsqrt(jnp.mean(o * o, axis=-1, keepdims=True) + NORM_EPS) * w.astype(jnp.float32)
    return from_heads(o).astype(z.dtype) * jax.nn.silu(z)


def dwconv_centred(x, w, grid):
    k_w = w.shape[0]
    pad = k_w // 2
    length = x.shape[1]
    xp = jnp.pad(x, ((0, 0), (pad, pad), (0, 0)))
    y = None
    for j in range(k_w):
        off = j - pad
        term = xp[:, j:j + length, :] * w[j]
        if grid and off != 0:
            col = jnp.arange(length) % GRID_W
            ok = (col + off >= 0) & (col + off < GRID_W)
            term = jnp.where(ok[None, :, None], term, jnp.zeros_like(term))
        y = term if y is None else y + term
    return y


def gdn_chunk_scan(q, k, v, g, beta, s0):
    b_, h_, length, dk = q.shape
    dv = v.shape[-1]
    n_ch = length // CHUNK

    def ch(t):
        return t.reshape((b_, h_, n_ch, CHUNK) + t.shape[3:])

    qc, kc, vc, bc = ch(q), ch(k), ch(v), ch(beta)
    gcum = jnp.cumsum(ch(g), axis=-1)
    idx = jnp.arange(CHUNK)
    incl = idx[:, None] >= idx[None, :]
    strict = idx[:, None] > idx[None, :]
    dec = jnp.exp(jnp.where(incl, gcum[..., :, None] - gcum[..., None, :], -jnp.inf))
    kk = jnp.einsum('bhnid,bhnjd->bhnij', kc, kc)
    m_low = jnp.where(strict, bc[..., :, None] * kk * dec, 0.0)
    a_mat = m_low + jnp.eye(CHUNK, dtype=m_low.dtype)
    rhs = jnp.concatenate([bc[..., None] * vc, (bc * jnp.exp(gcum))[..., None] * kc], axis=-1)
    sol = lax.linalg.triangular_solve(a_mat, rhs, left_side=True, lower=True, unit_diagonal=True)
    u_tilde, w_mat = sol[..., :dv], sol[..., dv:]
    a_qk = jnp.einsum('bhnid,bhnjd->bhnij', qc, kc) * dec
    q_dec = qc * jnp.exp(gcum)[..., None]
    k_end = kc * jnp.exp(gcum[..., -1:] - gcum)[..., None]
    g_end = jnp.exp(gcum[..., -1])

    def step(s, xs):
        u_t, w_c, a_c, qd, ke, ge = xs
        u = u_t - jnp.einsum('bhck,bhkv->bhcv', w_c, s)
        o = jnp.einsum('bhck,bhkv->bhcv', qd, s) + jnp.einsum('bhij,bhjv->bhiv', a_c, u)
        s = ge[..., None, None] * s + jnp.einsum('bhck,bhcv->bhkv', ke, u)
        return s, o

    xs = tuple(jnp.moveaxis(t, 2, 0) for t in (u_tilde, w_mat, a_qk, q_dec, k_end, g_end))
    s_fin, o = lax.scan(step, s0, xs)
    o = jnp.moveaxis(o, 0, 2).reshape(b_, h_, length, dv)
    return o, s_fin


def mlstm_chunk_scan(q, k, v, log_i, log_f, state):
    b_, h_, length, dk = q.shape
    dv = v.shape[-1]
    n_ch = length // CHUNK

    def ch(t):
        return t.reshape((b_, h_, n_ch, CHUNK) + t.shape[3:])

    qc, kc, vc, li = ch(q), ch(k), ch(v), ch(log_i)
    bcum = jnp.cumsum(ch(log_f), axis=-1)
    idx = jnp.arange(CHUNK)
    incl = idx[:, None] >= idx[None, :]
    d_log = jnp.where(incl, bcum[..., :, None] - bcum[..., None, :] + li[..., None, :], -jnp.inf)
    qk = jnp.einsum('bhnid,bhnjd->bhnij', qc, kc)
    end_log = bcum[..., -1:] - bcum + li

    def step(carry, xs):
        c_m, n_v, m_s = carry
        q_c, k_c, v_c, b_c, dl, qk_c, el = xs
        inter = b_c + m_s[..., None]
        m_i = jnp.maximum(inter, jnp.max(dl, axis=-1))
        w_inter = jnp.exp(inter - m_i)
        p = jnp.exp(dl - m_i[..., None]) * qk_c
        num = w_inter[..., None] * jnp.einsum('bhck,bhkv->bhcv', q_c, c_m) + jnp.einsum('bhij,bhjv->bhiv', p, v_c)
        den = w_inter * jnp.einsum('bhck,bhk->bhc', q_c, n_v) + jnp.sum(p, axis=-1)
        h = num / jnp.maximum(jnp.abs(den), jnp.exp(-m_i))[..., None]
        carry_log = b_c[..., -1] + m_s
        m_new = jnp.maximum(carry_log, jnp.max(el, axis=-1))
        w_c = jnp.exp(el - m_new[..., None])
        dec = jnp.exp(carry_log - m_new)
        c_m = dec[..., None, None] * c_m + jnp.einsum('bhc,bhck,bhcv->bhkv', w_c, k_c, v_c)
        n_v = dec[..., None] * n_v + jnp.einsum('bhc,bhck->bhk', w_c, k_c)
        return (c_m, n_v, m_new), h

    xs = tuple(jnp.moveaxis(t, 2, 0) for t in (qc, kc, vc, bcum, d_log, qk, end_log))
    state_fin, h = lax.scan(step, state, xs)
    h = jnp.moveaxis(h, 0, 2).reshape(b_, h_, length, dv)
    return h, state_fin


def gdn_prep(qkv, ab, conv_w, a_log, dt_bias, grid):
    b_, length, _ = qkv.shape
    qkv = jax.nn.silu(dwconv_centred(qkv, conv_w, grid)).astype(jnp.float32)
    q, k, v = jnp.split(qkv, 3, axis=-1)
    q = l2norm(to_heads(q, GDN_HEADS)) * HEAD_DIM ** -0.5
    k = l2norm(to_heads(k, GDN_HEADS))
    v = to_heads(v, GDN_HEADS)
    ab = ab.astype(jnp.float32).reshape(b_, length, 4, GDN_HEADS)
    g = -jnp.exp(a_log.astype(jnp.float32)) * jax.nn.softplus(ab[:, :, :2] + dt_bias.astype(jnp.float32))
    beta = jax.nn.sigmoid(ab[:, :, 2:])
    return q, k, v, g.transpose(2, 0, 3, 1), beta.transpose(2, 0, 3, 1)


def gdn_branch(qkv_l, ab_l, z_l, qkv_c, ab_c, z_c, conv_w, a_log, dt_bias, norm_w, ctx_out):
    ql, kl, vl, gl, bl = gdn_prep(qkv_l, ab_l, conv_w, a_log, dt_bias, True)
    qc, kc, vc, gc, bc = gdn_prep(qkv_c, ab_c, conv_w, a_log, dt_bias, False)
    s0 = jnp.zeros(qc.shape[:2] + (HEAD_DIM, HEAD_DIM), jnp.float32)
    o_cf, s_f = gdn_chunk_scan(qc, kc, vc, gc[0], bc[0], s0)
    o_cb, s_b = gdn_chunk_scan(flip_seq(qc), flip_seq(kc), flip_seq(vc), flip_seq(gc[1]), flip_seq(bc[1]), s0)
    o_lf, _ = gdn_chunk_scan(ql, kl, vl, gl[0], bl[0], s_f)
    o_lb, _ = gdn_chunk_scan(flip_seq(ql), flip_seq(kl), flip_seq(vl), flip_seq(gl[1]), flip_seq(bl[1]), s_b)
    y_l = head_out(o_lf + flip_seq(o_lb), norm_w, z_l)
    y_c = head_out(o_cf + flip_seq(o_cb), norm_w, z_c) if ctx_out else None
    return y_l, y_c


def mlstm_prep(qkv, o_pre, gates, gate_bias):
    b_, length, _ = qkv.shape
    q, k, v = jnp.split(qkv.astype(jnp.float32), 3, axis=-1)
    q = to_heads(q, MLSTM_HEADS)
    k = to_heads(k, MLSTM_HEADS) * HEAD_DIM ** -0.5
    v = to_heads(v, MLSTM_HEADS)
    gt = (gates.astype(jnp.float32).reshape(b_, length, 4, MLSTM_HEADS) + gate_bias.astype(jnp.float32)).transpose(2, 0, 3, 1)
    log_i = gt[0::2]
    log_f = jax.nn.log_sigmoid(gt[1::2])
    o_gate = jax.nn.sigmoid(to_heads(o_pre.astype(jnp.float32), MLSTM_HEADS))
    return q, k, v, log_i, log_f, o_gate


def mlstm_branch(qkv_l, o_l, g_l, z_l, qkv_c, o_c, g_c, z_c, gate_bias, norm_w, ctx_out):
    ql, kl, vl, lil, lfl, ogl = mlstm_prep(qkv_l, o_l, g_l, gate_bias)
    qc, kc, vc, lic, lfc, ogc = mlstm_prep(qkv_c, o_c, g_c, gate_bias)
    b_, h_ = qc.shape[:2]
    s0 = (jnp.zeros((b_, h_, HEAD_DIM, HEAD_DIM), jnp.float32), jnp.zeros((b_, h_, HEAD_DIM), jnp.float32), jnp.zeros((b_, h_), jnp.float32))
    h_cf, s_f = mlstm_chunk_scan(qc, kc, vc, lic[0], lfc[0], s0)
    h_cb, s_b = mlstm_chunk_scan(flip_seq(qc), flip_seq(kc), flip_seq(vc), flip_seq(lic[1]), flip_seq(lfc[1]), s0)
    h_lf, _ = mlstm_chunk_scan(ql, kl, vl, lil[0], lfl[0], s_f)
    h_lb, _ = mlstm_chunk_scan(flip_seq(ql), flip_seq(kl), flip_seq(vl), flip_seq(lil[1]), flip_seq(lfl[1]), s_b)
    y_l = head_out(ogl * (h_lf + flip_seq(h_lb)), norm_w, z_l)
    y_c = head_out(ogc * (h_cf + flip_seq(h_cb)), norm_w, z_c) if ctx_out else None
    return y_l, y_c


def hyena_filter_spectra(length, w1, b1, freq, w2, b2, w3):
    f32 = jnp.float32
    pos = jnp.arange(length, dtype=f32)
    t = pos / max(length - 1, 1)
    ang = 2.0 * math.pi * pos / length
    bands = jnp.linspace(1e-4, FILTER_BANDS - 1, FILTER_BANDS, dtype=f32)
    feats = jnp.concatenate([t[:, None], jnp.cos(ang[:, None] * bands), -jnp.sin(ang[:, None] * bands)], axis=-1)
    fr = freq.astype(f32)
    h = jnp.sin(fr * (feats @ w1.astype(f32) + b1.astype(f32)))
    h = jnp.sin(fr * (h @ w2.astype(f32) + b2.astype(f32)))
    h = (h @ w3.astype(f32)).reshape(length, HYENA_ORDER, 2, D_HYENA)
    deltas = jnp.abs(jnp.linspace(MIN_DECAY, MAX_DECAY, D_HYENA, dtype=f32))
    h = h * jnp.exp(-t[:, None, None, None] * deltas)
    h = h / (jnp.sum(jnp.abs(h), axis=0, keepdims=True) + NORM_EPS)
    spec = jnp.fft.rfft(h, n=2 * length, axis=0)
    return spec[:, :, 0] + jnp.conj(spec[:, :, 1])


def long_conv(y, spec):
    length = y.shape[1]
    yf = jnp.fft.rfft(y, n=2 * length, axis=1)
    return jnp.fft.irfft(yf * spec[None], n=2 * length, axis=1)[:, :length]


def hyena_branch(p, z, conv_w, w1, b1, freq, w2, b2, w3, skip, grid):
    length = p.shape[1]
    p = dwconv_centred(p, conv_w, grid)
    x1, x2, v = jnp.split(p, 3, axis=-1)
    spec = hyena_filter_spectra(length, w1, b1, freq, w2, b2, w3)
    sk = skip.astype(jnp.float32)
    y = v.astype(jnp.float32)
    y = x1.astype(jnp.float32) * (long_conv(y, spec[:, 0]) + sk[0] * y)
    y = x2.astype(jnp.float32) * (long_conv(y, spec[:, 1]) + sk[1] * y)
    return y.astype(z.dtype) * jax.nn.silu(z)


def setup_inputs(seed: int = 0) -> dict:
    key = jax.random.key(seed)
    ks = jax.random.split(key, 26)
    f32 = jnp.float32

    def nrm(k, shape, s):
        return jax.random.normal(k, shape, f32) * s

    x = nrm(ks[0], (BATCH, SEQ, D_MODEL), 1.0)
    c = nrm(ks[1], (BATCH, D_MODEL), 1.0)
    ctx = nrm(ks[2], (BATCH, CTX_LEN, D_MODEL), 1.0)
    c_ctx = nrm(ks[3], (D_MODEL,), 1.0)
    norm_w = 1.0 + nrm(ks[4], (DEPTH, D_MODEL), 0.02)
    mod_w = nrm(ks[5], (DEPTH, D_MODEL, 3 * D_MODEL), 0.5 * D_MODEL ** -0.5)
    mod_b = nrm(ks[6], (DEPTH, 3 * D_MODEL), 0.02)
    w_in = nrm(ks[7], (DEPTH, D_MODEL, D_IN), D_MODEL ** -0.5)
    gdn_conv = nrm(ks[8], (DEPTH, GDN_CONV, 3 * D_GDN), GDN_CONV ** -0.5)
    gdn_a_log = jnp.log(jax.random.uniform(ks[9], (DEPTH, 2, GDN_HEADS), f32, 1.0, 16.0))
    dt = jnp.exp(jax.random.uniform(ks[10], (DEPTH, 2, GDN_HEADS), f32, math.log(1e-3), math.log(1e-1)))
    gdn_dt_bias = dt + jnp.log(-jnp.expm1(-dt))
    gdn_norm = 1.0 + nrm(ks[11], (DEPTH, HEAD_DIM), 0.02)
    hy_conv = nrm(ks[12], (DEPTH, HYENA_CONV, 3 * D_HYENA), HYENA_CONV ** -0.5)
    hy_w1 = nrm(ks[13], (DEPTH, FILTER_EMB, FILTER_HIDDEN), FILTER_EMB ** -0.5)
    hy_b1 = nrm(ks[14], (DEPTH, FILTER_HIDDEN), 0.02)
    hy_freq = 1.0 + nrm(ks[15], (DEPTH, FILTER_HIDDEN), 0.02)
    hy_w2 = nrm(ks[16], (DEPTH, FILTER_HIDDEN, FILTER_HIDDEN), FILTER_HIDDEN ** -0.5)
    hy_b2 = nrm(ks[17], (DEPTH, FILTER_HIDDEN), 0.02)
    hy_w3 = nrm(ks[18], (DEPTH, FILTER_HIDDEN, HYENA_ORDER * 2 * D_HYENA), FILTER_HIDDEN ** -0.5)
    hy_skip = nrm(ks[19], (DEPTH, HYENA_ORDER, D_HYENA), 0.5)
    f_bias = jnp.linspace(3.0, 6.0, MLSTM_HEADS, dtype=f32)
    zero_b = jnp.zeros((MLSTM_HEADS,), f32)
    ml_gate_bias = jnp.stack([zero_b, f_bias, zero_b, f_bias])[None] + nrm(ks[20], (DEPTH, 4, MLSTM_HEADS), 0.1)
    ml_norm = 1.0 + nrm(ks[21], (DEPTH, HEAD_DIM), 0.02)
    w_out = nrm(ks[22], (DEPTH, D_MIX, D_MODEL), D_MIX ** -0.5)
    final_norm = 1.0 + nrm(ks[23], (D_MODEL,), 0.02)
    return {"x": x, "c": c, "ctx": ctx, "c_ctx": c_ctx, "norm_w": norm_w, "mod_w": mod_w, "mod_b": mod_b,
            "w_in": w_in, "gdn_conv": gdn_conv, "gdn_a_log": gdn_a_log, "gdn_dt_bias": gdn_dt_bias,
            "gdn_norm": gdn_norm, "hy_conv": hy_conv, "hy_w1": hy_w1, "hy_b1": hy_b1, "hy_freq": hy_freq,
            "hy_w2": hy_w2, "hy_b2": hy_b2, "hy_w3": hy_w3, "hy_skip": hy_skip, "ml_gate_bias": ml_gate_bias,
            "ml_norm": ml_norm, "w_out": w_out, "final_norm": final_norm}


def reference(x, c, ctx, c_ctx, norm_w, mod_w, mod_b, w_in, gdn_conv, gdn_a_log, gdn_dt_bias, gdn_norm,
              hy_conv, hy_w1, hy_b1, hy_freq, hy_w2, hy_b2, hy_w3, hy_skip, ml_gate_bias, ml_norm, w_out,
              final_norm):
    for layer in range(DEPTH):
        last = layer == DEPTH - 1
        sh, sc, gt = adaln(c[:, None, :], mod_w[layer], mod_b[layer])
        sh_c, sc_c, gt_c = adaln(c_ctx[None, None, :], mod_w[layer], mod_b[layer])
        u = (rms_norm(x, norm_w[layer]) * (1 + sc) + sh) @ w_in[layer]
        uc = (rms_norm(ctx, norm_w[layer]) * (1 + sc_c) + sh_c) @ w_in[layer]
        g_qkv, g_z, g_ab, h_p, h_z, m_qkv, m_o, m_z, m_g = split_cols(u)
        gc_qkv, gc_z, gc_ab, hc_p, hc_z, mc_qkv, mc_o, mc_z, mc_g = split_cols(uc)
        hy_params = (hy_conv[layer], hy_w1[layer], hy_b1[layer], hy_freq[layer], hy_w2[layer], hy_b2[layer],
                     hy_w3[layer], hy_skip[layer])
        y_gdn, y_gdn_c = gdn_branch(g_qkv, g_ab, g_z, gc_qkv, gc_ab, gc_z, gdn_conv[layer], gdn_a_log[layer],
                                    gdn_dt_bias[layer], gdn_norm[layer], not last)
        y_hy = hyena_branch(h_p, h_z, *hy_params, True)
        y_ml, y_ml_c = mlstm_branch(m_qkv, m_o, m_g, m_z, mc_qkv, mc_o, mc_g, mc_z, ml_gate_bias[layer],
                                    ml_norm[layer], not last)
        x = x + gt * (jnp.concatenate([y_gdn, y_hy, y_ml], axis=-1) @ w_out[layer])
        if not last:
            y_hy_c = hyena_branch(hc_p, hc_z, *hy_params, False)
            ctx = ctx + gt_c * (jnp.concatenate([y_gdn_c, y_hy_c, y_ml_c], axis=-1) @ w_out[layer])
    return rms_norm(x, final_norm)
```

```python
import contextlib
import math
import numpy as np
import concourse.bass as bass
import concourse.mybir as mybir
from concourse.bass_utils import run_bass_kernel_spmd

F32 = mybir.dt.float32
BF16 = mybir.dt.bfloat16
AF = mybir.ActivationFunctionType
ALU = mybir.AluOpType
AX = mybir.AxisListType

SEM_EPOCH = 24000
class Buf:
    __slots__ = ("name", "ws", "r", "dsem", "dcnt", "multi")

    def __init__(self, name, multi=False):
        self.name = name
        self.ws = {}
        self.r = {}
        self.dsem = None
        self.dcnt = 0
        self.multi = multi


class T:
    def __init__(self, t, name, psum=False):
        self.t = t
        self.b = Buf(name)
        self.psum = psum

    def __getitem__(self, k):
        return self.t[k]


class FW:
    def __init__(self, nc):
        self.nc = nc
        self.es = contextlib.ExitStack()
        self.eng = {"pe": nc.tensor, "act": nc.scalar, "dve": nc.vector, "pool": nc.gpsimd, "sp": nc.sync}
        self.sem, self.cnt, self.seen = {}, {}, {}
        for e in self.eng:
            self.sem[e] = self.es.enter_context(nc.semaphore("c_" + e))
            self.cnt[e] = 0
            self.seen[e] = {}
        self.pe_sems = {self.sem["pe"].num}
        self.nsem = 5
        self.ninstr = 0
        self.free_dsems = []

    def uname(self, name):
        self.nname = getattr(self, "nname", 0) + 1
        return "t%d_%s" % (self.nname, name)

    def sb(self, name, shape, dt=F32):
        return T(self.es.enter_context(self.nc.sbuf_tensor(self.uname(name), list(shape), dt)), name)

    def ps(self, name, shape, dt=F32):
        return T(self.es.enter_context(self.nc.psum_tensor(self.uname(name), list(shape), dt)), name, psum=True)

    def scope(self):
        return Scope(self)

    @staticmethod
    def _bufs(lst):
        out = []
        for x in lst:
            if x is None:
                continue
            out.append(x.b if isinstance(x, T) else x)
        return out

    def _wait(self, e, evs):
        eng = self.eng[e]
        seen = self.seen[e]
        best = {}
        for (s, v) in evs:
            k = s.num
            if best.get(k, (None, 0))[1] < v:
                best[k] = (s, v)
        for k, (s, v) in best.items():
            if e == "pe" and k in self.pe_sems:
                continue
            if seen.get(k, 0) < v:
                eng.wait_ge(s, v)
                seen[k] = v

    @staticmethod
    def _deps(reads, writes):
        evs = []
        for b in reads:
            evs.extend(b.ws.values())
        for b in writes:
            if not b.multi:
                evs.extend(b.ws.values())
            evs.extend(b.r.values())
        return evs

    @staticmethod
    def _put(d, ev):
        k = ev[0].num
        if k not in d or d[k][1] < ev[1]:
            d[k] = ev

    def _record(self, ev, reads, writes):
        for b in reads:
            self._put(b.r, ev)
        for b in writes:
            if b.multi:
                self._put(b.ws, ev)
            else:
                b.ws = {ev[0].num: ev}
                b.r = {}

    def op(self, e, fn, reads=(), writes=()):
        pr = [x for x in reads if isinstance(x, T) and x.psum]
        if pr:
            reads = [x for x in reads if not (isinstance(x, T) and x.psum)]
            writes = list(writes) + pr
        reads = self._bufs(reads)
        writes = self._bufs(writes)
        self._wait(e, self._deps(reads, writes))
        ins = fn(self.eng[e])
        if self.cnt[e] >= SEM_EPOCH:
            self.sem[e] = self.es.enter_context(self.nc.semaphore("c%d_%s" % (self.nsem, e)))
            self.nsem += 1
            self.cnt[e] = 0
            if e == "pe":
                self.pe_sems.add(self.sem[e].num)
        self.cnt[e] += 1
        self.ninstr += 1
        ins.then_inc(self.sem[e], 1)
        ev = (self.sem[e], self.cnt[e])
        self._record(ev, reads, writes)
        return ev

    def dma(self, e, out, in_, reads=(), writes=(), key=None, **kw):
        reads = self._bufs(reads)
        writes = self._bufs(writes)
        kb = key.b if isinstance(key, T) else key
        if kb.dsem is None:
            if self.free_dsems:
                kb.dsem, kb.dcnt = self.free_dsems.pop()
            else:
                kb.dsem = self.es.enter_context(self.nc.semaphore("d%d" % self.nsem))
                self.nsem += 1
        evs = self._deps(reads, writes)
        if kb.dcnt > 0:
            evs.append((kb.dsem, kb.dcnt))
        self._wait(e, evs)
        if kb.dcnt >= SEM_EPOCH:
            kb.dsem = self.es.enter_context(self.nc.semaphore("d%d" % self.nsem))
            self.nsem += 1
            kb.dcnt = 0
        ins = self.eng[e].dma_start(out=out, in_=in_, **kw)
        kb.dcnt += 16
        self.ninstr += 1
        ins.then_inc(kb.dsem, 16)
        ev = (kb.dsem, kb.dcnt)
        self._record(ev, reads, writes)
        return ev

    def fence(self, tiles, engines=("pe", "act", "dve", "pool", "sp")):
        evs = []
        for b in self._bufs(tiles):
            evs.extend(b.ws.values())
            evs.extend(b.r.values())
        for e in engines:
            self._wait(e, evs)

    def release(self, tiles):
        for t in tiles:
            b = t.b if isinstance(t, T) else t
            if b.dsem is not None:
                if b.dcnt < SEM_EPOCH // 2:
                    self.free_dsems.append((b.dsem, b.dcnt))
                b.dsem = None

    def mm(self, out_t, out_ap, lhsT_t, lhsT_ap, rhs_t, rhs_ap, start=True, stop=True):
        return self.op("pe", lambda g: g.matmul(out_ap, lhsT=lhsT_ap, rhs=rhs_ap, start=start, stop=stop),
                       reads=[lhsT_t, rhs_t], writes=[out_t])

    def tr(self, out_t, out_ap, in_t, in_ap, ident):
        n = in_ap.shape[0]
        return self.op("pe", lambda g: g.transpose(out_ap, in_ap, ident.t[0:n, 0:n]),
                       reads=[in_t, ident], writes=[out_t])


class Scope:
    def __init__(self, fw):
        self.fw = fw
        self.es = contextlib.ExitStack()
        self.tiles = []

    def __enter__(self):
        return self

    def sb(self, name, shape, dt=F32):
        t = T(self.es.enter_context(self.fw.nc.sbuf_tensor(self.fw.uname(name), list(shape), dt)), name)
        self.tiles.append(t)
        return t

    def ps(self, name, shape, dt=F32):
        t = T(self.es.enter_context(self.fw.nc.psum_tensor(self.fw.uname(name), list(shape), dt)), name, psum=True)
        self.tiles.append(t)
        return t

    def __exit__(self, *a):
        self.fw.fence(self.tiles)
        self.fw.release(self.tiles)
        self.es.close()
        return False


class RR:
    def __init__(self, items):
        self.items = list(items)
        self.i = 0

    def next(self):
        x = self.items[self.i % len(self.items)]
        self.i += 1
        return x

D = 2048
SEQ = 4096
CTXL = 256
NTOK = SEQ + CTXL
NTILE = NTOK // 128
KC = D // 128
DEPTH = 2
NH = 6
HD = 128
DG = 768
DHY = 512
EPS = 1e-6
FC = 3840
TC = 5168
T_GZ, T_GAB, T_HP, T_HZ, T_MV, T_MO, T_MZ, T_MG = 0, 768, 792, 2328, 2840, 3608, 4376, 5144
FCOLS = np.concatenate([np.arange(0, 2304), np.arange(5144, 6680)])
TCOLS = np.concatenate([np.arange(2304, 5144), np.arange(6680, 9008)])
Y_G, Y_H, Y_M = 0, 768, 1280


def build(dbg=None, layers=(0, 1), phases=("mod", "proj", "gdn", "hy", "ml", "out"), dbg_in=None, opts=None):
    dbg = dbg or {}
    dbg_in = dbg_in or {}
    opts = opts or {}
    nc = bass.Bass("TRN2", target_bir_lowering=False)
    fw = FW(nc)

    def din(name, shape, dt=F32):
        return nc.dram_tensor(name, list(shape), dt, kind="ExternalInput").ap()

    def dscr(name, shape, dt=F32):
        return nc.dram_tensor(name, list(shape), dt, kind="Internal").ap()

    big = ("proj" in phases) or ("out" in phases)
    x_in = din("x", [SEQ, D]) if big else None
    ctx_in = din("ctx", [CTXL, D]) if big else None
    cT_in = din("cT", [128, KC, 2])
    nwT_in = din("nwT", [128, DEPTH, KC])
    fnw_in = din("fnw", [1, D])
    modw_in = din("modw", [DEPTH, 128, KC, 3 * D]) if "mod" in phases else None
    modbT_in = din("modbT", [128, DEPTH, 32])
    modbg_in = din("modbg", [DEPTH, D])
    winF_in = din("winF", [DEPTH, 128, KC, FC]) if "proj" in phases else None
    winT_in = din("winT", [DEPTH, 128, KC, TC]) if "proj" in phases else None
    wout_in = din("wout", [DEPTH, 128, KC, D]) if "out" in phases else None
    ident_in = din("ident", [128, 128])
    cmask_in = din("cmask", [128, 6, 128])
    gpar_in = din("gpar", [DEPTH, 24])
    gconv_in = din("gconv", [DEPTH, 128, 18, 5])
    gnorm_in = din("gnorm", [DEPTH, 128])
    mnorm_in = din("mnorm", [DEPTH, 128])
    mgb_in = din("mgb", [DEPTH, 24])
    featL_in = din("featL", [33, SEQ])
    featC_in = din("featC", [33, CTXL])
    hyw1_in = din("hyw1", [DEPTH, 33, 64])
    hyw2_in = din("hyw2", [DEPTH, 64, 64])
    hyw3_in = din("hyw3", [DEPTH, 64, 2048])
    hyp_in = din("hyp", [DEPTH, 64, 3])
    hyskip_in = din("hyskip", [DEPTH, 1024])
    hyconv_in = din("hyconv", [DEPTH, 3, 1536])
    hyconvT_in = din("hyconvT", [DEPTH, 128, 12, 3])
    hyskipT_in = din("hyskipT", [DEPTH, 128, 2, 4])
    negt_in = din("negt", [64, 64])
    negtc_in = din("negtc", [1, CTXL])
    drow_in = din("drow", [1, 512])
    dcol_in = din("dcol", [128, 4])
    F1_in = din("F1", [64, 2, 128], BF16)
    F4_in = din("F4", [128, 2, 64], BF16)
    W2_in = din("W2", [64, 128, 4, 128], BF16)
    W3_in = din("W3", [128, 128, 128], BF16)
    identb_in = din("identb", [128, 128], BF16)
    identb = fw.sb("identb", [128, 128], BF16)
    fw.dma("sp", identb[:], identb_in[:, :], writes=[identb], key=identb)
    out_d = nc.dram_tensor("out", [SEQ, D], F32, kind="ExternalOutput").ap()

    xcur = dscr("xcur", [SEQ, D])
    ctxcur = dscr("ctxcur", [CTXL, D])
    UF = dscr("UF", [FC, NTOK])
    UT = dscr("UT", [NTOK, TC])
    Y = dscr("Y", [NTOK, D])
    b_xcur, b_ctxcur, b_UF, b_UT, b_Y, b_out = (Buf(n, multi=True) for n in ("xcur", "ctxcur", "UF", "UT", "Y", "out"))

    dbg_out = {}

    def dbg_tensor(name, shape):
        t = nc.dram_tensor("dbg_" + name, list(shape), F32, kind="ExternalOutput").ap()
        dbg_out[name] = t
        return t

    ident = fw.sb("ident", [128, 128])
    fw.dma("sp", ident[:], ident_in[:, :], writes=[ident], key=ident)
    cT = fw.sb("cT", [128, KC, 2])
    fw.dma("sp", cT[:], cT_in[:, :, :], writes=[cT], key=cT)
    scT = fw.sb("scT", [128, KC, 2])
    fw.op("act", lambda g: g.activation(out=scT[:], in_=cT[:], func=AF.Silu), reads=[cT], writes=[scT])
    nwT = fw.sb("nwT", [128, DEPTH, KC])
    fw.dma("sp", nwT[:], nwT_in[:, :, :], writes=[nwT], key=nwT)
    modbT = fw.sb("modbT", [128, DEPTH, 32])
    fw.dma("sp", modbT[:], modbT_in[:, :, :], writes=[modbT], key=modbT)
    modT = fw.sb("modT", [128, 32, 2])
    Amod = fw.sb("Amod", [128, 2, KC])
    Bmod = fw.sb("Bmod", [128, 2, KC])
    gtbc = [fw.sb("gtbc%d" % i, [128, D]) for i in range(2)]

    def phase_mod(l):
        with fw.scope() as sc:
            wb = [sc.sb("modw%d" % i, [128, KC, 512]) for i in range(2)]
            pss = [sc.ps("modps%d" % i, [128, 512]) for i in range(2)]
            gb = sc.sb("gbias", [128, D])
            rep = [sc.sb("rep%d" % i, [128, KC, 128]) for i in range(2)]
            for i in range(2):
                fw.op("dve", lambda g, i=i: g.tensor_copy(out=rep[i][:], in_=scT[:, :, i:i + 1].to_broadcast([128, KC, 128])),
                      reads=[scT], writes=[rep[i]])
            fw.dma("sp", gb[:], modbg_in[l:l + 1, :].partition_broadcast(128)[:, 0, :], writes=[gb], key=gb)
            for blk in range(12):
                w = wb[blk % 2]
                fw.dma("sp" if blk % 2 == 0 else "act", w[:], modw_in[l, :, :, blk * 512:(blk + 1) * 512],
                       writes=[w], key=w)
                if blk < 8:
                    p = pss[blk % 2]
                    for sub in range(4):
                        for kc in range(KC):
                            fw.mm(p, p[:, sub * 2:sub * 2 + 2], w, w[:, kc, sub * 128:(sub + 1) * 128],
                                  scT, scT[:, kc, :], start=(kc == 0), stop=(kc == KC - 1))
                    fw.op("dve", lambda g, p=p, blk=blk: g.tensor_tensor(
                        out=modT[:, blk * 4:(blk + 1) * 4, :],
                        in0=p[:, 0:8].rearrange("p (a b) -> p a b", b=2),
                        in1=modbT[:, l, blk * 4:(blk + 1) * 4].unsqueeze(2).to_broadcast([128, 4, 2]),
                        op=ALU.add), reads=[p, modbT], writes=[modT])
                else:
                    for i in range(2):
                        p = pss[i]
                        for kc in range(KC):
                            fw.mm(p, p[:, :], rep[i], rep[i][:, kc, :], w, w[:, kc, :],
                                  start=(kc == 0), stop=(kc == KC - 1))
                        cs = slice((blk - 8) * 512, (blk - 7) * 512)
                        fw.op("dve", lambda g, p=p, i=i, cs=cs: g.tensor_tensor(
                            out=gtbc[i][:, cs], in0=p[:, :], in1=gb[:, cs], op=ALU.add),
                            reads=[p, gb], writes=[gtbc[i]])
            for i in range(2):
                fw.op("dve", lambda g, i=i: g.scalar_tensor_tensor(
                    out=Amod[:, i, :], in0=modT[:, 16:32, i], scalar=1.0, in1=nwT[:, l, :],
                    op0=ALU.add, op1=ALU.mult), reads=[modT, nwT], writes=[Amod])
                fw.op("dve", lambda g, i=i: g.tensor_copy(out=Bmod[:, i, :], in_=modT[:, 0:16, i]),
                      reads=[modT], writes=[Bmod])

    def load_norm_transpose(sc, src_ap, src_buf, xt, ss, junk, tps, dstT, dst_cols, A_ap_fn, B_ap_fn, evq):
        fw.dma("sp", xt[:], src_ap, reads=[src_buf], writes=[xt], key=xt)
        if ss is not None:
            fw.op("act", lambda g: g.activation(out=junk[:], in_=xt[:], func=AF.Square, accum_out=ss[:, 0:1]),
                  reads=[xt], writes=[junk, ss])
            fw.op("dve", lambda g: g.tensor_scalar(out=ss[:, 1:2], in0=ss[:, 0:1], scalar1=1.0 / D, scalar2=EPS,
                                                    op0=ALU.mult, op1=ALU.add), reads=[ss], writes=[ss])
            fw.op("act", lambda g: g.sqrt(out=ss[:, 3:4], in_=ss[:, 1:2]), reads=[ss], writes=[ss])
            fw.op("dve", lambda g: g.reciprocal(out=ss[:, 2:3], in_=ss[:, 3:4]), reads=[ss], writes=[ss])
            fw.op("dve", lambda g: g.tensor_scalar(out=xt[:], in0=xt[:], scalar1=ss[:, 2:3], scalar2=None,
                                                    op0=ALU.mult), reads=[xt, ss], writes=[xt])
        for q4 in range(KC // 4):
            p = tps.next()
            for j in range(4):
                kc = q4 * 4 + j
                fw.tr(p, p[:, j * 128:(j + 1) * 128], xt, xt[:, kc * 128:(kc + 1) * 128], ident)
            for j in range(4):
                kc = q4 * 4 + j
                e = evq.next()
                if A_ap_fn is None:
                    if e == "act":
                        fw.op("act", lambda g, p=p, j=j, kc=kc: g.copy(out=dstT[:, kc, dst_cols], in_=p[:, j * 128:(j + 1) * 128]),
                              reads=[p], writes=[dstT])
                    else:
                        fw.op("dve", lambda g, p=p, j=j, kc=kc: g.tensor_copy(out=dstT[:, kc, dst_cols], in_=p[:, j * 128:(j + 1) * 128]),
                              reads=[p], writes=[dstT])
                else:
                    if e == "act":
                        fw.op("act", lambda g, p=p, j=j, kc=kc: g.activation(
                            out=dstT[:, kc, dst_cols], in_=p[:, j * 128:(j + 1) * 128], func=AF.Identity,
                            scale=A_ap_fn(kc), bias=B_ap_fn(kc)), reads=[p, Amod, Bmod], writes=[dstT])
                    else:
                        fw.op("dve", lambda g, p=p, j=j, kc=kc: g.tensor_scalar(
                            out=dstT[:, kc, dst_cols], in0=p[:, j * 128:(j + 1) * 128],
                            scalar1=A_ap_fn(kc), scalar2=B_ap_fn(kc), op0=ALU.mult, op1=ALU.add),
                            reads=[p, Amod, Bmod], writes=[dstT])

    def phase_proj(l):
        GT = 17
        xsrc, xbuf = (x_in, None) if l == 0 else (xcur, b_xcur)
        csrc, cbuf = (ctx_in, None) if l == 0 else (ctxcur, b_ctxcur)
        with fw.scope() as sc:
            xnT = sc.sb("xnT", [128, KC, GT * 128], BF16)
            xts = RR([sc.sb("xt%d" % i, [128, D]) for i in range(2)])
            junk = sc.sb("junk", [128, D])
            sss = RR([sc.sb("ss%d" % i, [128, 4]) for i in range(2)])
            tps = RR([sc.ps("tps%d" % i, [128, 512]) for i in range(2)])
            mps = RR([sc.ps("mps%d" % i, [128, 512]) for i in range(4)])
            wbs = RR([sc.sb("wb%d" % i, [128, KC, 512], BF16) for i in range(2)])
            sts = RR([sc.sb("st%d" % i, [128, 512]) for i in range(4)])
            evq = RR(["act", "dve"])
            for grp in range(2):
                for ti in range(GT):
                    gt_ = grp * GT + ti
                    if gt_ < 2:
                        src, sbuf, mi = csrc[gt_ * 128:(gt_ + 1) * 128, :], cbuf, 1
                    else:
                        src, sbuf, mi = xsrc[(gt_ - 2) * 128:(gt_ - 1) * 128, :], xbuf, 0
                    load_norm_transpose(sc, src, sbuf, xts.next(), sss.next(), junk, tps, xnT,
                                        slice(ti * 128, (ti + 1) * 128),
                                        lambda kc, mi=mi: Amod[:, mi, kc:kc + 1],
                                        lambda kc, mi=mi: Bmod[:, mi, kc:kc + 1], evq)
                tok0 = grp * GT * 128
                for c0 in range(0, TC, 512):
                    ncol = min(512, TC - c0)
                    w = wbs.next()
                    fw.dma("pool", w[:, :, 0:ncol], winT_in[l, :, :, c0:c0 + ncol], writes=[w], key=w)
                    for ti in range(GT):
                        p = mps.next()
                        for kc in range(KC):
                            fw.mm(p, p[:, 0:ncol], xnT, xnT[:, kc, ti * 128:(ti + 1) * 128], w, w[:, kc, 0:ncol],
                                  start=(kc == 0), stop=(kc == KC - 1))
                        st = sts.next()
                        e = evq.next()
                        if e == "act":
                            fw.op("act", lambda g, p=p, st=st: g.copy(out=st[:, 0:ncol], in_=p[:, 0:ncol]), reads=[p], writes=[st])
                        else:
                            fw.op("dve", lambda g, p=p, st=st: g.tensor_copy(out=st[:, 0:ncol], in_=p[:, 0:ncol]), reads=[p], writes=[st])
                        r0 = tok0 + ti * 128
                        fw.dma(e if e == 'act' else 'pool', UT[r0:r0 + 128, c0:c0 + ncol], st[:, 0:ncol], reads=[st], writes=[b_UT], key=st)
                ntk = GT * 128
                for c0 in range(0, FC, 512):
                    w = wbs.next()
                    ncf = min(512, FC - c0)
                    fw.dma("pool", w[:, :, 0:ncf], winF_in[l, :, :, c0:c0 + ncf], writes=[w], key=w)
                    for ct in range(4):
                        if c0 + ct * 128 >= FC:
                            break
                        for t0 in range(0, ntk, 512):
                            nt = min(512, ntk - t0)
                            p = mps.next()
                            for kc in range(KC):
                                fw.mm(p, p[:, 0:nt], w, w[:, kc, ct * 128:(ct + 1) * 128], xnT, xnT[:, kc, t0:t0 + nt],
                                      start=(kc == 0), stop=(kc == KC - 1))
                            st = sts.next()
                            e = evq.next()
                            if e == "act":
                                fw.op("act", lambda g, p=p, st=st, nt=nt: g.copy(out=st[:, 0:nt], in_=p[:, 0:nt]), reads=[p], writes=[st])
                            else:
                                fw.op("dve", lambda g, p=p, st=st, nt=nt: g.tensor_copy(out=st[:, 0:nt], in_=p[:, 0:nt]), reads=[p], writes=[st])
                            r0 = c0 + ct * 128
                            fw.dma(e if e == 'act' else 'pool', UF[r0:r0 + 128, tok0 + t0:tok0 + t0 + nt], st[:, 0:nt], reads=[st], writes=[b_UF], key=st)

    def phase_out(l):
        last = (l == DEPTH - 1)
        xsrc, xbuf = (x_in, None) if l == 0 else (xcur, b_xcur)
        with fw.scope() as sc:
            wo = sc.sb("wo", [128, KC, D], BF16)
            for h in range(4):
                fw.dma("pool", wo[:, h * 4:(h + 1) * 4, :], wout_in[l, :, h * 4:(h + 1) * 4, :], writes=[wo], key=wo)
            yts = RR([sc.sb("yt%d" % i, [128, D]) for i in range(2)])
            xts = RR([sc.sb("xr%d" % i, [128, D]) for i in range(2)])
            yT = RR([sc.sb("yT%d" % i, [128, KC, 128], BF16) for i in range(2)])
            tps = RR([sc.ps("tps%d" % i, [128, 512]) for i in range(2)])
            mps = RR([sc.ps("mps%d" % i, [128, 512]) for i in range(4)])
            evq = RR(["act", "dve"])
            ss = sc.sb("ss", [128, 4])
            junk = sc.sb("junk", [128, D])
            fnw = None
            if last:
                fnw = sc.sb("fnw", [128, D])
                fw.dma("sp", fnw[:], fnw_in[0:1, :].partition_broadcast(128)[:, 0, :], writes=[fnw], key=fnw)
            tiles = range(2, NTILE) if last else range(NTILE)
            for gt_ in tiles:
                isctx = gt_ < 2
                yt = yts.next()
                yTt = yT.next()
                load_norm_transpose(sc, Y[gt_ * 128:(gt_ + 1) * 128, :], b_Y, yt, None, None, tps, yTt,
                                    slice(0, 128), None, None, evq)
                xr = xts.next()
                if isctx:
                    src = (ctx_in if l == 0 else ctxcur)[gt_ * 128:(gt_ + 1) * 128, :]
                    sb_ = None if l == 0 else b_ctxcur
                else:
                    src = xsrc[(gt_ - 2) * 128:(gt_ - 1) * 128, :]
                    sb_ = xbuf
                fw.dma("act", xr[:], src, reads=[sb_], writes=[xr], key=xr)
                g_ = gtbc[1 if isctx else 0]
                for fb in range(4):
                    p = mps.next()
                    fs = slice(fb * 512, (fb + 1) * 512)
                    for kc in range(KC):
                        fw.mm(p, p[:, :], yTt, yTt[:, kc, :], wo, wo[:, kc, fs], start=(kc == 0), stop=(kc == KC - 1))
                    fw.op("dve", lambda g, p=p, fs=fs, g_=g_, yt=yt: g.tensor_tensor(out=yt[:, fs], in0=p[:, :], in1=g_[:, fs], op=ALU.mult),
                          reads=[p, g_], writes=[yt])
                    fw.op("pool", lambda g, fs=fs, yt=yt, xr=xr: g.tensor_tensor(out=xr[:, fs], in0=xr[:, fs], in1=yt[:, fs], op=ALU.add),
                          reads=[yt, xr], writes=[xr])
                if not last:
                    if isctx:
                        fw.dma("pool", ctxcur[gt_ * 128:(gt_ + 1) * 128, :], xr[:], reads=[xr], writes=[b_ctxcur], key=xr)
                    else:
                        fw.dma("pool", xcur[(gt_ - 2) * 128:(gt_ - 1) * 128, :], xr[:], reads=[xr], writes=[b_xcur], key=xr)
                else:
                    fw.op("act", lambda g, xr=xr: g.activation(out=junk[:], in_=xr[:], func=AF.Square, accum_out=ss[:, 0:1]),
                          reads=[xr], writes=[junk, ss])
                    fw.op("dve", lambda g: g.tensor_scalar(out=ss[:, 1:2], in0=ss[:, 0:1], scalar1=1.0 / D, scalar2=EPS,
                                                            op0=ALU.mult, op1=ALU.add), reads=[ss], writes=[ss])
                    fw.op("act", lambda g: g.sqrt(out=ss[:, 3:4], in_=ss[:, 1:2]), reads=[ss], writes=[ss])
                    fw.op("dve", lambda g: g.reciprocal(out=ss[:, 2:3], in_=ss[:, 3:4]), reads=[ss], writes=[ss])
                    fw.op("dve", lambda g, xr=xr: g.scalar_tensor_tensor(out=xr[:], in0=xr[:], scalar=ss[:, 2:3], in1=fnw[:],
                                                                         op0=ALU.mult, op1=ALU.mult), reads=[xr, ss, fnw], writes=[xr])
                    fw.dma("pool", out_d[(gt_ - 2) * 128:(gt_ - 1) * 128, :], xr[:], reads=[xr], writes=[b_out], key=xr)


    cmask = fw.sb("cmask", [128, 6, 128])
    fw.dma("sp", cmask[:], cmask_in[:, :, :], writes=[cmask], key=cmask)
    TRI = [cmask[:, 0, :], cmask[:, 1, :]]
    LS = [cmask[:, 2, :], cmask[:, 3, :]]
    LIT = [cmask[:, 4, :], cmask[:, 5, :]]
    trione = [fw.sb("trione%d" % d, [128, 129]) for d in range(2)]
    for d in range(2):
        fw.op("dve", lambda g, d=d: g.tensor_copy(out=trione[d][:, 0:128], in_=TRI[d]), reads=[cmask], writes=[trione[d]])
        fw.op("dve", lambda g, d=d: g.memset(trione[d][:, 128:129], 1.0), writes=[trione[d]])
    ones = fw.sb("ones", [128, 128])
    fw.op("dve", lambda g: g.memset(ones[:], 1.0), writes=[ones])
    epsc = fw.sb("epsc", [128, 1])
    fw.op("dve", lambda g: g.memset(epsc[:], EPS), writes=[epsc])
    NCH = NTILE

    def chunk_order(d):
        return list(range(NCH)) if d == 0 else [1, 0] + list(range(NCH - 1, 1, -1))

    OFs = [dscr("OF", [NTOK, DG]), dscr("OB", [NTOK, DG])]
    b_OF = [Buf("OF", multi=True), Buf("OB", multi=True)]

    def decay_prep(ws, gcol_ap, gsrc, d, need_D):
        fw.op("dve", lambda g: g.tensor_scalar(out=ws["gtri"][:], in0=TRI[d], scalar1=gcol_ap, scalar2=None, op0=ALU.mult),
              reads=[cmask, gsrc], writes=[ws["gtri"]])
        fw.op("pool", lambda g: g.tensor_copy(out=ws["grep"][:], in_=gcol_ap.to_broadcast([128, 128])),
              reads=[gsrc], writes=[ws["grep"]])
        pDT = ws["ps"].next()
        fw.mm(pDT, pDT[:, 0:128], cmask, LS[d], ws["gtri"], ws["gtri"][:])
        fw.op("act", lambda g: g.activation(out=ws["eDT"][:], in_=pDT[:, 0:128], func=AF.Exp), reads=[pDT], writes=[ws["eDT"]])
        if need_D:
            pD = ws["ps"].next()
            fw.mm(pD, pD[:, 0:128], ws["gtri"], ws["gtri"][:], cmask, LS[d])
            fw.op("act", lambda g: g.activation(out=ws["eD"][:], in_=pD[:, 0:128], func=AF.Exp), reads=[pD], writes=[ws["eD"]])
        pb = ws["ps"].next()
        fw.mm(pb, pb[:, 0:129], ws["grep"], ws["grep"][:], trione[d], trione[d][:])
        fw.op("act", lambda g: g.activation(out=ws["EG"][:], in_=pb[:, 0:128], func=AF.Exp), reads=[pb], writes=[ws["EG"]])
        fw.op("act", lambda g: g.activation(out=ws["sm"][:, 0:1], in_=pb[:, 128:129], func=AF.Exp), reads=[pb], writes=[ws["sm"]])
        fw.op("dve", lambda g: g.tensor_copy(out=ws["sm"][:, 1:2], in_=pb[:, 128:129]), reads=[pb], writes=[ws["sm"]])
        pc = ws["ps"].next()
        fw.mm(pc, pc[:, 0:1], cmask, TRI[d], gsrc, gcol_ap)
        fw.op("act", lambda g: g.activation(out=ws["sm"][:, 2:3], in_=pc[:, 0:1], func=AF.Exp), reads=[pc], writes=[ws["sm"]])
        fw.op("act", lambda g: g.activation(out=ws["sm"][:, 3:4], in_=pc[:, 0:1], func=AF.Exp, scale=-1.0, bias=ws["sm"][:, 1:2]),
              reads=[pc, ws["sm"]], writes=[ws["sm"]])

    def make_ws(sc, tag, names):
        ws = {}
        for n in names:
            ws[n] = sc.sb(tag + n, [128, 128])
        ws["sm"] = sc.sb(tag + "sm", [128, 8])
        return ws

    def phase_gdn(l, heads=range(NH)):
        last = (l == DEPTH - 1)
        with fw.scope() as sc:
            AB = sc.sb("AB", [128, NCH, 24])
            fw.dma("sp", AB[:], UT[:, T_GAB:T_GAB + 24].rearrange("(n p) c -> p n c", p=128), reads=[b_UT], writes=[AB], key=AB)
            gpar = sc.sb("gpar", [128, 24])
            fw.dma("sp", gpar[:], gpar_in[l:l + 1, :].partition_broadcast(128)[:, 0, :], writes=[gpar], key=gpar)
            G = sc.sb("G", [128, NCH, 12])
            NB = sc.sb("NB", [128, NCH, 12])
            BETA = sc.sb("BETA", [128, NCH, 12])
            fw.op("dve", lambda g: g.tensor_tensor(out=G[:], in0=AB[:, :, 0:12], in1=gpar[:, 12:24].unsqueeze(1).to_broadcast([128, NCH, 12]), op=ALU.add),
                  reads=[AB, gpar], writes=[G])
            fw.op("act", lambda g: g.activation(out=G[:], in_=G[:], func=AF.Exp), reads=[G], writes=[G])
            fw.op("act", lambda g: g.activation(out=G[:], in_=G[:], func=AF.Ln, bias=ones[:, 0:1]), reads=[G, ones], writes=[G])
            fw.op("act", lambda g: g.activation(out=gpar[:, 0:12], in_=gpar[:, 0:12], func=AF.Exp), reads=[gpar], writes=[gpar])
            fw.op("dve", lambda g: g.scalar_tensor_tensor(out=G[:], in0=G[:], scalar=-1.0, in1=gpar[:, 0:12].unsqueeze(1).to_broadcast([128, NCH, 12]),
                                                         op0=ALU.mult, op1=ALU.mult), reads=[G, gpar], writes=[G])
            fw.op("act", lambda g: g.activation(out=BETA[:], in_=AB[:, :, 12:24], func=AF.Sigmoid), reads=[AB], writes=[BETA])
            fw.op("dve", lambda g: g.tensor_scalar(out=NB[:], in0=BETA[:], scalar1=-1.0, scalar2=None, op0=ALU.mult), reads=[BETA], writes=[NB])
            cw = sc.sb("cw", [128, 18, 5])
            fw.dma("sp", cw[:], gconv_in[l, :, :, :], writes=[cw], key=cw)

            qkv_raw = [sc.sb("raw%d" % i, [128, NTOK]) for i in range(3)]
            qkv = [sc.sb("qkv%d" % i, [128, NTOK]) for i in range(3)]
            psl = [sc.ps("gps%d" % i, [128, 512]) for i in range(8)]
            PS = RR(psl)
            S = [sc.sb("S%d" % d, [128, 128]) for d in range(2)]
            names = ["gtri", "grep", "eDT", "eD", "EG", "Ktok", "Vtok", "kkm", "qkTm", "P", "PT", "P2", "P2T", "TT",
                     "AqkT", "QsT", "Ke", "bV", "R", "U", "O"]
            WS = []
            for i in range(2):
                w_ = make_ws(sc, "w%d" % i, names)
                w_["ps"] = PS
                WS.append(w_)
            cengs = RR(["dve"])
            for h in heads:
                for i in range(3):
                    r0 = i * DG + h * 128
                    fw.dma("sp" if i != 1 else "act", qkv_raw[i][:], UF[r0:r0 + 128, :], reads=[b_UF], writes=[qkv_raw[i]], key=qkv_raw[i])
                for i in range(3):
                    e = cengs.next()
                    src, dst = qkv_raw[i], qkv[i]
                    wcol = lambda j, i=i: cw[:, i * 6 + h, j:j + 1]
                    fw.op(e, lambda g, src=src, dst=dst: g.tensor_scalar(out=dst[:], in0=src[:], scalar1=wcol(2), scalar2=None, op0=ALU.mult),
                          reads=[src, cw], writes=[dst])
                    for j in (0, 1, 3, 4):
                        off = j - 2
                        a, b = max(0, -off), CTXL - max(0, off)
                        fw.op(e, lambda g, src=src, dst=dst, a=a, b=b, off=off, j=j: g.scalar_tensor_tensor(
                            out=dst[:, a:b], in0=src[:, a + off:b + off], scalar=wcol(j), in1=dst[:, a:b], op0=ALU.mult, op1=ALU.add),
                            reads=[src, cw, dst], writes=[dst])
                        a, b = max(0, -off), 64 - max(0, off)
                        sv = src[:, CTXL:].rearrange("p (r c) -> p r c", c=64)
                        dv = dst[:, CTXL:].rearrange("p (r c) -> p r c", c=64)
                        fw.op(e, lambda g, sv=sv, dv=dv, a=a, b=b, off=off, j=j, src=src, dst=dst: g.scalar_tensor_tensor(
                            out=dv[:, :, a:b], in0=sv[:, :, a + off:b + off], scalar=wcol(j), in1=dv[:, :, a:b], op0=ALU.mult, op1=ALU.add),
                            reads=[src, cw, dst], writes=[dst])
                    fw.op("act", lambda g, dst=dst: g.activation(out=dst[:], in_=dst[:], func=AF.Silu), reads=[dst], writes=[dst])
                for i in range(2):
                    x_, sq = qkv[i], qkv_raw[i]
                    fw.op("act", lambda g, x_=x_, sq=sq: g.activation(out=sq[:], in_=x_[:], func=AF.Square), reads=[x_], writes=[sq])
                    for t0 in range(0, NTOK, 512):
                        nt = min(512, NTOK - t0)
                        p = PS.next()
                        fw.mm(p, p[:, 0:nt], ones, ones[:], sq, sq[:, t0:t0 + nt])
                        fw.op("act", lambda g, p=p, sq=sq, t0=t0, nt=nt: g.activation(out=sq[:, t0:t0 + nt], in_=p[:, 0:nt], func=AF.Sqrt, bias=epsc[:, 0:1]),
                              reads=[p, epsc, sq], writes=[sq])
                    fw.op("dve", lambda g, sq=sq: g.reciprocal(out=sq[:], in_=sq[:]), reads=[sq], writes=[sq])
                    scl = HD ** -0.5 if i == 0 else 1.0
                    fw.op("dve", lambda g, x_=x_, sq=sq, scl=scl: g.scalar_tensor_tensor(out=x_[:], in0=x_[:], scalar=scl, in1=sq[:], op0=ALU.mult, op1=ALU.mult),
                          reads=[x_, sq], writes=[x_])
                qT, kT, vT = qkv
                for d in range(2):
                    fw.op("dve", lambda g, d=d: g.memset(S[d][:], 0.0), writes=[S[d]])
                orders = [chunk_order(0), chunk_order(1)]
                for step in range(NCH):
                    for d in range(2):
                        n = orders[d][step]
                        ws = WS[d]
                        u = d * 6 + h
                        cs = slice(n * 128, (n + 1) * 128)
                        gcol = G[:, n, u:u + 1]
                        decay_prep(ws, gcol, G, d, True)
                        p = PS.next()
                        fw.tr(p, p[:, 0:128], kT, kT[:, cs], ident)
                        fw.tr(p, p[:, 128:256], vT, vT[:, cs], ident)
                        fw.op("act", lambda g, p=p, ws=ws: g.activation(out=ws["Ke"][:], in_=p[:, 0:128], func=AF.Copy, scale=ws["sm"][:, 3:4]),
                              reads=[p, ws["sm"]], writes=[ws["Ke"]])
                        fw.op("dve", lambda g, p=p, ws=ws, n=n, u=u: g.tensor_scalar(out=ws["bV"][:], in0=p[:, 128:256], scalar1=BETA[:, n, u:u + 1], scalar2=None, op0=ALU.mult),
                              reads=[p, BETA], writes=[ws["bV"]])
                        fw.op("dve", lambda g, ws=ws, n=n, u=u: g.tensor_tensor(out=ws["sm"][:, 4:5], in0=ws["sm"][:, 2:3], in1=NB[:, n, u:u + 1], op=ALU.mult),
                              reads=[ws["sm"], NB], writes=[ws["sm"]])
                        p = PS.next()
                        fw.mm(p, p[:, 0:128], kT, kT[:, cs], kT, kT[:, cs])
                        fw.mm(p, p[:, 128:256], kT, kT[:, cs], qT, qT[:, cs])
                        fw.op("dve", lambda g, p=p, ws=ws, d=d: g.tensor_tensor(out=ws["kkm"][:], in0=p[:, 0:128], in1=LS[d], op=ALU.mult),
                              reads=[p, cmask], writes=[ws["kkm"]])
                        fw.op("dve", lambda g, p=p, ws=ws, d=d: g.tensor_tensor(out=ws["qkTm"][:], in0=p[:, 128:256], in1=LIT[d], op=ALU.mult),
                              reads=[p, cmask], writes=[ws["qkTm"]])
                        fw.op("dve", lambda g, ws=ws, n=n, u=u: g.scalar_tensor_tensor(out=ws["P"][:], in0=ws["eD"][:], scalar=NB[:, n, u:u + 1], in1=ws["kkm"][:],
                                                                                     op0=ALU.mult, op1=ALU.mult), reads=[ws["eD"], NB, ws["kkm"]], writes=[ws["P"]])
                        fw.op("pool", lambda g, ws=ws: g.tensor_tensor(out=ws["AqkT"][:], in0=ws["eDT"][:], in1=ws["qkTm"][:], op=ALU.mult),
                              reads=[ws["eDT"], ws["qkTm"]], writes=[ws["AqkT"]])
                        fw.op("pool", lambda g, ws=ws, cs=cs: g.tensor_tensor(out=ws["QsT"][:], in0=qT[:, cs], in1=ws["EG"][:], op=ALU.mult),
                              reads=[qT, ws["EG"]], writes=[ws["QsT"]])
                        p = PS.next()
                        fw.tr(p, p[:, 0:128], ws["P"], ws["P"][:], ident)
                        fw.op("act", lambda g, p=p, ws=ws: g.copy(out=ws["PT"][:], in_=p[:, 0:128]), reads=[p], writes=[ws["PT"]])
                        fw.op("dve", lambda g, p=p, ws=ws: g.tensor_tensor(out=ws["TT"][:], in0=p[:, 0:128], in1=ident[:], op=ALU.add),
                              reads=[p, ident], writes=[ws["TT"]])
                        Pc, PTc, Pn, PTn = "P", "PT", "P2", "P2T"
                        for lev in range(1, 7):
                            p = PS.next()
                            fw.mm(p, p[:, 0:128], ws[PTc], ws[PTc][:], ws[Pc], ws[Pc][:])
                            if lev < 6:
                                fw.mm(p, p[:, 128:256], ws[Pc], ws[Pc][:], ws[PTc], ws[PTc][:])
                            fw.op("act", lambda g, p=p, ws=ws, Pn=Pn: g.copy(out=ws[Pn][:], in_=p[:, 0:128]), reads=[p], writes=[ws[Pn]])
                            if lev < 6:
                                fw.op("dve", lambda g, p=p, ws=ws, PTn=PTn: g.tensor_copy(out=ws[PTn][:], in_=p[:, 128:256]), reads=[p], writes=[ws[PTn]])
                            p2 = PS.next()
                            fw.mm(p2, p2[:, 0:128], ws[Pn], ws[Pn][:], ws["TT"], ws["TT"][:])
                            fw.op("dve", lambda g, p2=p2, ws=ws: g.tensor_tensor(out=ws["TT"][:], in0=p2[:, 0:128], in1=ws["TT"][:], op=ALU.add),
                                  reads=[p2, ws["TT"]], writes=[ws["TT"]])
                            Pc, PTc, Pn, PTn = Pn, PTn, Pc, PTc
                        p = PS.next()
                        fw.mm(p, p[:, 0:128], kT, kT[:, cs], S[d], S[d][:])
                        fw.op("dve", lambda g, p=p, ws=ws: g.scalar_tensor_tensor(out=ws["R"][:], in0=p[:, 0:128], scalar=ws["sm"][:, 4:5], in1=ws["bV"][:],
                                                                               op0=ALU.mult, op1=ALU.add), reads=[p, ws["sm"], ws["bV"]], writes=[ws["R"]])
                        fw.mm(p, p[:, 128:256], ws["TT"], ws["TT"][:], ws["R"], ws["R"][:])
                        fw.op("act", lambda g, p=p, ws=ws: g.copy(out=ws["U"][:], in_=p[:, 128:256]), reads=[p], writes=[ws["U"]])
                        fw.mm(p, p[:, 256:384], ws["QsT"], ws["QsT"][:], S[d], S[d][:], start=True, stop=False)
                        fw.mm(p, p[:, 256:384], ws["AqkT"], ws["AqkT"][:], ws["U"], ws["U"][:], start=False, stop=True)
                        fw.mm(p, p[:, 384:512], ws["Ke"], ws["Ke"][:], ws["U"], ws["U"][:])
                        fw.op("dve", lambda g, p=p, ws=ws, d=d: g.scalar_tensor_tensor(out=S[d][:], in0=S[d][:], scalar=ws["sm"][:, 0:1], in1=p[:, 384:512],
                                                                                    op0=ALU.mult, op1=ALU.add), reads=[p, ws["sm"], S[d]], writes=[S[d]])
                        if not (last and n < 2):
                            fw.op("act", lambda g, p=p, ws=ws: g.copy(out=ws["O"][:], in_=p[:, 256:384]), reads=[p], writes=[ws["O"]])
                            fw.dma("act", OFs[d][n * 128:(n + 1) * 128, h * 128:(h + 1) * 128], ws["O"][:], reads=[ws["O"]], writes=[b_OF[d]], key=ws["O"])

    def finalize(l, kind):
        last = (l == DEPTH - 1)
        zcol = T_GZ if kind == "gdn" else T_MZ
        ycol = Y_G if kind == "gdn" else Y_M
        nw_in = gnorm_in if kind == "gdn" else mnorm_in
        with fw.scope() as sc:
            nwb = sc.sb("nwb", [128, 128])
            fw.dma("sp", nwb[:], nw_in[l:l + 1, :].partition_broadcast(128)[:, 0, :], writes=[nwb], key=nwb)
            A = RR([sc.sb("fa%d" % i, [128, DG]) for i in range(2)])
            B = RR([sc.sb("fb%d" % i, [128, DG]) for i in range(2)])
            Z = RR([sc.sb("fz%d" % i, [128, DG]) for i in range(2)])
            OGt = RR([sc.sb("fo%d" % i, [128, DG]) for i in range(2)])
            SQ = sc.sb("fsq", [128, DG])
            ssm = RR([sc.sb("fss%d" % i, [128, 12]) for i in range(2)])
            for n in (range(2, NCH) if last else range(NCH)):
                a, b_, z, ss = A.next(), B.next(), Z.next(), ssm.next()
                rs = slice(n * 128, (n + 1) * 128)
                fw.dma("sp", a[:], OFs[0][rs, :], reads=[b_OF[0]], writes=[a], key=a)
                fw.dma("sp", b_[:], OFs[1][rs, :], reads=[b_OF[1]], writes=[b_], key=b_)
                fw.dma("sp", z[:], UT[rs, zcol:zcol + DG], reads=[b_UT], writes=[z], key=z)
                fw.op("pool", lambda g, a=a, b_=b_: g.tensor_tensor(out=a[:], in0=a[:], in1=b_[:], op=ALU.add), reads=[a, b_], writes=[a])
                if kind == "ml":
                    og = OGt.next()
                    fw.dma("sp", og[:], UT[rs, T_MO:T_MO + DG], reads=[b_UT], writes=[og], key=og)
                    fw.op("act", lambda g, og=og: g.activation(out=og[:], in_=og[:], func=AF.Sigmoid), reads=[og], writes=[og])
                    fw.op("pool", lambda g, a=a, og=og: g.tensor_tensor(out=a[:], in0=a[:], in1=og[:], op=ALU.mult), reads=[a, og], writes=[a])
                fw.op("act", lambda g, a=a: g.activation(out=SQ[:], in_=a[:], func=AF.Square), reads=[a], writes=[SQ])
                fw.op("dve", lambda g, ss=ss: g.tensor_reduce(out=ss[:, 0:6], in_=SQ[:].rearrange("p (h c) -> p h c", c=128), axis=AX.X, op=ALU.add),
                      reads=[SQ], writes=[ss])
                fw.op("dve", lambda g, ss=ss: g.tensor_scalar(out=ss[:, 0:6], in0=ss[:, 0:6], scalar1=1.0 / HD, scalar2=EPS, op0=ALU.mult, op1=ALU.add),
                      reads=[ss], writes=[ss])
                fw.op("act", lambda g, ss=ss: g.sqrt(out=ss[:, 0:6], in_=ss[:, 0:6]), reads=[ss], writes=[ss])
                fw.op("dve", lambda g, ss=ss: g.reciprocal(out=ss[:, 6:12], in_=ss[:, 0:6]), reads=[ss], writes=[ss])
                a3 = a[:].rearrange("p (h c) -> p h c", c=128)
                fw.op("dve", lambda g, a3=a3, ss=ss, a=a: g.tensor_tensor(out=a3, in0=a3, in1=ss[:, 6:12].unsqueeze(2).to_broadcast([128, 6, 128]), op=ALU.mult),
                      reads=[a, ss], writes=[a])
                fw.op("pool", lambda g, a3=a3, a=a: g.tensor_tensor(out=a3, in0=a3, in1=nwb[:].unsqueeze(1).to_broadcast([128, 6, 128]), op=ALU.mult),
                      reads=[a, nwb], writes=[a])
                fw.op("act", lambda g, z=z: g.activation(out=z[:], in_=z[:], func=AF.Silu), reads=[z], writes=[z])
                fw.op("dve", lambda g, a=a, z=z: g.tensor_tensor(out=a[:], in0=a[:], in1=z[:], op=ALU.mult), reads=[a, z], writes=[a])
                fw.dma("act", Y[rs, ycol:ycol + DG], a[:], reads=[a], writes=[b_Y], key=a)

    def phase_ml(l, heads=range(NH)):
        last = (l == DEPTH - 1)
        with fw.scope() as sc:
            MG = sc.sb("MG", [128, NCH, 24])
            fw.dma("sp", MG[:], UT[:, T_MG:T_MG + 24].rearrange("(n p) c -> p n c", p=128), reads=[b_UT], writes=[MG], key=MG)
            mgb = sc.sb("mgb", [128, 24])
            fw.dma("sp", mgb[:], mgb_in[l:l + 1, :].partition_broadcast(128)[:, 0, :], writes=[mgb], key=mgb)
            fw.op("dve", lambda g: g.tensor_tensor(out=MG[:], in0=MG[:], in1=mgb[:].unsqueeze(1).to_broadcast([128, NCH, 24]), op=ALU.add),
                  reads=[MG, mgb], writes=[MG])
            ELI = sc.sb("ELI", [128, NCH, 12])
            LF = sc.sb("LF", [128, NCH, 12])
            for d in range(2):
                fw.op("act", lambda g, d=d: g.activation(out=ELI[:, :, d * 6:(d + 1) * 6], in_=MG[:, :, d * 12:d * 12 + 6], func=AF.Exp),
                      reads=[MG], writes=[ELI])
                fw.op("act", lambda g, d=d: g.activation(out=LF[:, :, d * 6:(d + 1) * 6], in_=MG[:, :, d * 12 + 6:d * 12 + 12], func=AF.Exp, scale=-1.0),
                      reads=[MG], writes=[LF])
            fw.op("act", lambda g: g.activation(out=LF[:], in_=LF[:], func=AF.Ln, bias=ones[:, 0:1]), reads=[LF, ones], writes=[LF])
            fw.op("dve", lambda g: g.tensor_scalar(out=LF[:], in0=LF[:], scalar1=-1.0, scalar2=None, op0=ALU.mult), reads=[LF], writes=[LF])
            fw.op("dve", lambda g: g.tensor_scalar(out=ELI[:], in0=ELI[:], scalar1=HD ** -0.5, scalar2=None, op0=ALU.mult), reads=[ELI], writes=[ELI])
            qT = sc.sb("mq", [128, NTOK])
            kT = sc.sb("mk", [128, NTOK])
            Vt = sc.sb("mv", [128, NCH, 129])
            fw.op("dve", lambda g: g.memset(Vt[:, :, 128:129], 1.0), writes=[Vt])
            PS = RR([sc.ps("mps%d" % i, [128, 512]) for i in range(8)])
            Cst = [sc.sb("C%d" % d, [128, 129]) for d in range(2)]
            names = ["gtri", "grep", "eDT", "EG", "Ke", "kqTm", "PT", "QsT", "H"]
            WS = []
            for i in range(4):
                w_ = make_ws(sc, "m%d" % i, names)
                w_["ps"] = PS
                WS.append(w_)
            wsi = 0
            for h in heads:
                fw.dma("sp", qT[:], UF[2304 + h * 128:2304 + (h + 1) * 128, :], reads=[b_UF], writes=[qT], key=qT)
                fw.dma("act", kT[:], UF[3072 + h * 128:3072 + (h + 1) * 128, :], reads=[b_UF], writes=[kT], key=kT)
                fw.dma("sp", Vt[:, :, 0:128], UT[:, T_MV + h * 128:T_MV + (h + 1) * 128].rearrange("(n p) c -> p n c", p=128),
                       reads=[b_UT], writes=[Vt], key=Vt)
                for d in range(2):
                    fw.op("dve", lambda g, d=d: g.memset(Cst[d][:], 0.0), writes=[Cst[d]])
                orders = [chunk_order(0), chunk_order(1)]
                for step in range(NCH):
                    for d in range(2):
                        n = orders[d][step]
                        ws = WS[wsi % 4]
                        wsi += 1
                        u = d * 6 + h
                        cs = slice(n * 128, (n + 1) * 128)
                        decay_prep(ws, LF[:, n, u:u + 1], LF, d, False)
                        fw.op("dve", lambda g, ws=ws, n=n, u=u: g.tensor_tensor(out=ws["sm"][:, 4:5], in0=ws["sm"][:, 3:4], in1=ELI[:, n, u:u + 1], op=ALU.mult),
                              reads=[ws["sm"], ELI], writes=[ws["sm"]])
                        p = PS.next()
                        fw.tr(p, p[:, 0:128], kT, kT[:, cs], ident)
                        fw.mm(p, p[:, 128:256], kT, kT[:, cs], qT, qT[:, cs])
                        fw.op("act", lambda g, p=p, ws=ws: g.activation(out=ws["Ke"][:], in_=p[:, 0:128], func=AF.Copy, scale=ws["sm"][:, 4:5]),
                              reads=[p, ws["sm"]], writes=[ws["Ke"]])
                        fw.op("dve", lambda g, p=p, ws=ws, d=d: g.tensor_tensor(out=ws["kqTm"][:], in0=p[:, 128:256], in1=LIT[d], op=ALU.mult),
                              reads=[p, cmask], writes=[ws["kqTm"]])
                        fw.op("dve", lambda g, ws=ws, n=n, u=u: g.scalar_tensor_tensor(out=ws["PT"][:], in0=ws["eDT"][:], scalar=ELI[:, n, u:u + 1], in1=ws["kqTm"][:],
                                                                                     op0=ALU.mult, op1=ALU.mult), reads=[ws["eDT"], ELI, ws["kqTm"]], writes=[ws["PT"]])
                        fw.op("pool", lambda g, ws=ws, cs=cs: g.tensor_tensor(out=ws["QsT"][:], in0=qT[:, cs], in1=ws["EG"][:], op=ALU.mult),
                              reads=[qT, ws["EG"]], writes=[ws["QsT"]])
                        p = PS.next()
                        fw.mm(p, p[:, 0:129], ws["QsT"], ws["QsT"][:], Cst[d], Cst[d][:], start=True, stop=False)
                        fw.mm(p, p[:, 0:129], ws["PT"], ws["PT"][:], Vt, Vt[:, n, :], start=False, stop=True)
                        fw.mm(p, p[:, 256:385], ws["Ke"], ws["Ke"][:], Vt, Vt[:, n, :])
                        fw.op("dve", lambda g, p=p, ws=ws, d=d: g.scalar_tensor_tensor(out=Cst[d][:], in0=Cst[d][:], scalar=ws["sm"][:, 0:1], in1=p[:, 256:385],
                                                                                    op0=ALU.mult, op1=ALU.add), reads=[p, ws["sm"], Cst[d]], writes=[Cst[d]])
                        if not (last and n < 2):
                            fw.op("act", lambda g, p=p, ws=ws: g.activation(out=ws["sm"][:, 7:8], in_=p[:, 128:129], func=AF.Abs), reads=[p], writes=[ws["sm"]])
                            fw.op("dve", lambda g, ws=ws: g.tensor_scalar(out=ws["sm"][:, 5:6], in0=ws["sm"][:, 7:8], scalar1=1.0, scalar2=None, op0=ALU.max),
                                  reads=[ws["sm"]], writes=[ws["sm"]])
                            fw.op("dve", lambda g, ws=ws: g.reciprocal(out=ws["sm"][:, 6:7], in_=ws["sm"][:, 5:6]), reads=[ws["sm"]], writes=[ws["sm"]])
                            fw.op("act", lambda g, p=p, ws=ws: g.activation(out=ws["H"][:], in_=p[:, 0:128], func=AF.Copy, scale=ws["sm"][:, 6:7]),
                                  reads=[p, ws["sm"]], writes=[ws["H"]])
                            fw.dma("act", OFs[d][n * 128:(n + 1) * 128, h * 128:(h + 1) * 128], ws["H"][:], reads=[ws["H"]], writes=[b_OF[d]], key=ws["H"])
        if opts.get("finalize", True):
            finalize(l, "ml")

    def phase_gdn_full(l, **kw):
        phase_gdn(l, **kw)
        if opts.get("finalize", True):
            finalize(l, "gdn")

    CGW = 32
    NCG = DHY // CGW
    S3 = 3 * CGW
    QG = 512 // CGW
    XC = dscr("XC", [NCG, 3, 64, 64 * CGW])
    b_XC = Buf("XC", multi=True)
    RG = 8

    def hy_filter_mlp(sc, l, featT_in, Lf, name):
        featT = sc.sb(name + "featT", [33, Lf])
        fw.dma("sp", featT[:], featT_in[:, :], writes=[featT], key=featT)
        w1 = sc.sb(name + "w1", [33, 64])
        fw.dma("sp", w1[:], hyw1_in[l, :, :], writes=[w1], key=w1)
        w2 = sc.sb(name + "w2", [64, 64])
        fw.dma("sp", w2[:], hyw2_in[l, :, :], writes=[w2], key=w2)
        hp = sc.sb(name + "hp", [64, 8])
        fw.dma("sp", hp[:, 0:3], hyp_in[l, :, :], writes=[hp], key=hp)
        fw.op("dve", lambda g: g.tensor_tensor(out=hp[:, 3:4], in0=hp[:, 0:1], in1=hp[:, 2:3], op=ALU.mult), reads=[hp], writes=[hp])
        fw.op("dve", lambda g: g.tensor_tensor(out=hp[:, 4:5], in0=hp[:, 1:2], in1=hp[:, 2:3], op=ALU.mult), reads=[hp], writes=[hp])
        fw.op("dve", lambda g: g.memset(hp[:, 5:6], -math.pi), writes=[hp])
        h1T = sc.sb(name + "h1T", [64, Lf])
        kt = sc.sb(name + "kt", [64, 512])
        h2T = sc.sb(name + "h2T", [64, Lf])
        pss = RR([sc.ps(name + "fps%d" % i, [64, 512]) for i in range(2)])
        for (wt, kdim, src, dst, bcol) in ((w1, 33, featT, h1T, 3), (w2, 64, h1T, h2T, 4)):
            for t0 in range(0, Lf, 512):
                nt = min(512, Lf - t0)
                p = pss.next()
                fw.mm(p, p[:, 0:nt], wt, wt[0:kdim, :], src, src[0:kdim, t0:t0 + nt])
                fw.op("dve", lambda g, p=p, dst=dst, t0=t0, nt=nt, bcol=bcol: g.tensor_scalar(
                    out=dst[:, t0:t0 + nt], in0=p[:, 0:nt], scalar1=hp[:, 2:3], scalar2=hp[:, bcol:bcol + 1], op0=ALU.mult, op1=ALU.add),
                    reads=[p, hp], writes=[dst])
                fw.op("dve", lambda g, dst=dst, t0=t0, nt=nt: g.tensor_scalar(
                    out=kt[:, 0:nt], in0=dst[:, t0:t0 + nt], scalar1=1.0 / (2.0 * math.pi), scalar2=12582912.0, op0=ALU.mult, op1=ALU.add),
                    reads=[dst], writes=[kt])
                fw.op("dve", lambda g, nt=nt: g.tensor_scalar(out=kt[:, 0:nt], in0=kt[:, 0:nt], scalar1=12582912.0, scalar2=None, op0=ALU.subtract),
                      reads=[kt], writes=[kt])
                fw.op("dve", lambda g, dst=dst, t0=t0, nt=nt: g.scalar_tensor_tensor(
                    out=dst[:, t0:t0 + nt], in0=dst[:, t0:t0 + nt], scalar=1.0 / (2.0 * math.pi), in1=kt[:, 0:nt], op0=ALU.mult, op1=ALU.subtract),
                    reads=[dst, kt], writes=[dst])
                fw.op("act", lambda g, dst=dst, t0=t0, nt=nt: g.activation(out=dst[:, t0:t0 + nt], in_=dst[:, t0:t0 + nt], func=AF.Sin, scale=2.0 * math.pi),
                      reads=[dst], writes=[dst])
        return h2T

    def phase_hy(l, cgs=None):
        cgs = range(NCG) if cgs is None else cgs
        last = (l == DEPTH - 1)
        with fw.scope() as sc:
            h2T = sc.sb("h2Tp", [64, SEQ])
            with fw.scope() as sm_:
                h2tmp = hy_filter_mlp(sm_, l, featL_in, SEQ, "L")
                fw.op("pool", lambda g: g.tensor_copy(out=h2T[:], in_=h2tmp[:]), reads=[h2tmp], writes=[h2T])
            w3t = sc.sb("w3t", [64, 2048])
            fw.dma("sp", w3t[:], hyw3_in[l, :, :], writes=[w3t], key=w3t)
            F1 = sc.sb("F1", [64, 2, 128], BF16)
            fw.dma("sp", F1[:], F1_in[:, :, :], writes=[F1], key=F1)
            F4 = sc.sb("F4", [128, 2, 64], BF16)
            fw.dma("sp", F4[:], F4_in[:, :, :], writes=[F4], key=F4)
            negt = sc.sb("negt", [64, 64])
            fw.dma("sp", negt[:], negt_in[:, :], writes=[negt], key=negt)
            drow = sc.sb("drow", [64, 512])
            fw.dma("sp", drow[:], drow_in[0:1, :].partition_broadcast(64)[:, 0, :], writes=[drow], key=drow)
            skipb = sc.sb("skipb", [64, 2, 512])
            fw.dma("sp", skipb[:], hyskip_in[l:l + 1, :].partition_broadcast(64)[:, 0, :].rearrange("p (o c) -> p o c", o=2), writes=[skipb], key=skipb)
            PS = RR([sc.ps("hps%d" % i, [128, 512]) for i in range(5)])
            PSB = RR([sc.ps("hpb%d" % i, [128, 1024], BF16) for i in range(1)])
            VH = sc.sb("VH", [64, S3, 64], BF16)
            vf = sc.sb("vf", [64, CGW, 64])
            Wt = sc.sb("Wt", [64, CGW, 64])
            Zp = sc.sb("Zp", [128, CGW, 128], BF16)
            evq = RR(["act", "dve"])

            def evac(p_ap, out_ap, ptile, otile, eng=None):
                e = eng or evq.next()
                if e == "act":
                    fw.op("act", lambda g: g.copy(out=out_ap, in_=p_ap), reads=[ptile], writes=[otile])
                else:
                    fw.op(e, lambda g: g.tensor_copy(out=out_ap, in_=p_ap), reads=[ptile], writes=[otile])

            for cg in cgs:
                c0 = cg * CGW
                with fw.scope() as s0:
                    raw = [s0.sb("raw%d" % i, [64, 64, CGW]) for i in range(4)]
                    cv = [s0.sb("cv%d" % i, [64, CGW, 64]) for i in range(3)]
                    tmp = s0.sb("ctmp", [64, 64, CGW])
                    cwb = s0.sb("cwb", [64, 3, 3, CGW])
                    fw.dma("sp", cwb[:], hyconv_in[l, :, :].rearrange("j (a c) -> j a c", a=3)[:, :, c0:c0 + CGW].partition_broadcast(64),
                           writes=[cwb], key=cwb)
                    for a in range(4):
                        col = (T_HP + a * 512 + c0) if a < 3 else (T_HZ + c0)
                        fw.dma("sp" if a % 2 == 0 else "act", raw[a][:],
                               UT[CTXL:, col:col + CGW].rearrange("(p q) c -> p q c", q=64), reads=[b_UT], writes=[raw[a]], key=raw[a])
                    for a in range(3):
                        dst = vf if a == 2 else cv[a]
                        dv = dst[:].rearrange("p c q -> p q c")
                        fw.op("dve", lambda g, a=a, dv=dv, dst=dst: g.tensor_tensor(out=dv, in0=raw[a][:], in1=cwb[:, 1, a, :].unsqueeze(1).to_broadcast([64, 64, CGW]), op=ALU.mult),
                              reads=[raw[a], cwb], writes=[dst])
                        fw.op("pool", lambda g, a=a: g.tensor_tensor(out=tmp[:, 0:63, :], in0=raw[a][:, 0:63, :], in1=cwb[:, 0, a, :].unsqueeze(1).to_broadcast([64, 63, CGW]), op=ALU.mult),
                              reads=[raw[a], cwb], writes=[tmp])
                        fw.op("dve", lambda g, dv=dv, dst=dst: g.tensor_tensor(out=dv[:, 1:64, :], in0=dv[:, 1:64, :], in1=tmp[:, 0:63, :], op=ALU.add),
                              reads=[dst, tmp], writes=[dst])
                        fw.op("pool", lambda g, a=a: g.tensor_tensor(out=tmp[:, 0:63, :], in0=raw[a][:, 1:64, :], in1=cwb[:, 2, a, :].unsqueeze(1).to_broadcast([64, 63, CGW]), op=ALU.mult),
                              reads=[raw[a], cwb], writes=[tmp])
                        fw.op("dve", lambda g, dv=dv, dst=dst: g.tensor_tensor(out=dv[:, 0:63, :], in0=dv[:, 0:63, :], in1=tmp[:, 0:63, :], op=ALU.add),
                              reads=[dst, tmp], writes=[dst])
                    fw.op("act", lambda g: g.activation(out=cv[2][:].rearrange("p c q -> p q c"), in_=raw[3][:], func=AF.Silu), reads=[raw[3]], writes=[cv[2]])
                    for a in range(3):
                        fw.dma("act", XC[cg, a, :, :], cv[a][:].rearrange("p c q -> p (c q)"), reads=[cv[a]], writes=[b_XC], key=cv[a])
                    fw.op("act", lambda g: g.copy(out=VH[:, 0:CGW, :], in_=vf[:]), reads=[vf], writes=[VH])
                    for q in range(64):
                        fw.op("act", lambda g, q=q: g.activation(out=Wt[:, :, q], in_=drow[:, c0:c0 + CGW], func=AF.Exp, scale=negt[:, q:q + 1]),
                              reads=[drow, negt], writes=[Wt])
                for o in range(2):
                    with fw.scope() as sa:
                        Hraw = sa.sb("Hraw", [64, 64, 2 * CGW])
                        Hw = sa.sb("Hw", [64, 2 * CGW, 64])
                        part = sa.sb("part", [64, 2 * CGW])
                        tot = sa.sb("tot", [64, 2 * CGW])
                        h2v = h2T[:, :].rearrange("k (p q) -> k q p", q=64)
                        w3v = w3t[:, o * 1024:(o + 1) * 1024].rearrange("k (d c) -> k d c", d=2)[:, :, c0:c0 + CGW]
                        NQ = 512 // (2 * CGW)
                        for q0 in range(0, 64, NQ):
                            p = PS.next()
                            for j in range(NQ):
                                fw.mm(p, p[0:64, j * 2 * CGW:(j + 1) * 2 * CGW], h2T, h2v[:, q0 + j, :], w3t, w3v)
                            evac(p[0:64, :], Hraw[:, q0:q0 + NQ, :].rearrange("p q c -> p (q c)"), p, Hraw)
                        for d in range(2):
                            fw.op("dve", lambda g, d=d: g.tensor_tensor(out=Hw[:, d * CGW:(d + 1) * CGW, :], in0=Hraw[:, :, d * CGW:(d + 1) * CGW].rearrange("p q c -> p c q"),
                                                                       in1=Wt[:], op=ALU.mult), reads=[Hraw, Wt], writes=[Hw])
                        fw.op("act", lambda g: g.activation(out=Hraw[:].rearrange("p q c -> p (q c)"), in_=Hw[:].rearrange("p c q -> p (c q)"), func=AF.Abs),
                              reads=[Hw], writes=[Hraw])
                        fw.op("dve", lambda g: g.tensor_reduce(out=part[:], in_=Hraw[:].rearrange("p q c -> p (q c)").rearrange("p (c q) -> p c q", q=64), axis=AX.X, op=ALU.add),
                              reads=[Hraw], writes=[part])
                        p = PS.next()
                        fw.mm(p, p[0:64, 0:2 * CGW], ones, ones[0:64, 0:64], part, part[:])
                        fw.op("dve", lambda g, p=p: g.tensor_scalar(out=tot[:], in0=p[0:64, 0:2 * CGW], scalar1=EPS, scalar2=None, op0=ALU.add), reads=[p], writes=[tot])
                        fw.op("dve", lambda g: g.reciprocal(out=tot[:], in_=tot[:]), reads=[tot], writes=[tot])
                        fw.op("dve", lambda g: g.tensor_tensor(out=VH[:, CGW:S3, :], in0=Hw[:], in1=tot[:].unsqueeze(2).to_broadcast([64, 2 * CGW, 64]), op=ALU.mult),
                              reads=[Hw, tot], writes=[VH])
                    with fw.scope() as sb_:
                        Bt = sb_.sb("Bt", [64, S3, 2, 64], BF16)
                        w2s = RR([sb_.sb("w2s%d" % i, [64, RG, 4, 128], BF16) for i in range(2)])
                        w3s = RR([sb_.sb("w3s%d" % i, [128, RG, 128], BF16) for i in range(2)])
                        XS = RR([sb_.sb("XS%d" % i, [128, RG, 2, S3]) for i in range(2)])
                        KA = sb_.sb("KA", [128, RG, CGW])
                        KB = sb_.sb("KB", [128, RG, CGW])
                        Yt = RR([sb_.sb("Yt%d" % i, [128, RG, CGW], BF16) for i in range(2)])
                        for rh in range(2):
                            for s4 in range(0, S3, 4):
                                p = PS.next()
                                for j in range(4):
                                    fw.mm(p, p[0:64, j * 128:(j + 1) * 128], VH, VH[:, s4 + j, :], F1, F1[:, rh, :])
                                evac(p[0:64, :], Bt[:, s4:s4 + 4, :, :].rearrange("q s i r -> q (s i r)"), p, Bt)
                            for rg in range(64 // RG):
                                r0 = rh * 64 + rg * RG
                                w2 = w2s.next()
                                fw.dma("sp", w2[:], W2_in[:, r0:r0 + RG, :, :], writes=[w2], key=w2)
                                w3_ = w3s.next()
                                fw.dma("pool", w3_[:], W3_in[:, r0:r0 + RG, :], writes=[w3_], key=w3_)
                                xs = XS.next()
                                for rl in range(RG):
                                    rloc = rg * RG + rl
                                    p = PS.next()
                                    for v in range(2):
                                        fw.mm(p, p[:, v * 256:v * 256 + S3], w2, w2[:, rl, 2 * v, :], Bt, Bt[:, :, 0, rloc], start=True, stop=False)
                                        fw.mm(p, p[:, v * 256:v * 256 + S3], w2, w2[:, rl, 2 * v + 1, :], Bt, Bt[:, :, 1, rloc], start=False, stop=True)
                                    evac(p[:, :].rearrange("p (v c) -> p v c", v=2)[:, :, 0:S3], xs[:, rl, :, :], p, xs)
                                fw.op("pool", lambda g, xs=xs: g.tensor_tensor(out=KA[0:64], in0=xs[0:64, :, 0, CGW:2 * CGW], in1=xs[0:64, :, 0, 2 * CGW:S3], op=ALU.add), reads=[xs], writes=[KA])
                                fw.op("pool", lambda g, xs=xs: g.tensor_tensor(out=KA[64:128], in0=xs[64:128, :, 1, CGW:2 * CGW], in1=xs[64:128, :, 1, 2 * CGW:S3], op=ALU.add), reads=[xs], writes=[KA])
                                fw.op("pool", lambda g, xs=xs: g.tensor_tensor(out=KB[0:64], in0=xs[0:64, :, 1, 2 * CGW:S3], in1=xs[0:64, :, 1, CGW:2 * CGW], op=ALU.subtract), reads=[xs], writes=[KB])
                                fw.op("pool", lambda g, xs=xs: g.tensor_tensor(out=KB[64:128], in0=xs[64:128, :, 0, CGW:2 * CGW], in1=xs[64:128, :, 0, 2 * CGW:S3], op=ALU.subtract), reads=[xs], writes=[KB])
                                fw.op("dve", lambda g, xs=xs: g.tensor_tensor(out=KA[:], in0=KA[:], in1=xs[:, :, 0, 0:CGW], op=ALU.mult), reads=[KA, xs], writes=[KA])
                                fw.op("dve", lambda g, xs=xs: g.tensor_tensor(out=KB[:], in0=KB[:], in1=xs[:, :, 1, 0:CGW], op=ALU.mult), reads=[KB, xs], writes=[KB])
                                yt = Yt.next()
                                fw.op("dve", lambda g, yt=yt: g.tensor_tensor(out=yt[:], in0=KA[:], in1=KB[:], op=ALU.add), reads=[KA, KB], writes=[yt])
                                p = PS.next()
                                for rl in range(RG):
                                    fw.mm(p, p[:, rl * CGW:(rl + 1) * CGW], w3_, w3_[:, rl, :], yt, yt[:, rl, :])
                                evac(p[:, 0:RG * CGW].rearrange("p (r c) -> p r c", c=CGW), Zp[:, :, r0:r0 + RG].rearrange("p c r -> p r c"), p, Zp)
                    with fw.scope() as sc_:
                        ZT = sc_.sb("ZT", [128, CGW, 2, 64], BF16)
                        xo = sc_.sb("xo", [64, CGW, 64])
                        sv = sc_.sb("sv", [64, CGW, 64])
                        fw.dma("sp", xo[:].rearrange("p c q -> p (c q)"), XC[cg, o, :, :], reads=[b_XC], writes=[xo], key=xo)
                        fw.op("pool", lambda g: g.tensor_tensor(out=sv[:], in0=vf[:], in1=skipb[:, o, c0:c0 + CGW].unsqueeze(2).to_broadcast([64, CGW, 64]), op=ALU.mult),
                              reads=[vf, skipb], writes=[sv])
                        if o == 1:
                            sz = sc_.sb("sz", [64, CGW, 64])
                            yo = sc_.sb("yo", [64, 64, CGW])
                            fw.dma("act", sz[:].rearrange("p c q -> p (c q)"), XC[cg, 2, :, :], reads=[b_XC], writes=[sz], key=sz)
                            fw.op("pool", lambda g: g.tensor_tensor(out=xo[:], in0=xo[:], in1=sz[:], op=ALU.mult), reads=[xo, sz], writes=[xo])
                        for c8 in range(0, CGW, 8):
                            pb = PSB.next()
                            for j in range(8):
                                fw.tr(pb, pb[:, j * 128:(j + 1) * 128], Zp, Zp[:, c8 + j, :], identb)
                            evac(pb[:, :], ZT[:, c8:c8 + 8, :, :].rearrange("r c i q -> r (c i q)"), pb, ZT)
                        for q0 in range(0, 64, QG):
                            p = PS.next()
                            fw.mm(p, p[0:64, :], F4, F4[:, 0, :], ZT, ZT[:, :, 0, q0:q0 + QG], start=True, stop=False)
                            fw.mm(p, p[0:64, :], F4, F4[:, 1, :], ZT, ZT[:, :, 1, q0:q0 + QG], start=False, stop=True)
                            pv = p[0:64, :].rearrange("p (c q) -> p c q", q=QG)
                            fw.op("dve", lambda g, pv=pv, q0=q0: g.tensor_tensor(out=sv[:, :, q0:q0 + QG], in0=pv, in1=sv[:, :, q0:q0 + QG], op=ALU.add), reads=[p, sv], writes=[sv])
                            if o == 0:
                                fw.op("pool", lambda g, q0=q0: g.tensor_tensor(out=vf[:, :, q0:q0 + QG], in0=sv[:, :, q0:q0 + QG], in1=xo[:, :, q0:q0 + QG], op=ALU.mult),
                                      reads=[sv, xo], writes=[vf])
                            else:
                                fw.op("pool", lambda g, q0=q0: g.tensor_tensor(out=yo[:, q0:q0 + QG, :].rearrange("p q c -> p c q"), in0=sv[:, :, q0:q0 + QG], in1=xo[:, :, q0:q0 + QG], op=ALU.mult),
                                      reads=[sv, xo], writes=[yo])
                        if o == 0:
                            fw.op("act", lambda g: g.copy(out=VH[:, 0:CGW, :], in_=vf[:]), reads=[vf], writes=[VH])
                        else:
                            fw.dma("act", Y[CTXL:, Y_H + c0:Y_H + c0 + CGW].rearrange("(p q) c -> p q c", q=64), yo[:], reads=[yo], writes=[b_Y], key=yo)

    def phase_hy_ctx(l):
        with fw.scope() as sc:
            h2Tc = hy_filter_mlp(sc, l, featC_in, CTXL, "C")
            w3c = sc.sb("w3c", [64, 2048])
            fw.dma("sp", w3c[:], hyw3_in[l, :, :], writes=[w3c], key=w3c)
            negtc = sc.sb("negtc", [128, CTXL])
            fw.dma("sp", negtc[:], negtc_in[0:1, :].partition_broadcast(128)[:, 0, :], writes=[negtc], key=negtc)
            dcol = sc.sb("dcol", [128, 4])
            fw.dma("sp", dcol[:], dcol_in[:, :], writes=[dcol], key=dcol)
            cwT = sc.sb("cwT", [128, 12, 3])
            fw.dma("sp", cwT[:], hyconvT_in[l, :, :, :], writes=[cwT], key=cwT)
            skc = sc.sb("skc", [128, 2, 4])
            fw.dma("sp", skc[:], hyskipT_in[l, :, :, :], writes=[skc], key=skc)
            PS = RR([sc.ps("cps%d" % i, [128, 512]) for i in range(4)])
            XT = sc.sb("XT", [128, 12, CTXL])
            XCc = sc.sb("XCc", [128, 12, CTXL])
            zt = [sc.sb("zt%d" % i, [128, 512]) for i in range(2)]
            yo = [sc.sb("yoc%d" % i, [128, 512]) for i in range(2)]
            tl = sc.sb("ctile", [128, 2048])
            for tt in range(2):
                fw.dma("sp", tl[:], UT[tt * 128:(tt + 1) * 128, T_HP:T_HP + 2048], reads=[b_UT], writes=[tl], key=tl)
                for g4 in range(3):
                    p = PS.next()
                    for j in range(4):
                        gi = g4 * 4 + j
                        fw.tr(p, p[:, j * 128:(j + 1) * 128], tl, tl[:, gi * 128:(gi + 1) * 128], ident)
                    fw.op("act", lambda g, p=p, g4=g4, tt=tt: g.copy(out=XT[:, g4 * 4:(g4 + 1) * 4, tt * 128:(tt + 1) * 128],
                                                                   in_=p[:, :].rearrange("p (g t) -> p g t", g=4)), reads=[p], writes=[XT])
                fw.op("act", lambda g, tt=tt: g.activation(out=zt[tt][:], in_=tl[:, 1536:2048], func=AF.Silu), reads=[tl], writes=[zt[tt]])
            for gi in range(12):
                fw.op("dve", lambda g, gi=gi: g.tensor_scalar(out=XCc[:, gi, :], in0=XT[:, gi, :], scalar1=cwT[:, gi, 1:2], scalar2=None, op0=ALU.mult),
                      reads=[XT, cwT], writes=[XCc])
                fw.op("dve", lambda g, gi=gi: g.scalar_tensor_tensor(out=XCc[:, gi, 1:CTXL], in0=XT[:, gi, 0:CTXL - 1], scalar=cwT[:, gi, 0:1], in1=XCc[:, gi, 1:CTXL],
                                                                    op0=ALU.mult, op1=ALU.add), reads=[XT, cwT, XCc], writes=[XCc])
                fw.op("dve", lambda g, gi=gi: g.scalar_tensor_tensor(out=XCc[:, gi, 0:CTXL - 1], in0=XT[:, gi, 1:CTXL], scalar=cwT[:, gi, 2:3], in1=XCc[:, gi, 0:CTXL - 1],
                                                                    op0=ALU.mult, op1=ALU.add), reads=[XT, cwT, XCc], writes=[XCc])
            Wc = sc.sb("Wc", [128, CTXL])
            hf = [sc.sb("hf%d" % i, [128, CTXL]) for i in range(2)]
            habs = sc.sb("habs", [128, CTXL])
            hs = sc.sb("hs", [128, 4])
            acc = sc.sb("acc", [128, CTXL])
            acc2 = sc.sb("acc2", [128, CTXL])
            tmpc = sc.sb("tmpc", [128, CTXL])
            vcur = sc.sb("vcur", [128, CTXL])
            for gi in range(4):
                fw.op("act", lambda g, gi=gi: g.activation(out=Wc[:], in_=negtc[:], func=AF.Exp, scale=dcol[:, gi:gi + 1]), reads=[negtc, dcol], writes=[Wc])
                fw.op("pool", lambda g, gi=gi: g.tensor_copy(out=vcur[:], in_=XCc[:, 8 + gi, :]), reads=[XCc], writes=[vcur])
                for o in range(2):
                    for dr in range(2):
                        col0 = o * 1024 + dr * 512 + gi * 128
                        p = PS.next()
                        fw.mm(p, p[:, 0:CTXL], w3c, w3c[:, col0:col0 + 128], h2Tc, h2Tc[:, :])
                        fw.op("dve", lambda g, p=p, dr=dr: g.tensor_tensor(out=hf[dr][:], in0=p[:, 0:CTXL], in1=Wc[:], op=ALU.mult), reads=[p, Wc], writes=[hf[dr]])
                        fw.op("act", lambda g, dr=dr: g.activation(out=habs[:], in_=hf[dr][:], func=AF.Abs), reads=[hf[dr]], writes=[habs])
                        fw.op("dve", lambda g: g.tensor_reduce(out=hs[:, 0:1], in_=habs[:], axis=AX.X, op=ALU.add), reads=[habs], writes=[hs])
                        fw.op("dve", lambda g: g.tensor_scalar(out=hs[:, 1:2], in0=hs[:, 0:1], scalar1=EPS, scalar2=None, op0=ALU.add), reads=[hs], writes=[hs])
                        fw.op("dve", lambda g: g.reciprocal(out=hs[:, 2:3], in_=hs[:, 1:2]), reads=[hs], writes=[hs])
                        fw.op("dve", lambda g, dr=dr: g.tensor_scalar(out=hf[dr][:], in0=hf[dr][:], scalar1=hs[:, 2:3], scalar2=None, op0=ALU.mult), reads=[hf[dr], hs], writes=[hf[dr]])
                    fw.op("dve", lambda g, o=o, gi=gi: g.tensor_scalar(out=acc[:], in0=vcur[:], scalar1=skc[:, o, gi:gi + 1], scalar2=None, op0=ALU.mult),
                          reads=[vcur, skc], writes=[acc])
                    fw.op("pool", lambda g: g.memset(acc2[:], 0.0), writes=[acc2])
                    for m in range(CTXL):
                        n_ = CTXL - m
                        fw.op("dve", lambda g, m=m, n_=n_: g.scalar_tensor_tensor(out=acc[:, m:], in0=vcur[:, 0:n_], scalar=hf[0][:, m:m + 1], in1=acc[:, m:],
                                                                              op0=ALU.mult, op1=ALU.add), reads=[vcur, hf[0], acc], writes=[acc])
                        fw.op("pool", lambda g, m=m, n_=n_: g.tensor_scalar(out=tmpc[:, 0:n_], in0=vcur[:, m:], scalar1=hf[1][:, m:m + 1], scalar2=None, op0=ALU.mult),
                              reads=[vcur, hf[1]], writes=[tmpc])
                        fw.op("pool", lambda g, n_=n_: g.tensor_tensor(out=acc2[:, 0:n_], in0=acc2[:, 0:n_], in1=tmpc[:, 0:n_], op=ALU.add),
                              reads=[acc2, tmpc], writes=[acc2])
                    fw.op("dve", lambda g: g.tensor_tensor(out=acc[:], in0=acc[:], in1=acc2[:], op=ALU.add), reads=[acc, acc2], writes=[acc])
                    fw.op("dve", lambda g, o=o, gi=gi: g.tensor_tensor(out=vcur[:], in0=acc[:], in1=XCc[:, o * 4 + gi, :], op=ALU.mult), reads=[acc, XCc], writes=[vcur])
                for tt in range(2):
                    p = PS.next()
                    fw.tr(p, p[:, 0:128], vcur, vcur[:, tt * 128:(tt + 1) * 128], ident)
                    fw.op("dve", lambda g, p=p, tt=tt, gi=gi: g.tensor_tensor(out=yo[tt][:, gi * 128:(gi + 1) * 128], in0=p[:, 0:128], in1=zt[tt][:, gi * 128:(gi + 1) * 128], op=ALU.mult),
                          reads=[p, zt[tt]], writes=[yo[tt]])
            for tt in range(2):
                fw.dma("act", Y[tt * 128:(tt + 1) * 128, Y_H:Y_H + DHY], yo[tt][:], reads=[yo[tt]], writes=[b_Y], key=yo[tt])

    def phase_hy_full(l, **kw):
        phase_hy(l, **kw)
        if l != DEPTH - 1 and opts.get("hyctx", True):
            phase_hy_ctx(l)
    scr = {"UF": (UF, b_UF), "UT": (UT, b_UT), "Y": (Y, b_Y), "xcur": (xcur, b_xcur), "ctxcur": (ctxcur, b_ctxcur),
           "OF": (OFs[0], b_OF[0]), "OB": (OFs[1], b_OF[1])}
    for key, (name, sl) in dbg_in.items():
        dst, dbuf = scr[name]
        dst = dst[sl]
        src = din("dbgin_" + key, list(dst.shape))
        fw.dma("sp", dst, src, writes=[dbuf], key=dbuf)
    PH = {"mod": phase_mod, "proj": phase_proj, "out": phase_out, "gdn": phase_gdn_full, "ml": phase_ml, "hy": phase_hy_full}
    for l in layers:
        for ph in ("mod", "proj", "gdn", "hy", "ml", "out"):
            if ph in phases and ph in PH:
                PH[ph](l, **opts.get(ph, {}))
    bd = Buf("dbg", multi=True)
    for name, sl in dbg.items():
        src, sbuf = scr[name]
        if sl != 1:
            src = src[sl]
        dt_ = dbg_tensor(name, src.shape)
        fw.dma("sp", dt_, src, reads=[sbuf], writes=[bd], key=bd)
    fw.fence([b_out, bd], engines=("sp",))
    return nc, fw


_HYC = {}


def make_hy_consts():
    if _HYC:
        return _HYC
    import ml_dtypes
    bf = ml_dtypes.bfloat16
    f = np.float32
    N = 2 * SEQ

    def feats(L):
        pos = np.arange(L, dtype=f)
        t = pos / f(max(L - 1, 1))
        ang = (f(2.0 * math.pi) * pos / f(L)).astype(f)
        bands = np.linspace(1e-4, 15, 16, dtype=f)
        ft = np.concatenate([t[:, None], np.cos(ang[:, None] * bands), -np.sin(ang[:, None] * bands)], axis=-1).astype(f)
        return np.ascontiguousarray(ft.T), t
    featL, tL = feats(SEQ)
    featC, tC = feats(CTXL)
    mind, maxd = math.log(1e-2) / 1.5, math.log(1e-2) / 0.3
    deltas = np.abs(np.linspace(mind, maxd, DHY, dtype=f)).astype(f)
    p = np.arange(64, dtype=np.float64)[:, None]
    r = np.arange(128, dtype=np.float64)[None, :]
    ang = 2 * np.pi * p * r / 128
    F1 = np.stack([np.concatenate([np.cos(ang[:, h * 64:(h + 1) * 64]), -np.sin(ang[:, h * 64:(h + 1) * 64])], axis=1) for h in range(2)], axis=1)
    q = np.arange(64, dtype=np.float64)[:, None, None]
    rr = np.arange(128, dtype=np.float64)[None, :, None]
    s_ = np.arange(64, dtype=np.float64)[None, None, :]
    th = 2 * np.pi * (q * s_ / 64 + q * rr / N)
    c, d = np.cos(th), -np.sin(th)
    W2 = np.stack([np.concatenate([c, d], -1), np.concatenate([-d, c], -1),
                   np.concatenate([-d, c], -1), np.concatenate([-c, -d], -1)], axis=2)
    s2 = np.arange(64, dtype=np.float64)[:, None, None]
    q2 = np.arange(64, dtype=np.float64)[None, None, :]
    th2 = 2 * np.pi * (q2 * s2 / 64 + q2 * rr / N)
    c2, d2 = np.cos(th2), np.sin(th2)
    W3 = np.concatenate([np.concatenate([c2, d2], -1), np.concatenate([-d2, c2], -1)], axis=0)
    r4 = np.arange(128, dtype=np.float64)[:, None]
    p4 = np.arange(64, dtype=np.float64)[None, :]
    a4 = 2 * np.pi * r4 * p4 / 128
    F4 = np.stack([np.cos(a4) / N, -np.sin(a4) / N], axis=1)
    _HYC.update({
        "featL": featL, "featC": featC,
        "negt": np.ascontiguousarray(-tL.reshape(64, 64)), "negtc": np.ascontiguousarray(-tC.reshape(1, CTXL)),
        "drow": np.ascontiguousarray(deltas.reshape(1, DHY)), "dcol": np.ascontiguousarray(deltas.reshape(4, 128).T),
        "F1": np.ascontiguousarray(F1.astype(f)).astype(bf), "F4": np.ascontiguousarray(F4.astype(f)).astype(bf),
        "W2": np.ascontiguousarray(W2.astype(f)).astype(bf), "W3": np.ascontiguousarray(W3.astype(f)).astype(bf),
        "identb": np.eye(128, dtype=f).astype(bf),
    })
    return _HYC


def make_cmask():
    i = np.arange(128)
    P_, F_ = i[:, None], i[None, :]
    m = np.stack([P_ <= F_, P_ >= F_, F_ < P_, F_ > P_, F_ >= P_, F_ <= P_], axis=1)
    return np.ascontiguousarray(m.astype(np.float32))


def make_in_maps(inputs, cores=range(8)):
    f = np.float32
    g = lambda k: np.asarray(inputs[k], dtype=f)
    x, c, ctx, c_ctx = g("x"), g("c"), g("ctx"), g("c_ctx")
    norm_w, mod_w, mod_b, w_in, w_out = g("norm_w"), g("mod_w"), g("mod_b"), g("w_in"), g("w_out")

    def pk(a):
        L_, _, N = a.shape
        return np.ascontiguousarray(a.reshape(L_, KC, 128, N).transpose(0, 2, 1, 3))

    shared = {
        "nwT": np.ascontiguousarray(norm_w.reshape(DEPTH, KC, 128).transpose(2, 0, 1)),
        "fnw": np.ascontiguousarray(g("final_norm").reshape(1, D)),
        "modw": pk(mod_w),
        "modbT": np.ascontiguousarray(mod_b[:, :2 * D].reshape(DEPTH, 32, 128).transpose(2, 0, 1)),
        "modbg": np.ascontiguousarray(mod_b[:, 2 * D:]),
        "winF": pk(w_in[:, :, FCOLS]),
        "winT": pk(w_in[:, :, TCOLS]),
        "wout": pk(w_out),
        "ident": np.eye(128, dtype=f),
        "cmask": make_cmask(),
        **make_hy_consts(),
        "hyw1": g("hy_w1"), "hyw2": g("hy_w2"), "hyw3": g("hy_w3"),
        "hyp": np.ascontiguousarray(np.stack([g("hy_b1"), g("hy_b2"), g("hy_freq")], axis=-1)),
        "hyskip": np.ascontiguousarray(g("hy_skip").reshape(DEPTH, 1024)),
        "hyconv": g("hy_conv"),
        "hyskipT": np.ascontiguousarray(g("hy_skip").reshape(DEPTH, 2, 4, 128).transpose(0, 3, 1, 2)),
        "hyconvT": np.ascontiguousarray(g("hy_conv").transpose(0, 2, 1).reshape(DEPTH, 12, 128, 3).transpose(0, 2, 1, 3)),
        "gnorm": g("gdn_norm"), "mnorm": g("ml_norm"), "mgb": np.ascontiguousarray(g("ml_gate_bias").reshape(DEPTH, 24)),
        "gpar": np.ascontiguousarray(np.concatenate([g("gdn_a_log").reshape(DEPTH, 12), g("gdn_dt_bias").reshape(DEPTH, 12)], axis=1)),
        "gconv": np.ascontiguousarray(g("gdn_conv").transpose(0, 2, 1).reshape(DEPTH, 18, 128, 5).transpose(0, 2, 1, 3)),
    }
    maps = []
    for b in cores:
        cc = np.stack([c[b], c_ctx], axis=-1)
        m = dict(shared)
        m["x"] = np.ascontiguousarray(x[b])
        m["ctx"] = np.ascontiguousarray(ctx[b])
        m["cT"] = np.ascontiguousarray(cc.reshape(KC, 128, 2).transpose(1, 0, 2))
        maps.append(m)
    return maps


_CACHE = {}


def kernel(**inputs):
    if "nc" not in _CACHE:
        _CACHE["nc"] = build()[0]
    nc = _CACHE["nc"]
    maps = make_in_maps(inputs)
    res = run_bass_kernel_spmd(nc, maps, core_ids=list(range(8)))
    return np.stack([np.asarray(r["out"]) for r in res.results], axis=0).astype(np.float32)
```

```python
import contextlib
import math
import numpy as np
import concourse.bass as bass
import concourse.mybir as mybir
from concourse.bass_utils import run_bass_kernel_spmd

F32 = mybir.dt.float32
BF16 = mybir.dt.bfloat16
AF = mybir.ActivationFunctionType
ALU = mybir.AluOpType
AX = mybir.AxisListType

SEM_EPOCH = 24000
class Buf:
    __slots__ = ("name", "ws", "r", "dsem", "dcnt", "multi")

    def __init__(self, name, multi=False):
        self.name = name
        self.ws = {}
        self.r = {}
        self.dsem = None
        self.dcnt = 0
        self.multi = multi


class T:
    def __init__(self, t, name, psum=False):
        self.t = t
        self.b = Buf(name)
        self.psum = psum

    def __getitem__(self, k):
        return self.t[k]


class FW:
    def __init__(self, nc):
        self.nc = nc
        self.es = contextlib.ExitStack()
        self.eng = {"pe": nc.tensor, "act": nc.scalar, "dve": nc.vector, "pool": nc.gpsimd, "sp": nc.sync}
        self.sem, self.cnt, self.seen = {}, {}, {}
        for e in self.eng:
            self.sem[e] = self.es.enter_context(nc.semaphore("c_" + e))
            self.cnt[e] = 0
            self.seen[e] = {}
        self.pe_sems = {self.sem["pe"].num}
        self.nsem = 5
        self.ninstr = 0
        self.free_dsems = []

    def uname(self, name):
        self.nname = getattr(self, "nname", 0) + 1
        return "t%d_%s" % (self.nname, name)

    def sb(self, name, shape, dt=F32):
        return T(self.es.enter_context(self.nc.sbuf_tensor(self.uname(name), list(shape), dt)), name)

    def ps(self, name, shape, dt=F32):
        return T(self.es.enter_context(self.nc.psum_tensor(self.uname(name), list(shape), dt)), name, psum=True)

    def scope(self):
        return Scope(self)

    @staticmethod
    def _bufs(lst):
        out = []
        for x in lst:
            if x is None:
                continue
            out.append(x.b if isinstance(x, T) else x)
        return out

    def _wait(self, e, evs):
        eng = self.eng[e]
        seen = self.seen[e]
        best = {}
        for (s, v) in evs:
            k = s.num
            if best.get(k, (None, 0))[1] < v:
                best[k] = (s, v)
        for k, (s, v) in best.items():
            if e == "pe" and k in self.pe_sems:
                continue
            if seen.get(k, 0) < v:
                eng.wait_ge(s, v)
                seen[k] = v

    @staticmethod
    def _deps(reads, writes):
        evs = []
        for b in reads:
            evs.extend(b.ws.values())
        for b in writes:
            if not b.multi:
                evs.extend(b.ws.values())
            evs.extend(b.r.values())
        return evs

    @staticmethod
    def _put(d, ev):
        k = ev[0].num
        if k not in d or d[k][1] < ev[1]:
            d[k] = ev

    def _record(self, ev, reads, writes):
        for b in reads:
            self._put(b.r, ev)
        for b in writes:
            if b.multi:
                self._put(b.ws, ev)
            else:
                b.ws = {ev[0].num: ev}
                b.r = {}

    def op(self, e, fn, reads=(), writes=()):
        pr = [x for x in reads if isinstance(x, T) and x.psum]
        if pr:
            reads = [x for x in reads if not (isinstance(x, T) and x.psum)]
            writes = list(writes) + pr
        reads = self._bufs(reads)
        writes = self._bufs(writes)
        self._wait(e, self._deps(reads, writes))
        ins = fn(self.eng[e])
        if self.cnt[e] >= SEM_EPOCH:
            self.sem[e] = self.es.enter_context(self.nc.semaphore("c%d_%s" % (self.nsem, e)))
            self.nsem += 1
            self.cnt[e] = 0
            if e == "pe":
                self.pe_sems.add(self.sem[e].num)
        self.cnt[e] += 1
        self.ninstr += 1
        ins.then_inc(self.sem[e], 1)
        ev = (self.sem[e], self.cnt[e])
        self._record(ev, reads, writes)
        return ev

    def dma(self, e, out, in_, reads=(), writes=(), key=None, **kw):
        reads = self._bufs(reads)
        writes = self._bufs(writes)
        kb = key.b if isinstance(key, T) else key
        if kb.dsem is None:
            if self.free_dsems:
                kb.dsem, kb.dcnt = self.free_dsems.pop()
            else:
                kb.dsem = self.es.enter_context(self.nc.semaphore("d%d" % self.nsem))
                self.nsem += 1
        evs = self._deps(reads, writes)
        if kb.dcnt > 0:
            evs.append((kb.dsem, kb.dcnt))
        self._wait(e, evs)
        if kb.dcnt >= SEM_EPOCH:
            kb.dsem = self.es.enter_context(self.nc.semaphore("d%d" % self.nsem))
            self.nsem += 1
            kb.dcnt = 0
        ins = self.eng[e].dma_start(out=out, in_=in_, **kw)
        kb.dcnt += 16
        self.ninstr += 1
        ins.then_inc(kb.dsem, 16)
        ev = (kb.dsem, kb.dcnt)
        self._record(ev, reads, writes)
        return ev

    def fence(self, tiles, engines=("pe", "act", "dve", "pool", "sp")):
        evs = []
        for b in self._bufs(tiles):
            evs.extend(b.ws.values())
            evs.extend(b.r.values())
        for e in engines:
            self._wait(e, evs)

    def release(self, tiles):
        for t in tiles:
            b = t.b if isinstance(t, T) else t
            if b.dsem is not None:
                if b.dcnt < SEM_EPOCH // 2:
                    self.free_dsems.append((b.dsem, b.dcnt))
                b.dsem = None

    def mm(self, out_t, out_ap, lhsT_t, lhsT_ap, rhs_t, rhs_ap, start=True, stop=True):
        return self.op("pe", lambda g: g.matmul(out_ap, lhsT=lhsT_ap, rhs=rhs_ap, start=start, stop=stop),
                       reads=[lhsT_t, rhs_t], writes=[out_t])

    def tr(self, out_t, out_ap, in_t, in_ap, ident):
        n = in_ap.shape[0]
        return self.op("pe", lambda g: g.transpose(out_ap, in_ap, ident.t[0:n, 0:n]),
                       reads=[in_t, ident], writes=[out_t])


class Scope:
    def __init__(self, fw):
        self.fw = fw
        self.es = contextlib.ExitStack()
        self.tiles = []

    def __enter__(self):
        return self

    def sb(self, name, shape, dt=F32):
        t = T(self.es.enter_context(self.fw.nc.sbuf_tensor(self.fw.uname(name), list(shape), dt)), name)
        self.tiles.append(t)
        return t

    def ps(self, name, shape, dt=F32):
        t = T(self.es.enter_context(self.fw.nc.psum_tensor(self.fw.uname(name), list(shape), dt)), name, psum=True)
        self.tiles.append(t)
        return t

    def __exit__(self, *a):
        self.fw.fence(self.tiles)
        self.fw.release(self.tiles)
        self.es.close()
        return False


def run_rr(gens):
    gens = list(gens)
    while gens:
        for g_ in list(gens):
            try:
                next(g_)
            except StopIteration:
                gens.remove(g_)


class RR:
    def __init__(self, items):
        self.items = list(items)
        self.i = 0

    def next(self):
        x = self.items[self.i % len(self.items)]
        self.i += 1
        return x

D = 2048
SEQ = 4096
CTXL = 256
NTOK = SEQ + CTXL
NTILE = NTOK // 128
KC = D // 128
DEPTH = 2
NH = 6
HD = 128
DG = 768
DHY = 512
EPS = 1e-6
FC = 3840
TC = 5168
T_GZ, T_GAB, T_HP, T_HZ, T_MV, T_MO, T_MZ, T_MG = 0, 768, 792, 2328, 2840, 3608, 4376, 5144
FCOLS = np.concatenate([np.arange(0, 2304), np.arange(5144, 6680)])
TCOLS = np.concatenate([np.arange(2304, 5144), np.arange(6680, 9008)])
Y_G, Y_H, Y_M = 0, 768, 1280


def build(dbg=None, layers=(0, 1), phases=("mod", "proj", "gdn", "hy", "ml", "out"), dbg_in=None, opts=None):
    dbg = dbg or {}
    dbg_in = dbg_in or {}
    opts = opts or {}
    nc = bass.Bass("TRN2", target_bir_lowering=False)
    fw = FW(nc)

    def din(name, shape, dt=F32):
        return nc.dram_tensor(name, list(shape), dt, kind="ExternalInput").ap()

    def dscr(name, shape, dt=F32):
        return nc.dram_tensor(name, list(shape), dt, kind="Internal").ap()

    big = ("proj" in phases) or ("out" in phases)
    x_in = din("x", [SEQ, D]) if big else None
    ctx_in = din("ctx", [CTXL, D]) if big else None
    cT_in = din("cT", [128, KC, 2])
    nwT_in = din("nwT", [128, DEPTH, KC])
    fnw_in = din("fnw", [1, D])
    modw_in = din("modw", [DEPTH, 128, KC, 3 * D]) if "mod" in phases else None
    modbT_in = din("modbT", [128, DEPTH, 32])
    modbg_in = din("modbg", [DEPTH, D])
    winF_in = din("winF", [DEPTH, 128, KC, FC]) if "proj" in phases else None
    winT_in = din("winT", [DEPTH, 128, KC, TC]) if "proj" in phases else None
    wout_in = din("wout", [DEPTH, 128, KC, D]) if "out" in phases else None
    ident_in = din("ident", [128, 128])
    cmask_in = din("cmask", [128, 6, 128])
    gpar_in = din("gpar", [DEPTH, 24])
    gconv_in = din("gconv", [DEPTH, 128, 18, 5])
    gnorm_in = din("gnorm", [DEPTH, 128])
    mnorm_in = din("mnorm", [DEPTH, 128])
    mgb_in = din("mgb", [DEPTH, 24])
    featL_in = din("featL", [33, SEQ])
    featC_in = din("featC", [33, CTXL])
    hyw1_in = din("hyw1", [DEPTH, 33, 64])
    hyw2_in = din("hyw2", [DEPTH, 64, 64])
    hyw3_in = din("hyw3", [DEPTH, 64, 2048])
    hyp_in = din("hyp", [DEPTH, 64, 3])
    hyskip_in = din("hyskip", [DEPTH, 1024])
    hyconv_in = din("hyconv", [DEPTH, 3, 1536])
    hyconvT_in = din("hyconvT", [DEPTH, 128, 12, 3])
    hyskipT_in = din("hyskipT", [DEPTH, 128, 2, 4])
    negt_in = din("negt", [64, 64])
    negtc_in = din("negtc", [1, CTXL])
    drow_in = din("drow", [1, 512])
    dcol_in = din("dcol", [128, 4])
    F1_in = din("F1", [64, 2, 128], BF16)
    F4_in = din("F4", [128, 2, 64], BF16)
    W2_in = din("W2", [64, 128, 4, 128], BF16)
    W3_in = din("W3", [128, 128, 128], BF16)
    identb_in = din("identb", [128, 128], BF16)
    identb = fw.sb("identb", [128, 128], BF16)
    fw.dma("sp", identb[:], identb_in[:, :], writes=[identb], key=identb)
    out_d = nc.dram_tensor("out", [SEQ, D], F32, kind="ExternalOutput").ap()

    xcur = dscr("xcur", [SEQ, D])
    ctxcur = dscr("ctxcur", [CTXL, D])
    UF = dscr("UF", [FC, NTOK])
    UT = dscr("UT", [NTOK, TC])
    Y = dscr("Y", [NTOK, D])
    b_xcur, b_ctxcur, b_UF, b_UT, b_Y, b_out = (Buf(n, multi=True) for n in ("xcur", "ctxcur", "UF", "UT", "Y", "out"))

    dbg_out = {}

    def dbg_tensor(name, shape):
        t = nc.dram_tensor("dbg_" + name, list(shape), F32, kind="ExternalOutput").ap()
        dbg_out[name] = t
        return t

    ident = fw.sb("ident", [128, 128])
    fw.dma("sp", ident[:], ident_in[:, :], writes=[ident], key=ident)
    cT = fw.sb("cT", [128, KC, 2])
    fw.dma("sp", cT[:], cT_in[:, :, :], writes=[cT], key=cT)
    scT = fw.sb("scT", [128, KC, 2])
    fw.op("act", lambda g: g.activation(out=scT[:], in_=cT[:], func=AF.Silu), reads=[cT], writes=[scT])
    nwT = fw.sb("nwT", [128, DEPTH, KC])
    fw.dma("sp", nwT[:], nwT_in[:, :, :], writes=[nwT], key=nwT)
    modbT = fw.sb("modbT", [128, DEPTH, 32])
    fw.dma("sp", modbT[:], modbT_in[:, :, :], writes=[modbT], key=modbT)
    modT = fw.sb("modT", [128, 32, 2])
    Amod = fw.sb("Amod", [128, 2, KC])
    Bmod = fw.sb("Bmod", [128, 2, KC])
    gtbc = [fw.sb("gtbc%d" % i, [128, D]) for i in range(2)]

    def phase_mod(l):
        with fw.scope() as sc:
            wb = [sc.sb("modw%d" % i, [128, KC, 512]) for i in range(2)]
            pss = [sc.ps("modps%d" % i, [128, 512]) for i in range(2)]
            gb = sc.sb("gbias", [128, D])
            rep = [sc.sb("rep%d" % i, [128, KC, 128]) for i in range(2)]
            for i in range(2):
                fw.op("dve", lambda g, i=i: g.tensor_copy(out=rep[i][:], in_=scT[:, :, i:i + 1].to_broadcast([128, KC, 128])),
                      reads=[scT], writes=[rep[i]])
            fw.dma("sp", gb[:], modbg_in[l:l + 1, :].partition_broadcast(128)[:, 0, :], writes=[gb], key=gb)
            for blk in range(12):
                w = wb[blk % 2]
                fw.dma("sp" if blk % 2 == 0 else "act", w[:], modw_in[l, :, :, blk * 512:(blk + 1) * 512],
                       writes=[w], key=w)
                if blk < 8:
                    p = pss[blk % 2]
                    for sub in range(4):
                        for kc in range(KC):
                            fw.mm(p, p[:, sub * 2:sub * 2 + 2], w, w[:, kc, sub * 128:(sub + 1) * 128],
                                  scT, scT[:, kc, :], start=(kc == 0), stop=(kc == KC - 1))
                    fw.op("dve", lambda g, p=p, blk=blk: g.tensor_tensor(
                        out=modT[:, blk * 4:(blk + 1) * 4, :],
                        in0=p[:, 0:8].rearrange("p (a b) -> p a b", b=2),
                        in1=modbT[:, l, blk * 4:(blk + 1) * 4].unsqueeze(2).to_broadcast([128, 4, 2]),
                        op=ALU.add), reads=[p, modbT], writes=[modT])
                else:
                    for i in range(2):
                        p = pss[i]
                        for kc in range(KC):
                            fw.mm(p, p[:, :], rep[i], rep[i][:, kc, :], w, w[:, kc, :],
                                  start=(kc == 0), stop=(kc == KC - 1))
                        cs = slice((blk - 8) * 512, (blk - 7) * 512)
                        fw.op("dve", lambda g, p=p, i=i, cs=cs: g.tensor_tensor(
                            out=gtbc[i][:, cs], in0=p[:, :], in1=gb[:, cs], op=ALU.add),
                            reads=[p, gb], writes=[gtbc[i]])
            for i in range(2):
                fw.op("dve", lambda g, i=i: g.scalar_tensor_tensor(
                    out=Amod[:, i, :], in0=modT[:, 16:32, i], scalar=1.0, in1=nwT[:, l, :],
                    op0=ALU.add, op1=ALU.mult), reads=[modT, nwT], writes=[Amod])
                fw.op("dve", lambda g, i=i: g.tensor_copy(out=Bmod[:, i, :], in_=modT[:, 0:16, i]),
                      reads=[modT], writes=[Bmod])

    def load_norm_transpose(sc, src_ap, src_buf, xt, ss, junk, tps, dstT, dst_cols, A_ap_fn, B_ap_fn, evq):
        fw.dma("sp", xt[:], src_ap, reads=[src_buf], writes=[xt], key=xt)
        if ss is not None:
            fw.op("act", lambda g: g.activation(out=junk[:], in_=xt[:], func=AF.Square, accum_out=ss[:, 0:1]),
                  reads=[xt], writes=[junk, ss])
            fw.op("dve", lambda g: g.tensor_scalar(out=ss[:, 1:2], in0=ss[:, 0:1], scalar1=1.0 / D, scalar2=EPS,
                                                    op0=ALU.mult, op1=ALU.add), reads=[ss], writes=[ss])
            fw.op("act", lambda g: g.sqrt(out=ss[:, 3:4], in_=ss[:, 1:2]), reads=[ss], writes=[ss])
            fw.op("dve", lambda g: g.reciprocal(out=ss[:, 2:3], in_=ss[:, 3:4]), reads=[ss], writes=[ss])
            fw.op("dve", lambda g: g.tensor_scalar(out=xt[:], in0=xt[:], scalar1=ss[:, 2:3], scalar2=None,
                                                    op0=ALU.mult), reads=[xt, ss], writes=[xt])
        for q4 in range(KC // 4):
            p = tps.next()
            for j in range(4):
                kc = q4 * 4 + j
                fw.tr(p, p[:, j * 128:(j + 1) * 128], xt, xt[:, kc * 128:(kc + 1) * 128], ident)
            for j in range(4):
                kc = q4 * 4 + j
                e = evq.next()
                if A_ap_fn is None:
                    if e == "act":
                        fw.op("act", lambda g, p=p, j=j, kc=kc: g.copy(out=dstT[:, kc, dst_cols], in_=p[:, j * 128:(j + 1) * 128]),
                              reads=[p], writes=[dstT])
                    else:
                        fw.op("dve", lambda g, p=p, j=j, kc=kc: g.tensor_copy(out=dstT[:, kc, dst_cols], in_=p[:, j * 128:(j + 1) * 128]),
                              reads=[p], writes=[dstT])
                else:
                    if e == "act":
                        fw.op("act", lambda g, p=p, j=j, kc=kc: g.activation(
                            out=dstT[:, kc, dst_cols], in_=p[:, j * 128:(j + 1) * 128], func=AF.Identity,
                            scale=A_ap_fn(kc), bias=B_ap_fn(kc)), reads=[p, Amod, Bmod], writes=[dstT])
                    else:
                        fw.op("dve", lambda g, p=p, j=j, kc=kc: g.tensor_scalar(
                            out=dstT[:, kc, dst_cols], in0=p[:, j * 128:(j + 1) * 128],
                            scalar1=A_ap_fn(kc), scalar2=B_ap_fn(kc), op0=ALU.mult, op1=ALU.add),
                            reads=[p, Amod, Bmod], writes=[dstT])

    def phase_proj(l):
        GT = 17
        xsrc, xbuf = (x_in, None) if l == 0 else (xcur, b_xcur)
        csrc, cbuf = (ctx_in, None) if l == 0 else (ctxcur, b_ctxcur)
        with fw.scope() as sc:
            xnT = sc.sb("xnT", [128, KC, GT * 128], BF16)
            xts = RR([sc.sb("xt%d" % i, [128, D]) for i in range(2)])
            junk = sc.sb("junk", [128, D])
            sss = RR([sc.sb("ss%d" % i, [128, 4]) for i in range(2)])
            tps = RR([sc.ps("tps%d" % i, [128, 512]) for i in range(2)])
            mps = RR([sc.ps("mps%d" % i, [128, 512]) for i in range(4)])
            wbs = RR([sc.sb("wb%d" % i, [128, KC, 512], BF16) for i in range(2)])
            sts = RR([sc.sb("st%d" % i, [128, 512]) for i in range(4)])
            evq = RR(["act", "dve"])
            for grp in range(2):
                for ti in range(GT):
                    gt_ = grp * GT + ti
                    if gt_ < 2:
                        src, sbuf, mi = csrc[gt_ * 128:(gt_ + 1) * 128, :], cbuf, 1
                    else:
                        src, sbuf, mi = xsrc[(gt_ - 2) * 128:(gt_ - 1) * 128, :], xbuf, 0
                    load_norm_transpose(sc, src, sbuf, xts.next(), sss.next(), junk, tps, xnT,
                                        slice(ti * 128, (ti + 1) * 128),
                                        lambda kc, mi=mi: Amod[:, mi, kc:kc + 1],
                                        lambda kc, mi=mi: Bmod[:, mi, kc:kc + 1], evq)
                tok0 = grp * GT * 128
                for c0 in range(0, TC, 512):
                    ncol = min(512, TC - c0)
                    w = wbs.next()
                    fw.dma("pool", w[:, :, 0:ncol], winT_in[l, :, :, c0:c0 + ncol], writes=[w], key=w)
                    for ti in range(GT):
                        p = mps.next()
                        for kc in range(KC):
                            fw.mm(p, p[:, 0:ncol], xnT, xnT[:, kc, ti * 128:(ti + 1) * 128], w, w[:, kc, 0:ncol],
                                  start=(kc == 0), stop=(kc == KC - 1))
                        st = sts.next()
                        e = evq.next()
                        if e == "act":
                            fw.op("act", lambda g, p=p, st=st: g.copy(out=st[:, 0:ncol], in_=p[:, 0:ncol]), reads=[p], writes=[st])
                        else:
                            fw.op("dve", lambda g, p=p, st=st: g.tensor_copy(out=st[:, 0:ncol], in_=p[:, 0:ncol]), reads=[p], writes=[st])
                        r0 = tok0 + ti * 128
                        fw.dma(e if e == 'act' else 'pool', UT[r0:r0 + 128, c0:c0 + ncol], st[:, 0:ncol], reads=[st], writes=[b_UT], key=st)
                ntk = GT * 128
                for c0 in range(0, FC, 512):
                    w = wbs.next()
                    ncf = min(512, FC - c0)
                    fw.dma("pool", w[:, :, 0:ncf], winF_in[l, :, :, c0:c0 + ncf], writes=[w], key=w)
                    for ct in range(4):
                        if c0 + ct * 128 >= FC:
                            break
                        for t0 in range(0, ntk, 512):
                            nt = min(512, ntk - t0)
                            p = mps.next()
                            for kc in range(KC):
                                fw.mm(p, p[:, 0:nt], w, w[:, kc, ct * 128:(ct + 1) * 128], xnT, xnT[:, kc, t0:t0 + nt],
                                      start=(kc == 0), stop=(kc == KC - 1))
                            st = sts.next()
                            e = evq.next()
                            if e == "act":
                                fw.op("act", lambda g, p=p, st=st, nt=nt: g.copy(out=st[:, 0:nt], in_=p[:, 0:nt]), reads=[p], writes=[st])
                            else:
                                fw.op("dve", lambda g, p=p, st=st, nt=nt: g.tensor_copy(out=st[:, 0:nt], in_=p[:, 0:nt]), reads=[p], writes=[st])
                            r0 = c0 + ct * 128
                            fw.dma(e if e == 'act' else 'pool', UF[r0:r0 + 128, tok0 + t0:tok0 + t0 + nt], st[:, 0:nt], reads=[st], writes=[b_UF], key=st)

    def phase_out(l):
        last = (l == DEPTH - 1)
        xsrc, xbuf = (x_in, None) if l == 0 else (xcur, b_xcur)
        with fw.scope() as sc:
            wo = sc.sb("wo", [128, KC, D], BF16)
            for h in range(4):
                fw.dma("pool", wo[:, h * 4:(h + 1) * 4, :], wout_in[l, :, h * 4:(h + 1) * 4, :], writes=[wo], key=wo)
            yts = RR([sc.sb("yt%d" % i, [128, D]) for i in range(2)])
            xts = RR([sc.sb("xr%d" % i, [128, D]) for i in range(2)])
            yT = RR([sc.sb("yT%d" % i, [128, KC, 128], BF16) for i in range(2)])
            tps = RR([sc.ps("tps%d" % i, [128, 512]) for i in range(2)])
            mps = RR([sc.ps("mps%d" % i, [128, 512]) for i in range(4)])
            evq = RR(["act", "dve"])
            ss = sc.sb("ss", [128, 4])
            junk = sc.sb("junk", [128, D])
            fnw = None
            if last:
                fnw = sc.sb("fnw", [128, D])
                fw.dma("sp", fnw[:], fnw_in[0:1, :].partition_broadcast(128)[:, 0, :], writes=[fnw], key=fnw)
            tiles = range(2, NTILE) if last else range(NTILE)
            for gt_ in tiles:
                isctx = gt_ < 2
                yt = yts.next()
                yTt = yT.next()
                load_norm_transpose(sc, Y[gt_ * 128:(gt_ + 1) * 128, :], b_Y, yt, None, None, tps, yTt,
                                    slice(0, 128), None, None, evq)
                xr = xts.next()
                if isctx:
                    src = (ctx_in if l == 0 else ctxcur)[gt_ * 128:(gt_ + 1) * 128, :]
                    sb_ = None if l == 0 else b_ctxcur
                else:
                    src = xsrc[(gt_ - 2) * 128:(gt_ - 1) * 128, :]
                    sb_ = xbuf
                fw.dma("act", xr[:], src, reads=[sb_], writes=[xr], key=xr)
                g_ = gtbc[1 if isctx else 0]
                for fb in range(4):
                    p = mps.next()
                    fs = slice(fb * 512, (fb + 1) * 512)
                    for kc in range(KC):
                        fw.mm(p, p[:, :], yTt, yTt[:, kc, :], wo, wo[:, kc, fs], start=(kc == 0), stop=(kc == KC - 1))
                    fw.op("dve", lambda g, p=p, fs=fs, g_=g_, yt=yt: g.tensor_tensor(out=yt[:, fs], in0=p[:, :], in1=g_[:, fs], op=ALU.mult),
                          reads=[p, g_], writes=[yt])
                    fw.op("pool", lambda g, fs=fs, yt=yt, xr=xr: g.tensor_tensor(out=xr[:, fs], in0=xr[:, fs], in1=yt[:, fs], op=ALU.add),
                          reads=[yt, xr], writes=[xr])
                if not last:
                    if isctx:
                        fw.dma("pool", ctxcur[gt_ * 128:(gt_ + 1) * 128, :], xr[:], reads=[xr], writes=[b_ctxcur], key=xr)
                    else:
                        fw.dma("pool", xcur[(gt_ - 2) * 128:(gt_ - 1) * 128, :], xr[:], reads=[xr], writes=[b_xcur], key=xr)
                else:
                    fw.op("act", lambda g, xr=xr: g.activation(out=junk[:], in_=xr[:], func=AF.Square, accum_out=ss[:, 0:1]),
                          reads=[xr], writes=[junk, ss])
                    fw.op("dve", lambda g: g.tensor_scalar(out=ss[:, 1:2], in0=ss[:, 0:1], scalar1=1.0 / D, scalar2=EPS,
                                                            op0=ALU.mult, op1=ALU.add), reads=[ss], writes=[ss])
                    fw.op("act", lambda g: g.sqrt(out=ss[:, 3:4], in_=ss[:, 1:2]), reads=[ss], writes=[ss])
                    fw.op("dve", lambda g: g.reciprocal(out=ss[:, 2:3], in_=ss[:, 3:4]), reads=[ss], writes=[ss])
                    fw.op("dve", lambda g, xr=xr: g.scalar_tensor_tensor(out=xr[:], in0=xr[:], scalar=ss[:, 2:3], in1=fnw[:],
                                                                         op0=ALU.mult, op1=ALU.mult), reads=[xr, ss, fnw], writes=[xr])
                    fw.dma("pool", out_d[(gt_ - 2) * 128:(gt_ - 1) * 128, :], xr[:], reads=[xr], writes=[b_out], key=xr)


    cmask = fw.sb("cmask", [128, 6, 128])
    fw.dma("sp", cmask[:], cmask_in[:, :, :], writes=[cmask], key=cmask)
    TRI = [cmask[:, 0, :], cmask[:, 1, :]]
    LS = [cmask[:, 2, :], cmask[:, 3, :]]
    LIT = [cmask[:, 4, :], cmask[:, 5, :]]
    trione = [fw.sb("trione%d" % d, [128, 129]) for d in range(2)]
    for d in range(2):
        fw.op("dve", lambda g, d=d: g.tensor_copy(out=trione[d][:, 0:128], in_=TRI[d]), reads=[cmask], writes=[trione[d]])
        fw.op("dve", lambda g, d=d: g.memset(trione[d][:, 128:129], 1.0), writes=[trione[d]])
    ones = fw.sb("ones", [128, 128])
    fw.op("dve", lambda g: g.memset(ones[:], 1.0), writes=[ones])
    epsc = fw.sb("epsc", [128, 1])
    fw.op("dve", lambda g: g.memset(epsc[:], EPS), writes=[epsc])
    NCH = NTILE

    def chunk_order(d):
        return list(range(NCH)) if d == 0 else [1, 0] + list(range(NCH - 1, 1, -1))

    OFs = [dscr("OF", [NTOK, DG]), dscr("OB", [NTOK, DG])]
    b_OF = [Buf("OF", multi=True), Buf("OB", multi=True)]

    def decay_prep(ws, gcol_ap, gsrc, d, need_D):
        fw.op("dve", lambda g: g.tensor_scalar(out=ws["gtri"][:], in0=TRI[d], scalar1=gcol_ap, scalar2=None, op0=ALU.mult),
              reads=[cmask, gsrc], writes=[ws["gtri"]])
        fw.op("pool", lambda g: g.tensor_copy(out=ws["grep"][:], in_=gcol_ap.to_broadcast([128, 128])),
              reads=[gsrc], writes=[ws["grep"]])
        pDT = ws["ps"].next()
        fw.mm(pDT, pDT[:, 0:128], cmask, LS[d], ws["gtri"], ws["gtri"][:])
        fw.op("act", lambda g: g.activation(out=ws["eDT"][:], in_=pDT[:, 0:128], func=AF.Exp), reads=[pDT], writes=[ws["eDT"]])
        if need_D:
            pD = ws["ps"].next()
            fw.mm(pD, pD[:, 0:128], ws["gtri"], ws["gtri"][:], cmask, LS[d])
            fw.op("act", lambda g: g.activation(out=ws["eD"][:], in_=pD[:, 0:128], func=AF.Exp), reads=[pD], writes=[ws["eD"]])
        pb = ws["ps"].next()
        fw.mm(pb, pb[:, 0:129], ws["grep"], ws["grep"][:], trione[d], trione[d][:])
        fw.op("act", lambda g: g.activation(out=ws["EG"][:], in_=pb[:, 0:128], func=AF.Exp), reads=[pb], writes=[ws["EG"]])
        fw.op("act", lambda g: g.activation(out=ws["sm"][:, 0:1], in_=pb[:, 128:129], func=AF.Exp), reads=[pb], writes=[ws["sm"]])
        fw.op("dve", lambda g: g.tensor_copy(out=ws["sm"][:, 1:2], in_=pb[:, 128:129]), reads=[pb], writes=[ws["sm"]])
        pc = ws["ps"].next()
        fw.mm(pc, pc[:, 0:1], cmask, TRI[d], gsrc, gcol_ap)
        fw.op("act", lambda g: g.activation(out=ws["sm"][:, 2:3], in_=pc[:, 0:1], func=AF.Exp), reads=[pc], writes=[ws["sm"]])
        fw.op("act", lambda g: g.activation(out=ws["sm"][:, 3:4], in_=pc[:, 0:1], func=AF.Exp, scale=-1.0, bias=ws["sm"][:, 1:2]),
              reads=[pc, ws["sm"]], writes=[ws["sm"]])

    def make_ws(sc, tag, names):
        ws = {}
        for n in names:
            ws[n] = sc.sb(tag + n, [128, 128])
        ws["sm"] = sc.sb(tag + "sm", [128, 8])
        return ws

    def phase_gdn(l, heads=range(NH)):
        last = (l == DEPTH - 1)
        with fw.scope() as sc:
            AB = sc.sb("AB", [128, NCH, 24])
            fw.dma("sp", AB[:], UT[:, T_GAB:T_GAB + 24].rearrange("(n p) c -> p n c", p=128), reads=[b_UT], writes=[AB], key=AB)
            gpar = sc.sb("gpar", [128, 24])
            fw.dma("sp", gpar[:], gpar_in[l:l + 1, :].partition_broadcast(128)[:, 0, :], writes=[gpar], key=gpar)
            G = sc.sb("G", [128, NCH, 12])
            NB = sc.sb("NB", [128, NCH, 12])
            BETA = sc.sb("BETA", [128, NCH, 12])
            fw.op("dve", lambda g: g.tensor_tensor(out=G[:], in0=AB[:, :, 0:12], in1=gpar[:, 12:24].unsqueeze(1).to_broadcast([128, NCH, 12]), op=ALU.add),
                  reads=[AB, gpar], writes=[G])
            fw.op("act", lambda g: g.activation(out=G[:], in_=G[:], func=AF.Exp), reads=[G], writes=[G])
            fw.op("act", lambda g: g.activation(out=G[:], in_=G[:], func=AF.Ln, bias=ones[:, 0:1]), reads=[G, ones], writes=[G])
            fw.op("act", lambda g: g.activation(out=gpar[:, 0:12], in_=gpar[:, 0:12], func=AF.Exp), reads=[gpar], writes=[gpar])
            fw.op("dve", lambda g: g.scalar_tensor_tensor(out=G[:], in0=G[:], scalar=-1.0, in1=gpar[:, 0:12].unsqueeze(1).to_broadcast([128, NCH, 12]),
                                                         op0=ALU.mult, op1=ALU.mult), reads=[G, gpar], writes=[G])
            fw.op("act", lambda g: g.activation(out=BETA[:], in_=AB[:, :, 12:24], func=AF.Sigmoid), reads=[AB], writes=[BETA])
            fw.op("dve", lambda g: g.tensor_scalar(out=NB[:], in0=BETA[:], scalar1=-1.0, scalar2=None, op0=ALU.mult), reads=[BETA], writes=[NB])
            cw = sc.sb("cw", [128, 18, 5])
            fw.dma("sp", cw[:], gconv_in[l, :, :, :], writes=[cw], key=cw)

            qkv_raw = [sc.sb("raw%d" % i, [128, NTOK]) for i in range(3)]
            qkv = [sc.sb("qkv%d" % i, [128, NTOK]) for i in range(3)]
            psl = [sc.ps("gps%d" % i, [128, 512]) for i in range(8)]
            PS = RR(psl)
            S = [sc.sb("S%d" % d, [128, 128]) for d in range(2)]
            names = ["gtri", "grep", "eDT", "eD", "EG", "Ktok", "Vtok", "kkm", "qkTm", "P", "PT", "P2", "P2T", "TT",
                     "AqkT", "QsT", "Ke", "bV", "R", "U", "O"]
            WS = []
            for i in range(4):
                w_ = make_ws(sc, "w%d" % i, names)
                w_["ps"] = PS
                WS.append(w_)
            cengs = RR(["dve"])
            for h in heads:
                for i in range(3):
                    r0 = i * DG + h * 128
                    fw.dma("sp" if i != 1 else "act", qkv_raw[i][:], UF[r0:r0 + 128, :], reads=[b_UF], writes=[qkv_raw[i]], key=qkv_raw[i])
                for i in range(3):
                    e = cengs.next()
                    src, dst = qkv_raw[i], qkv[i]
                    wcol = lambda j, i=i: cw[:, i * 6 + h, j:j + 1]
                    fw.op(e, lambda g, src=src, dst=dst: g.tensor_scalar(out=dst[:], in0=src[:], scalar1=wcol(2), scalar2=None, op0=ALU.mult),
                          reads=[src, cw], writes=[dst])
                    for j in (0, 1, 3, 4):
                        off = j - 2
                        a, b = max(0, -off), CTXL - max(0, off)
                        fw.op(e, lambda g, src=src, dst=dst, a=a, b=b, off=off, j=j: g.scalar_tensor_tensor(
                            out=dst[:, a:b], in0=src[:, a + off:b + off], scalar=wcol(j), in1=dst[:, a:b], op0=ALU.mult, op1=ALU.add),
                            reads=[src, cw, dst], writes=[dst])
                        a, b = max(0, -off), 64 - max(0, off)
                        sv = src[:, CTXL:].rearrange("p (r c) -> p r c", c=64)
                        dv = dst[:, CTXL:].rearrange("p (r c) -> p r c", c=64)
                        fw.op(e, lambda g, sv=sv, dv=dv, a=a, b=b, off=off, j=j, src=src, dst=dst: g.scalar_tensor_tensor(
                            out=dv[:, :, a:b], in0=sv[:, :, a + off:b + off], scalar=wcol(j), in1=dv[:, :, a:b], op0=ALU.mult, op1=ALU.add),
                            reads=[src, cw, dst], writes=[dst])
                    fw.op("act", lambda g, dst=dst: g.activation(out=dst[:], in_=dst[:], func=AF.Silu), reads=[dst], writes=[dst])
                for i in range(2):
                    x_, sq = qkv[i], qkv_raw[i]
                    fw.op("act", lambda g, x_=x_, sq=sq: g.activation(out=sq[:], in_=x_[:], func=AF.Square), reads=[x_], writes=[sq])
                    for t0 in range(0, NTOK, 512):
                        nt = min(512, NTOK - t0)
                        p = PS.next()
                        fw.mm(p, p[:, 0:nt], ones, ones[:], sq, sq[:, t0:t0 + nt])
                        fw.op("act", lambda g, p=p, sq=sq, t0=t0, nt=nt: g.activation(out=sq[:, t0:t0 + nt], in_=p[:, 0:nt], func=AF.Sqrt, bias=epsc[:, 0:1]),
                              reads=[p, epsc, sq], writes=[sq])
                    fw.op("dve", lambda g, sq=sq: g.reciprocal(out=sq[:], in_=sq[:]), reads=[sq], writes=[sq])
                    scl = HD ** -0.5 if i == 0 else 1.0
                    fw.op("dve", lambda g, x_=x_, sq=sq, scl=scl: g.scalar_tensor_tensor(out=x_[:], in0=x_[:], scalar=scl, in1=sq[:], op0=ALU.mult, op1=ALU.mult),
                          reads=[x_, sq], writes=[x_])
                qT, kT, vT = qkv
                for d in range(2):
                    fw.op("dve", lambda g, d=d: g.memset(S[d][:], 0.0), writes=[S[d]])
                orders = [chunk_order(0), chunk_order(1)]

                def unit_vars(step, d):
                    n = orders[d][step]
                    return n, WS[(step % 2) * 2 + d], d * 6 + h, slice(n * 128, (n + 1) * 128)

                def g_prep(step, d):
                    n, ws, u, cs = unit_vars(step, d)
                    gcol = G[:, n, u:u + 1]
                    u = d * 6 + h
                    cs = slice(n * 128, (n + 1) * 128)
                    gcol = G[:, n, u:u + 1]
                    decay_prep(ws, gcol, G, d, True)
                    yield
                    p = PS.next()
                    fw.tr(p, p[:, 0:128], kT, kT[:, cs], ident)
                    fw.tr(p, p[:, 128:256], vT, vT[:, cs], ident)
                    fw.op("act", lambda g, p=p, ws=ws: g.activation(out=ws["Ke"][:], in_=p[:, 0:128], func=AF.Copy, scale=ws["sm"][:, 3:4]),
                          reads=[p, ws["sm"]], writes=[ws["Ke"]])
                    fw.op("dve", lambda g, p=p, ws=ws, n=n, u=u: g.tensor_scalar(out=ws["bV"][:], in0=p[:, 128:256], scalar1=BETA[:, n, u:u + 1], scalar2=None, op0=ALU.mult),
                          reads=[p, BETA], writes=[ws["bV"]])
                    fw.op("dve", lambda g, ws=ws, n=n, u=u: g.tensor_tensor(out=ws["sm"][:, 4:5], in0=ws["sm"][:, 2:3], in1=NB[:, n, u:u + 1], op=ALU.mult),
                          reads=[ws["sm"], NB], writes=[ws["sm"]])
                    yield
                    p = PS.next()
                    fw.mm(p, p[:, 0:128], kT, kT[:, cs], kT, kT[:, cs])
                    fw.mm(p, p[:, 128:256], kT, kT[:, cs], qT, qT[:, cs])
                    fw.op("dve", lambda g, p=p, ws=ws, d=d: g.tensor_tensor(out=ws["kkm"][:], in0=p[:, 0:128], in1=LS[d], op=ALU.mult),
                          reads=[p, cmask], writes=[ws["kkm"]])
                    fw.op("dve", lambda g, p=p, ws=ws, d=d: g.tensor_tensor(out=ws["qkTm"][:], in0=p[:, 128:256], in1=LIT[d], op=ALU.mult),
                          reads=[p, cmask], writes=[ws["qkTm"]])
                    yield
                    fw.op("dve", lambda g, ws=ws, n=n, u=u: g.scalar_tensor_tensor(out=ws["P"][:], in0=ws["eD"][:], scalar=NB[:, n, u:u + 1], in1=ws["kkm"][:],
                                                                                 op0=ALU.mult, op1=ALU.mult), reads=[ws["eD"], NB, ws["kkm"]], writes=[ws["P"]])
                    fw.op("pool", lambda g, ws=ws: g.tensor_tensor(out=ws["AqkT"][:], in0=ws["eDT"][:], in1=ws["qkTm"][:], op=ALU.mult),
                          reads=[ws["eDT"], ws["qkTm"]], writes=[ws["AqkT"]])
                    fw.op("pool", lambda g, ws=ws, cs=cs: g.tensor_tensor(out=ws["QsT"][:], in0=qT[:, cs], in1=ws["EG"][:], op=ALU.mult),
                          reads=[qT, ws["EG"]], writes=[ws["QsT"]])
                    yield
                    p = PS.next()
                    fw.tr(p, p[:, 0:128], ws["P"], ws["P"][:], ident)
                    fw.op("act", lambda g, p=p, ws=ws: g.copy(out=ws["PT"][:], in_=p[:, 0:128]), reads=[p], writes=[ws["PT"]])
                    fw.op("dve", lambda g, p=p, ws=ws: g.tensor_tensor(out=ws["TT"][:], in0=p[:, 0:128], in1=ident[:], op=ALU.add),
                          reads=[p, ident], writes=[ws["TT"]])
                    Pc, PTc, Pn, PTn = "P", "PT", "P2", "P2T"
                    yield
                    for lev in range(1, 7):
                        p = PS.next()
                        fw.mm(p, p[:, 0:128], ws[PTc], ws[PTc][:], ws[Pc], ws[Pc][:])
                        if lev < 6:
                            fw.mm(p, p[:, 128:256], ws[Pc], ws[Pc][:], ws[PTc], ws[PTc][:])
                        fw.op("act", lambda g, p=p, ws=ws, Pn=Pn: g.copy(out=ws[Pn][:], in_=p[:, 0:128]), reads=[p], writes=[ws[Pn]])
                        if lev < 6:
                            fw.op("dve", lambda g, p=p, ws=ws, PTn=PTn: g.tensor_copy(out=ws[PTn][:], in_=p[:, 128:256]), reads=[p], writes=[ws[PTn]])
                        yield
                        p2 = PS.next()
                        fw.mm(p2, p2[:, 0:128], ws[Pn], ws[Pn][:], ws["TT"], ws["TT"][:])
                        fw.op("dve", lambda g, p2=p2, ws=ws: g.tensor_tensor(out=ws["TT"][:], in0=p2[:, 0:128], in1=ws["TT"][:], op=ALU.add),
                              reads=[p2, ws["TT"]], writes=[ws["TT"]])
                        Pc, PTc, Pn, PTn = Pn, PTn, Pc, PTc

                def g_seq(step, d):
                    n, ws, u, cs = unit_vars(step, d)
                    p = PS.next()
                    fw.mm(p, p[:, 0:128], kT, kT[:, cs], S[d], S[d][:])
                    fw.op("dve", lambda g, p=p, ws=ws: g.scalar_tensor_tensor(out=ws["R"][:], in0=p[:, 0:128], scalar=ws["sm"][:, 4:5], in1=ws["bV"][:],
                                                                           op0=ALU.mult, op1=ALU.add), reads=[p, ws["sm"], ws["bV"]], writes=[ws["R"]])
                    yield
                    fw.mm(p, p[:, 128:256], ws["TT"], ws["TT"][:], ws["R"], ws["R"][:])
                    fw.op("act", lambda g, p=p, ws=ws: g.copy(out=ws["U"][:], in_=p[:, 128:256]), reads=[p], writes=[ws["U"]])
                    yield
                    fw.mm(p, p[:, 256:384], ws["QsT"], ws["QsT"][:], S[d], S[d][:], start=True, stop=False)
                    fw.mm(p, p[:, 256:384], ws["AqkT"], ws["AqkT"][:], ws["U"], ws["U"][:], start=False, stop=True)
                    fw.mm(p, p[:, 384:512], ws["Ke"], ws["Ke"][:], ws["U"], ws["U"][:])
                    fw.op("dve", lambda g, p=p, ws=ws, d=d: g.scalar_tensor_tensor(out=S[d][:], in0=S[d][:], scalar=ws["sm"][:, 0:1], in1=p[:, 384:512],
                                                                                op0=ALU.mult, op1=ALU.add), reads=[p, ws["sm"], S[d]], writes=[S[d]])
                    if not (last and n < 2):
                        fw.op("act", lambda g, p=p, ws=ws: g.copy(out=ws["O"][:], in_=p[:, 256:384]), reads=[p], writes=[ws["O"]])
                        fw.dma("act", OFs[d][n * 128:(n + 1) * 128, h * 128:(h + 1) * 128], ws["O"][:], reads=[ws["O"]], writes=[b_OF[d]], key=ws["O"])


                    yield

                run_rr([g_prep(0, 0), g_prep(0, 1)])
                for step in range(NCH):
                    gens = [g_seq(step, 0), g_seq(step, 1)]
                    if step + 1 < NCH:
                        gens += [g_prep(step + 1, 0), g_prep(step + 1, 1)]
                    run_rr(gens)

    def finalize(l, kind):
        last = (l == DEPTH - 1)
        zcol = T_GZ if kind == "gdn" else T_MZ
        ycol = Y_G if kind == "gdn" else Y_M
        nw_in = gnorm_in if kind == "gdn" else mnorm_in
        with fw.scope() as sc:
            nwb = sc.sb("nwb", [128, 128])
            fw.dma("sp", nwb[:], nw_in[l:l + 1, :].partition_broadcast(128)[:, 0, :], writes=[nwb], key=nwb)
            A = RR([sc.sb("fa%d" % i, [128, DG]) for i in range(2)])
            B = RR([sc.sb("fb%d" % i, [128, DG]) for i in range(2)])
            Z = RR([sc.sb("fz%d" % i, [128, DG]) for i in range(2)])
            OGt = RR([sc.sb("fo%d" % i, [128, DG]) for i in range(2)])
            SQ = sc.sb("fsq", [128, DG])
            ssm = RR([sc.sb("fss%d" % i, [128, 12]) for i in range(2)])
            for n in (range(2, NCH) if last else range(NCH)):
                a, b_, z, ss = A.next(), B.next(), Z.next(), ssm.next()
                rs = slice(n * 128, (n + 1) * 128)
                fw.dma("sp", a[:], OFs[0][rs, :], reads=[b_OF[0]], writes=[a], key=a)
                fw.dma("sp", b_[:], OFs[1][rs, :], reads=[b_OF[1]], writes=[b_], key=b_)
                fw.dma("sp", z[:], UT[rs, zcol:zcol + DG], reads=[b_UT], writes=[z], key=z)
                fw.op("pool", lambda g, a=a, b_=b_: g.tensor_tensor(out=a[:], in0=a[:], in1=b_[:], op=ALU.add), reads=[a, b_], writes=[a])
                if kind == "ml":
                    og = OGt.next()
                    fw.dma("sp", og[:], UT[rs, T_MO:T_MO + DG], reads=[b_UT], writes=[og], key=og)
                    fw.op("act", lambda g, og=og: g.activation(out=og[:], in_=og[:], func=AF.Sigmoid), reads=[og], writes=[og])
                    fw.op("pool", lambda g, a=a, og=og: g.tensor_tensor(out=a[:], in0=a[:], in1=og[:], op=ALU.mult), reads=[a, og], writes=[a])
                fw.op("act", lambda g, a=a: g.activation(out=SQ[:], in_=a[:], func=AF.Square), reads=[a], writes=[SQ])
                fw.op("dve", lambda g, ss=ss: g.tensor_reduce(out=ss[:, 0:6], in_=SQ[:].rearrange("p (h c) -> p h c", c=128), axis=AX.X, op=ALU.add),
                      reads=[SQ], writes=[ss])
                fw.op("dve", lambda g, ss=ss: g.tensor_scalar(out=ss[:, 0:6], in0=ss[:, 0:6], scalar1=1.0 / HD, scalar2=EPS, op0=ALU.mult, op1=ALU.add),
                      reads=[ss], writes=[ss])
                fw.op("act", lambda g, ss=ss: g.sqrt(out=ss[:, 0:6], in_=ss[:, 0:6]), reads=[ss], writes=[ss])
                fw.op("dve", lambda g, ss=ss: g.reciprocal(out=ss[:, 6:12], in_=ss[:, 0:6]), reads=[ss], writes=[ss])
                a3 = a[:].rearrange("p (h c) -> p h c", c=128)
                fw.op("dve", lambda g, a3=a3, ss=ss, a=a: g.tensor_tensor(out=a3, in0=a3, in1=ss[:, 6:12].unsqueeze(2).to_broadcast([128, 6, 128]), op=ALU.mult),
                      reads=[a, ss], writes=[a])
                fw.op("pool", lambda g, a3=a3, a=a: g.tensor_tensor(out=a3, in0=a3, in1=nwb[:].unsqueeze(1).to_broadcast([128, 6, 128]), op=ALU.mult),
                      reads=[a, nwb], writes=[a])
                fw.op("act", lambda g, z=z: g.activation(out=z[:], in_=z[:], func=AF.Silu), reads=[z], writes=[z])
                fw.op("dve", lambda g, a=a, z=z: g.tensor_tensor(out=a[:], in0=a[:], in1=z[:], op=ALU.mult), reads=[a, z], writes=[a])
                fw.dma("act", Y[rs, ycol:ycol + DG], a[:], reads=[a], writes=[b_Y], key=a)

    def phase_ml(l, heads=range(NH)):
        last = (l == DEPTH - 1)
        with fw.scope() as sc:
            MG = sc.sb("MG", [128, NCH, 24])
            fw.dma("sp", MG[:], UT[:, T_MG:T_MG + 24].rearrange("(n p) c -> p n c", p=128), reads=[b_UT], writes=[MG], key=MG)
            mgb = sc.sb("mgb", [128, 24])
            fw.dma("sp", mgb[:], mgb_in[l:l + 1, :].partition_broadcast(128)[:, 0, :], writes=[mgb], key=mgb)
            fw.op("dve", lambda g: g.tensor_tensor(out=MG[:], in0=MG[:], in1=mgb[:].unsqueeze(1).to_broadcast([128, NCH, 24]), op=ALU.add),
                  reads=[MG, mgb], writes=[MG])
            ELI = sc.sb("ELI", [128, NCH, 12])
            LF = sc.sb("LF", [128, NCH, 12])
            for d in range(2):
                fw.op("act", lambda g, d=d: g.activation(out=ELI[:, :, d * 6:(d + 1) * 6], in_=MG[:, :, d * 12:d * 12 + 6], func=AF.Exp),
                      reads=[MG], writes=[ELI])
                fw.op("act", lambda g, d=d: g.activation(out=LF[:, :, d * 6:(d + 1) * 6], in_=MG[:, :, d * 12 + 6:d * 12 + 12], func=AF.Exp, scale=-1.0),
                      reads=[MG], writes=[LF])
            fw.op("act", lambda g: g.activation(out=LF[:], in_=LF[:], func=AF.Ln, bias=ones[:, 0:1]), reads=[LF, ones], writes=[LF])
            fw.op("dve", lambda g: g.tensor_scalar(out=LF[:], in0=LF[:], scalar1=-1.0, scalar2=None, op0=ALU.mult), reads=[LF], writes=[LF])
            fw.op("dve", lambda g: g.tensor_scalar(out=ELI[:], in0=ELI[:], scalar1=HD ** -0.5, scalar2=None, op0=ALU.mult), reads=[ELI], writes=[ELI])
            qT = sc.sb("mq", [128, NTOK])
            kT = sc.sb("mk", [128, NTOK])
            Vt = sc.sb("mv", [128, NCH, 129])
            fw.op("dve", lambda g: g.memset(Vt[:, :, 128:129], 1.0), writes=[Vt])
            PS = RR([sc.ps("mps%d" % i, [128, 512]) for i in range(8)])
            Cst = [sc.sb("C%d" % d, [128, 129]) for d in range(2)]
            names = ["gtri", "grep", "eDT", "EG", "Ke", "kqTm", "PT", "QsT", "H"]
            WS = []
            for i in range(4):
                w_ = make_ws(sc, "m%d" % i, names)
                w_["ps"] = PS
                WS.append(w_)
            wsi = 0
            for h in heads:
                fw.dma("sp", qT[:], UF[2304 + h * 128:2304 + (h + 1) * 128, :], reads=[b_UF], writes=[qT], key=qT)
                fw.dma("act", kT[:], UF[3072 + h * 128:3072 + (h + 1) * 128, :], reads=[b_UF], writes=[kT], key=kT)
                fw.dma("sp", Vt[:, :, 0:128], UT[:, T_MV + h * 128:T_MV + (h + 1) * 128].rearrange("(n p) c -> p n c", p=128),
                       reads=[b_UT], writes=[Vt], key=Vt)
                for d in range(2):
                    fw.op("dve", lambda g, d=d: g.memset(Cst[d][:], 0.0), writes=[Cst[d]])
                orders = [chunk_order(0), chunk_order(1)]

                def unit_vars(step, d):
                    n = orders[d][step]
                    return n, WS[(step % 2) * 2 + d], d * 6 + h, slice(n * 128, (n + 1) * 128)

                def m_prep(step, d):
                    n, ws, u, cs = unit_vars(step, d)
                    decay_prep(ws, LF[:, n, u:u + 1], LF, d, False)
                    fw.op("dve", lambda g, ws=ws, n=n, u=u: g.tensor_tensor(out=ws["sm"][:, 4:5], in0=ws["sm"][:, 3:4], in1=ELI[:, n, u:u + 1], op=ALU.mult),
                          reads=[ws["sm"], ELI], writes=[ws["sm"]])
                    yield
                    p = PS.next()
                    fw.tr(p, p[:, 0:128], kT, kT[:, cs], ident)
                    fw.mm(p, p[:, 128:256], kT, kT[:, cs], qT, qT[:, cs])
                    fw.op("act", lambda g, p=p, ws=ws: g.activation(out=ws["Ke"][:], in_=p[:, 0:128], func=AF.Copy, scale=ws["sm"][:, 4:5]),
                          reads=[p, ws["sm"]], writes=[ws["Ke"]])
                    yield
                    fw.op("dve", lambda g, p=p, ws=ws, d=d: g.tensor_tensor(out=ws["kqTm"][:], in0=p[:, 128:256], in1=LIT[d], op=ALU.mult),
                          reads=[p, cmask], writes=[ws["kqTm"]])
                    fw.op("dve", lambda g, ws=ws, n=n, u=u: g.scalar_tensor_tensor(out=ws["PT"][:], in0=ws["eDT"][:], scalar=ELI[:, n, u:u + 1], in1=ws["kqTm"][:],
                                                                                 op0=ALU.mult, op1=ALU.mult), reads=[ws["eDT"], ELI, ws["kqTm"]], writes=[ws["PT"]])
                    fw.op("pool", lambda g, ws=ws, cs=cs: g.tensor_tensor(out=ws["QsT"][:], in0=qT[:, cs], in1=ws["EG"][:], op=ALU.mult),
                          reads=[qT, ws["EG"]], writes=[ws["QsT"]])
                    yield

                def m_seq(step, d):
                    n, ws, u, cs = unit_vars(step, d)
                    p = PS.next()
                    fw.mm(p, p[:, 0:129], ws["QsT"], ws["QsT"][:], Cst[d], Cst[d][:], start=True, stop=False)
                    fw.mm(p, p[:, 0:129], ws["PT"], ws["PT"][:], Vt, Vt[:, n, :], start=False, stop=True)
                    yield
                    fw.mm(p, p[:, 256:385], ws["Ke"], ws["Ke"][:], Vt, Vt[:, n, :])
                    fw.op("dve", lambda g, p=p, ws=ws, d=d: g.scalar_tensor_tensor(out=Cst[d][:], in0=Cst[d][:], scalar=ws["sm"][:, 0:1], in1=p[:, 256:385],
                                                                                op0=ALU.mult, op1=ALU.add), reads=[p, ws["sm"], Cst[d]], writes=[Cst[d]])
                    if not (last and n < 2):
                        fw.op("act", lambda g, p=p, ws=ws: g.activation(out=ws["sm"][:, 7:8], in_=p[:, 128:129], func=AF.Abs), reads=[p], writes=[ws["sm"]])
                        fw.op("dve", lambda g, ws=ws: g.tensor_scalar(out=ws["sm"][:, 5:6], in0=ws["sm"][:, 7:8], scalar1=1.0, scalar2=None, op0=ALU.max),
                              reads=[ws["sm"]], writes=[ws["sm"]])
                        fw.op("dve", lambda g, ws=ws: g.reciprocal(out=ws["sm"][:, 6:7], in_=ws["sm"][:, 5:6]), reads=[ws["sm"]], writes=[ws["sm"]])
                        fw.op("act", lambda g, p=p, ws=ws: g.activation(out=ws["H"][:], in_=p[:, 0:128], func=AF.Copy, scale=ws["sm"][:, 6:7]),
                              reads=[p, ws["sm"]], writes=[ws["H"]])
                        fw.dma("act", OFs[d][n * 128:(n + 1) * 128, h * 128:(h + 1) * 128], ws["H"][:], reads=[ws["H"]], writes=[b_OF[d]], key=ws["H"])

                    yield

                run_rr([m_prep(0, 0), m_prep(0, 1)])
                for step in range(NCH):
                    gens = [m_seq(step, 0), m_seq(step, 1)]
                    if step + 1 < NCH:
                        gens += [m_prep(step + 1, 0), m_prep(step + 1, 1)]
                    run_rr(gens)
        if opts.get("finalize", True):
            finalize(l, "ml")

    def phase_gdn_full(l, **kw):
        phase_gdn(l, **kw)
        if opts.get("finalize", True):
            finalize(l, "gdn")

    CGW = 64
    NCG = DHY // CGW
    S3 = 3 * CGW
    QG = 512 // CGW
    XC = dscr("XC", [NCG, 3, 64, 64 * CGW])
    b_XC = Buf("XC", multi=True)
    RG = 8

    def hy_filter_mlp(sc, l, featT_in, Lf, name):
        featT = sc.sb(name + "featT", [33, Lf])
        fw.dma("sp", featT[:], featT_in[:, :], writes=[featT], key=featT)
        w1 = sc.sb(name + "w1", [33, 64])
        fw.dma("sp", w1[:], hyw1_in[l, :, :], writes=[w1], key=w1)
        w2 = sc.sb(name + "w2", [64, 64])
        fw.dma("sp", w2[:], hyw2_in[l, :, :], writes=[w2], key=w2)
        hp = sc.sb(name + "hp", [64, 8])
        fw.dma("sp", hp[:, 0:3], hyp_in[l, :, :], writes=[hp], key=hp)
        fw.op("dve", lambda g: g.tensor_tensor(out=hp[:, 3:4], in0=hp[:, 0:1], in1=hp[:, 2:3], op=ALU.mult), reads=[hp], writes=[hp])
        fw.op("dve", lambda g: g.tensor_tensor(out=hp[:, 4:5], in0=hp[:, 1:2], in1=hp[:, 2:3], op=ALU.mult), reads=[hp], writes=[hp])
        fw.op("dve", lambda g: g.memset(hp[:, 5:6], -math.pi), writes=[hp])
        h1T = sc.sb(name + "h1T", [64, Lf])
        kt = sc.sb(name + "kt", [64, 512])
        h2T = sc.sb(name + "h2T", [64, Lf])
        pss = RR([sc.ps(name + "fps%d" % i, [64, 512]) for i in range(2)])
        for (wt, kdim, src, dst, bcol) in ((w1, 33, featT, h1T, 3), (w2, 64, h1T, h2T, 4)):
            for t0 in range(0, Lf, 512):
                nt = min(512, Lf - t0)
                p = pss.next()
                fw.mm(p, p[:, 0:nt], wt, wt[0:kdim, :], src, src[0:kdim, t0:t0 + nt])
                fw.op("dve", lambda g, p=p, dst=dst, t0=t0, nt=nt, bcol=bcol: g.tensor_scalar(
                    out=dst[:, t0:t0 + nt], in0=p[:, 0:nt], scalar1=hp[:, 2:3], scalar2=hp[:, bcol:bcol + 1], op0=ALU.mult, op1=ALU.add),
                    reads=[p, hp], writes=[dst])
                fw.op("dve", lambda g, dst=dst, t0=t0, nt=nt: g.tensor_scalar(
                    out=kt[:, 0:nt], in0=dst[:, t0:t0 + nt], scalar1=1.0 / (2.0 * math.pi), scalar2=12582912.0, op0=ALU.mult, op1=ALU.add),
                    reads=[dst], writes=[kt])
                fw.op("dve", lambda g, nt=nt: g.tensor_scalar(out=kt[:, 0:nt], in0=kt[:, 0:nt], scalar1=12582912.0, scalar2=None, op0=ALU.subtract),
                      reads=[kt], writes=[kt])
                fw.op("dve", lambda g, dst=dst, t0=t0, nt=nt: g.scalar_tensor_tensor(
                    out=dst[:, t0:t0 + nt], in0=dst[:, t0:t0 + nt], scalar=1.0 / (2.0 * math.pi), in1=kt[:, 0:nt], op0=ALU.mult, op1=ALU.subtract),
                    reads=[dst, kt], writes=[dst])
                fw.op("act", lambda g, dst=dst, t0=t0, nt=nt: g.activation(out=dst[:, t0:t0 + nt], in_=dst[:, t0:t0 + nt], func=AF.Sin, scale=2.0 * math.pi),
                      reads=[dst], writes=[dst])
        return h2T

    def phase_hy(l, cgs=None):
        cgs = range(NCG) if cgs is None else cgs
        last = (l == DEPTH - 1)
        with fw.scope() as sc:
            h2T = sc.sb("h2Tp", [64, SEQ])
            with fw.scope() as sm_:
                h2tmp = hy_filter_mlp(sm_, l, featL_in, SEQ, "L")
                fw.op("pool", lambda g: g.tensor_copy(out=h2T[:], in_=h2tmp[:]), reads=[h2tmp], writes=[h2T])
            w3t = sc.sb("w3t", [64, 2048])
            fw.dma("sp", w3t[:], hyw3_in[l, :, :], writes=[w3t], key=w3t)
            F1 = sc.sb("F1", [64, 2, 128], BF16)
            fw.dma("sp", F1[:], F1_in[:, :, :], writes=[F1], key=F1)
            F4 = sc.sb("F4", [128, 2, 64], BF16)
            fw.dma("sp", F4[:], F4_in[:, :, :], writes=[F4], key=F4)
            negt = sc.sb("negt", [64, 64])
            fw.dma("sp", negt[:], negt_in[:, :], writes=[negt], key=negt)
            drow = sc.sb("drow", [64, 512])
            fw.dma("sp", drow[:], drow_in[0:1, :].partition_broadcast(64)[:, 0, :], writes=[drow], key=drow)
            skipb = sc.sb("skipb", [64, 2, 512])
            fw.dma("sp", skipb[:], hyskip_in[l:l + 1, :].partition_broadcast(64)[:, 0, :].rearrange("p (o c) -> p o c", o=2), writes=[skipb], key=skipb)
            PS = RR([sc.ps("hps%d" % i, [128, 512]) for i in range(5)])
            PSB = RR([sc.ps("hpb%d" % i, [128, 1024], BF16) for i in range(1)])
            VH = sc.sb("VH", [64, S3, 64], BF16)
            vf = sc.sb("vf", [64, CGW, 64])
            evq = RR(["act", "dve"])

            def evac(p_ap, out_ap, ptile, otile, eng=None):
                e = eng or evq.next()
                if e == "act":
                    fw.op("act", lambda g: g.copy(out=out_ap, in_=p_ap), reads=[ptile], writes=[otile])
                else:
                    fw.op(e, lambda g: g.tensor_copy(out=out_ap, in_=p_ap), reads=[ptile], writes=[otile])

            for cg in cgs:
                c0 = cg * CGW
                with fw.scope() as s0:
                    raws = RR([s0.sb("raw%d" % i, [64, 64, CGW]) for i in range(2)])
                    cvs = RR([s0.sb("cv%d" % i, [64, CGW, 64]) for i in range(2)])
                    tmp = s0.sb("ctmp", [64, 64, CGW])
                    cwb = s0.sb("cwb", [64, 3, 3, CGW])
                    fw.dma("sp", cwb[:], hyconv_in[l, :, :].rearrange("j (a c) -> j a c", a=3)[:, :, c0:c0 + CGW].partition_broadcast(64),
                           writes=[cwb], key=cwb)
                    for a in (2, 0, 1, 3):
                        col = (T_HP + a * 512 + c0) if a < 3 else (T_HZ + c0)
                        raw = raws.next()
                        fw.dma("sp" if a % 2 == 0 else "act", raw[:],
                               UT[CTXL:, col:col + CGW].rearrange("(p q) c -> p q c", q=64), reads=[b_UT], writes=[raw], key=raw)
                        if a == 3:
                            cvt = cvs.next()
                            fw.op("act", lambda g, cvt=cvt, raw=raw: g.activation(out=cvt[:].rearrange("p c q -> p q c"), in_=raw[:], func=AF.Silu), reads=[raw], writes=[cvt])
                            fw.dma("act", XC[cg, 2, :, :], cvt[:].rearrange("p c q -> p (c q)"), reads=[cvt], writes=[b_XC], key=cvt)
                            continue
                        dst = vf if a == 2 else cvs.next()
                        dv = dst[:].rearrange("p c q -> p q c")
                        fw.op("dve", lambda g, a=a, dv=dv, dst=dst, raw=raw: g.tensor_tensor(out=dv, in0=raw[:], in1=cwb[:, 1, a, :].unsqueeze(1).to_broadcast([64, 64, CGW]), op=ALU.mult),
                              reads=[raw, cwb], writes=[dst])
                        fw.op("pool", lambda g, a=a, raw=raw: g.tensor_tensor(out=tmp[:, 0:63, :], in0=raw[:, 0:63, :], in1=cwb[:, 0, a, :].unsqueeze(1).to_broadcast([64, 63, CGW]), op=ALU.mult),
                              reads=[raw, cwb], writes=[tmp])
                        fw.op("dve", lambda g, dv=dv, dst=dst: g.tensor_tensor(out=dv[:, 1:64, :], in0=dv[:, 1:64, :], in1=tmp[:, 0:63, :], op=ALU.add),
                              reads=[dst, tmp], writes=[dst])
                        fw.op("pool", lambda g, a=a, raw=raw: g.tensor_tensor(out=tmp[:, 0:63, :], in0=raw[:, 1:64, :], in1=cwb[:, 2, a, :].unsqueeze(1).to_broadcast([64, 63, CGW]), op=ALU.mult),
                              reads=[raw, cwb], writes=[tmp])
                        fw.op("dve", lambda g, dv=dv, dst=dst: g.tensor_tensor(out=dv[:, 0:63, :], in0=dv[:, 0:63, :], in1=tmp[:, 0:63, :], op=ALU.add),
                              reads=[dst, tmp], writes=[dst])
                        if a < 2:
                            fw.dma("act", XC[cg, a, :, :], dst[:].rearrange("p c q -> p (c q)"), reads=[dst], writes=[b_XC], key=dst)
                    fw.op("act", lambda g: g.copy(out=VH[:, 0:CGW, :], in_=vf[:]), reads=[vf], writes=[VH])
                for o in range(2):
                    with fw.scope() as sa:
                        Wt = sa.sb("Wt", [64, CGW, 64])
                        for q in range(64):
                            fw.op("act", lambda g, q=q: g.activation(out=Wt[:, :, q], in_=drow[:, c0:c0 + CGW], func=AF.Exp, scale=negt[:, q:q + 1]),
                                  reads=[drow, negt], writes=[Wt])
                        Hraw = sa.sb("Hraw", [64, 64, 2 * CGW])
                        Hw = sa.sb("Hw", [64, 2 * CGW, 64])
                        part = sa.sb("part", [64, 2 * CGW])
                        tot = sa.sb("tot", [64, 2 * CGW])
                        h2v = h2T[:, :].rearrange("k (p q) -> k q p", q=64)
                        w3v = w3t[:, o * 1024:(o + 1) * 1024].rearrange("k (d c) -> k d c", d=2)[:, :, c0:c0 + CGW]
                        NQ = 512 // (2 * CGW)
                        for q0 in range(0, 64, NQ):
                            p = PS.next()
                            for j in range(NQ):
                                fw.mm(p, p[0:64, j * 2 * CGW:(j + 1) * 2 * CGW], h2T, h2v[:, q0 + j, :], w3t, w3v)
                            evac(p[0:64, :], Hraw[:, q0:q0 + NQ, :].rearrange("p q c -> p (q c)"), p, Hraw)
                        for d in range(2):
                            fw.op("dve", lambda g, d=d: g.tensor_tensor(out=Hw[:, d * CGW:(d + 1) * CGW, :], in0=Hraw[:, :, d * CGW:(d + 1) * CGW].rearrange("p q c -> p c q"),
                                                                       in1=Wt[:], op=ALU.mult), reads=[Hraw, Wt], writes=[Hw])
                        fw.op("act", lambda g: g.activation(out=Hraw[:].rearrange("p q c -> p (q c)"), in_=Hw[:].rearrange("p c q -> p (c q)"), func=AF.Abs),
                              reads=[Hw], writes=[Hraw])
                        fw.op("dve", lambda g: g.tensor_reduce(out=part[:], in_=Hraw[:].rearrange("p q c -> p (q c)").rearrange("p (c q) -> p c q", q=64), axis=AX.X, op=ALU.add),
                              reads=[Hraw], writes=[part])
                        p = PS.next()
                        fw.mm(p, p[0:64, 0:2 * CGW], ones, ones[0:64, 0:64], part, part[:])
                        fw.op("dve", lambda g, p=p: g.tensor_scalar(out=tot[:], in0=p[0:64, 0:2 * CGW], scalar1=EPS, scalar2=None, op0=ALU.add), reads=[p], writes=[tot])
                        fw.op("dve", lambda g: g.reciprocal(out=tot[:], in_=tot[:]), reads=[tot], writes=[tot])
                        fw.op("dve", lambda g: g.tensor_tensor(out=VH[:, CGW:S3, :], in0=Hw[:], in1=tot[:].unsqueeze(2).to_broadcast([64, 2 * CGW, 64]), op=ALU.mult),
                              reads=[Hw, tot], writes=[VH])
                    with fw.scope() as sbc:
                     Zp = sbc.sb("Zp", [128, CGW, 128], BF16)
                     with fw.scope() as sb_:
                         Bt = sb_.sb("Bt", [64, S3, 2, 64], BF16)
                         w2s = RR([sb_.sb("w2s%d" % i, [64, RG, 4, 128], BF16) for i in range(2)])
                         w3s = RR([sb_.sb("w3s%d" % i, [128, RG, 128], BF16) for i in range(2)])
                         XS = RR([sb_.sb("XS%d" % i, [128, RG, 2, S3]) for i in range(1)])
                         KA = sb_.sb("KA", [128, RG, CGW])
                         KB = sb_.sb("KB", [128, RG, CGW])
                         Yt = RR([sb_.sb("Yt%d" % i, [128, RG, CGW], BF16) for i in range(2)])
                         for rh in range(2):
                             for s4 in range(0, S3, 4):
                                 p = PS.next()
                                 for j in range(4):
                                     fw.mm(p, p[0:64, j * 128:(j + 1) * 128], VH, VH[:, s4 + j, :], F1, F1[:, rh, :])
                                 evac(p[0:64, :], Bt[:, s4:s4 + 4, :, :].rearrange("q s i r -> q (s i r)"), p, Bt)
                             for rg in range(64 // RG):
                                 r0 = rh * 64 + rg * RG
                                 w2 = w2s.next()
                                 fw.dma("sp", w2[:], W2_in[:, r0:r0 + RG, :, :], writes=[w2], key=w2)
                                 w3_ = w3s.next()
                                 fw.dma("pool", w3_[:], W3_in[:, r0:r0 + RG, :], writes=[w3_], key=w3_)
                                 xs = XS.next()
                                 for rl in range(RG):
                                     rloc = rg * RG + rl
                                     p = PS.next()
                                     for v in range(2):
                                         fw.mm(p, p[:, v * 256:v * 256 + S3], w2, w2[:, rl, 2 * v, :], Bt, Bt[:, :, 0, rloc], start=True, stop=False)
                                         fw.mm(p, p[:, v * 256:v * 256 + S3], w2, w2[:, rl, 2 * v + 1, :], Bt, Bt[:, :, 1, rloc], start=False, stop=True)
                                     evac(p[:, :].rearrange("p (v c) -> p v c", v=2)[:, :, 0:S3], xs[:, rl, :, :], p, xs)
                                 fw.op("pool", lambda g, xs=xs: g.tensor_tensor(out=KA[0:64], in0=xs[0:64, :, 0, CGW:2 * CGW], in1=xs[0:64, :, 0, 2 * CGW:S3], op=ALU.add), reads=[xs], writes=[KA])
                                 fw.op("pool", lambda g, xs=xs: g.tensor_tensor(out=KA[64:128], in0=xs[64:128, :, 1, CGW:2 * CGW], in1=xs[64:128, :, 1, 2 * CGW:S3], op=ALU.add), reads=[xs], writes=[KA])
                                 fw.op("pool", lambda g, xs=xs: g.tensor_tensor(out=KB[0:64], in0=xs[0:64, :, 1, 2 * CGW:S3], in1=xs[0:64, :, 1, CGW:2 * CGW], op=ALU.subtract), reads=[xs], writes=[KB])
                                 fw.op("pool", lambda g, xs=xs: g.tensor_tensor(out=KB[64:128], in0=xs[64:128, :, 0, CGW:2 * CGW], in1=xs[64:128, :, 0, 2 * CGW:S3], op=ALU.subtract), reads=[xs], writes=[KB])
                                 fw.op("dve", lambda g, xs=xs: g.tensor_tensor(out=KA[:], in0=KA[:], in1=xs[:, :, 0, 0:CGW], op=ALU.mult), reads=[KA, xs], writes=[KA])
                                 fw.op("dve", lambda g, xs=xs: g.tensor_tensor(out=KB[:], in0=KB[:], in1=xs[:, :, 1, 0:CGW], op=ALU.mult), reads=[KB, xs], writes=[KB])
                                 yt = Yt.next()
                                 fw.op("dve", lambda g, yt=yt: g.tensor_tensor(out=yt[:], in0=KA[:], in1=KB[:], op=ALU.add), reads=[KA, KB], writes=[yt])
                                 p = PS.next()
                                 for rl in range(RG):
                                     fw.mm(p, p[:, rl * CGW:(rl + 1) * CGW], w3_, w3_[:, rl, :], yt, yt[:, rl, :])
                                 evac(p[:, 0:RG * CGW].rearrange("p (r c) -> p r c", c=CGW), Zp[:, :, r0:r0 + RG].rearrange("p c r -> p r c"), p, Zp)
                     with fw.scope() as sc_:
                         ZT = sc_.sb("ZT", [128, CGW, 2, 64], BF16)
                         xo = sc_.sb("xo", [64, CGW, 64])
                         sv = sc_.sb("sv", [64, CGW, 64])
                         fw.dma("sp", xo[:].rearrange("p c q -> p (c q)"), XC[cg, o, :, :], reads=[b_XC], writes=[xo], key=xo)
                         fw.op("pool", lambda g: g.tensor_tensor(out=sv[:], in0=vf[:], in1=skipb[:, o, c0:c0 + CGW].unsqueeze(2).to_broadcast([64, CGW, 64]), op=ALU.mult),
                               reads=[vf, skipb], writes=[sv])
                         if o == 1:
                             sz = sc_.sb("sz", [64, CGW, 64])
                             yo = sc_.sb("yo", [64, 64, CGW])
                             fw.dma("act", sz[:].rearrange("p c q -> p (c q)"), XC[cg, 2, :, :], reads=[b_XC], writes=[sz], key=sz)
                             fw.op("pool", lambda g: g.tensor_tensor(out=xo[:], in0=xo[:], in1=sz[:], op=ALU.mult), reads=[xo, sz], writes=[xo])
                         for c8 in range(0, CGW, 8):
                             pb = PSB.next()
                             for j in range(8):
                                 fw.tr(pb, pb[:, j * 128:(j + 1) * 128], Zp, Zp[:, c8 + j, :], identb)
                             evac(pb[:, :], ZT[:, c8:c8 + 8, :, :].rearrange("r c i q -> r (c i q)"), pb, ZT)
                         for q0 in range(0, 64, QG):
                             p = PS.next()
                             fw.mm(p, p[0:64, :], F4, F4[:, 0, :], ZT, ZT[:, :, 0, q0:q0 + QG], start=True, stop=False)
                             fw.mm(p, p[0:64, :], F4, F4[:, 1, :], ZT, ZT[:, :, 1, q0:q0 + QG], start=False, stop=True)
                             pv = p[0:64, :].rearrange("p (c q) -> p c q", q=QG)
                             fw.op("dve", lambda g, pv=pv, q0=q0: g.tensor_tensor(out=sv[:, :, q0:q0 + QG], in0=pv, in1=sv[:, :, q0:q0 + QG], op=ALU.add), reads=[p, sv], writes=[sv])
                             if o == 0:
                                 fw.op("pool", lambda g, q0=q0: g.tensor_tensor(out=vf[:, :, q0:q0 + QG], in0=sv[:, :, q0:q0 + QG], in1=xo[:, :, q0:q0 + QG], op=ALU.mult),
                                       reads=[sv, xo], writes=[vf])
                             else:
                                 fw.op("pool", lambda g, q0=q0: g.tensor_tensor(out=yo[:, q0:q0 + QG, :].rearrange("p q c -> p c q"), in0=sv[:, :, q0:q0 + QG], in1=xo[:, :, q0:q0 + QG], op=ALU.mult),
                                       reads=[sv, xo], writes=[yo])
                         if o == 0:
                             fw.op("act", lambda g: g.copy(out=VH[:, 0:CGW, :], in_=vf[:]), reads=[vf], writes=[VH])
                         else:
                             fw.dma("act", Y[CTXL:, Y_H + c0:Y_H + c0 + CGW].rearrange("(p q) c -> p q c", q=64), yo[:], reads=[yo], writes=[b_Y], key=yo)

    def phase_hy_ctx(l):
        with fw.scope() as sc:
            h2Tc = hy_filter_mlp(sc, l, featC_in, CTXL, "C")
            w3c = sc.sb("w3c", [64, 2048])
            fw.dma("sp", w3c[:], hyw3_in[l, :, :], writes=[w3c], key=w3c)
            negtc = sc.sb("negtc", [128, CTXL])
            fw.dma("sp", negtc[:], negtc_in[0:1, :].partition_broadcast(128)[:, 0, :], writes=[negtc], key=negtc)
            dcol = sc.sb("dcol", [128, 4])
            fw.dma("sp", dcol[:], dcol_in[:, :], writes=[dcol], key=dcol)
            cwT = sc.sb("cwT", [128, 12, 3])
            fw.dma("sp", cwT[:], hyconvT_in[l, :, :, :], writes=[cwT], key=cwT)
            skc = sc.sb("skc", [128, 2, 4])
            fw.dma("sp", skc[:], hyskipT_in[l, :, :, :], writes=[skc], key=skc)
            PS = RR([sc.ps("cps%d" % i, [128, 512]) for i in range(4)])
            XT = sc.sb("XT", [128, 12, CTXL])
            XCc = sc.sb("XCc", [128, 12, CTXL])
            zt = [sc.sb("zt%d" % i, [128, 512]) for i in range(2)]
            yo = [sc.sb("yoc%d" % i, [128, 512]) for i in range(2)]
            tl = sc.sb("ctile", [128, 2048])
            for tt in range(2):
                fw.dma("sp", tl[:], UT[tt * 128:(tt + 1) * 128, T_HP:T_HP + 2048], reads=[b_UT], writes=[tl], key=tl)
                for g4 in range(3):
                    p = PS.next()
                    for j in range(4):
                        gi = g4 * 4 + j
                        fw.tr(p, p[:, j * 128:(j + 1) * 128], tl, tl[:, gi * 128:(gi + 1) * 128], ident)
                    fw.op("act", lambda g, p=p, g4=g4, tt=tt: g.copy(out=XT[:, g4 * 4:(g4 + 1) * 4, tt * 128:(tt + 1) * 128],
                                                                   in_=p[:, :].rearrange("p (g t) -> p g t", g=4)), reads=[p], writes=[XT])
                fw.op("act", lambda g, tt=tt: g.activation(out=zt[tt][:], in_=tl[:, 1536:2048], func=AF.Silu), reads=[tl], writes=[zt[tt]])
            for gi in range(12):
                fw.op("dve", lambda g, gi=gi: g.tensor_scalar(out=XCc[:, gi, :], in0=XT[:, gi, :], scalar1=cwT[:, gi, 1:2], scalar2=None, op0=ALU.mult),
                      reads=[XT, cwT], writes=[XCc])
                fw.op("dve", lambda g, gi=gi: g.scalar_tensor_tensor(out=XCc[:, gi, 1:CTXL], in0=XT[:, gi, 0:CTXL - 1], scalar=cwT[:, gi, 0:1], in1=XCc[:, gi, 1:CTXL],
                                                                    op0=ALU.mult, op1=ALU.add), reads=[XT, cwT, XCc], writes=[XCc])
                fw.op("dve", lambda g, gi=gi: g.scalar_tensor_tensor(out=XCc[:, gi, 0:CTXL - 1], in0=XT[:, gi, 1:CTXL], scalar=cwT[:, gi, 2:3], in1=XCc[:, gi, 0:CTXL - 1],
                                                                    op0=ALU.mult, op1=ALU.add), reads=[XT, cwT, XCc], writes=[XCc])
            Wc = sc.sb("Wc", [128, CTXL])
            hf = [sc.sb("hf%d" % i, [128, CTXL]) for i in range(2)]
            habs = sc.sb("habs", [128, CTXL])
            hs = sc.sb("hs", [128, 4])
            acc = sc.sb("acc", [128, CTXL])
            acc2 = sc.sb("acc2", [128, CTXL])
            tmpc = sc.sb("tmpc", [128, CTXL])
            vcur = sc.sb("vcur", [128, CTXL])
            for gi in range(4):
                fw.op("act", lambda g, gi=gi: g.activation(out=Wc[:], in_=negtc[:], func=AF.Exp, scale=dcol[:, gi:gi + 1]), reads=[negtc, dcol], writes=[Wc])
                fw.op("pool", lambda g, gi=gi: g.tensor_copy(out=vcur[:], in_=XCc[:, 8 + gi, :]), reads=[XCc], writes=[vcur])
                for o in range(2):
                    for dr in range(2):
                        col0 = o * 1024 + dr * 512 + gi * 128
                        p = PS.next()
                        fw.mm(p, p[:, 0:CTXL], w3c, w3c[:, col0:col0 + 128], h2Tc, h2Tc[:, :])
                        fw.op("dve", lambda g, p=p, dr=dr: g.tensor_tensor(out=hf[dr][:], in0=p[:, 0:CTXL], in1=Wc[:], op=ALU.mult), reads=[p, Wc], writes=[hf[dr]])
                        fw.op("act", lambda g, dr=dr: g.activation(out=habs[:], in_=hf[dr][:], func=AF.Abs), reads=[hf[dr]], writes=[habs])
                        fw.op("dve", lambda g: g.tensor_reduce(out=hs[:, 0:1], in_=habs[:], axis=AX.X, op=ALU.add), reads=[habs], writes=[hs])
                        fw.op("dve", lambda g: g.tensor_scalar(out=hs[:, 1:2], in0=hs[:, 0:1], scalar1=EPS, scalar2=None, op0=ALU.add), reads=[hs], writes=[hs])
                        fw.op("dve", lambda g: g.reciprocal(out=hs[:, 2:3], in_=hs[:, 1:2]), reads=[hs], writes=[hs])
                        fw.op("dve", lambda g, dr=dr: g.tensor_scalar(out=hf[dr][:], in0=hf[dr][:], scalar1=hs[:, 2:3], scalar2=None, op0=ALU.mult), reads=[hf[dr], hs], writes=[hf[dr]])
                    fw.op("dve", lambda g, o=o, gi=gi: g.tensor_scalar(out=acc[:], in0=vcur[:], scalar1=skc[:, o, gi:gi + 1], scalar2=None, op0=ALU.mult),
                          reads=[vcur, skc], writes=[acc])
                    fw.op("pool", lambda g: g.memset(acc2[:], 0.0), writes=[acc2])
                    for m in range(CTXL):
                        n_ = CTXL - m
                        fw.op("dve", lambda g, m=m, n_=n_: g.scalar_tensor_tensor(out=acc[:, m:], in0=vcur[:, 0:n_], scalar=hf[0][:, m:m + 1], in1=acc[:, m:],
                                                                              op0=ALU.mult, op1=ALU.add), reads=[vcur, hf[0], acc], writes=[acc])
                        fw.op("dve", lambda g, m=m, n_=n_: g.scalar_tensor_tensor(out=acc2[:, 0:n_], in0=vcur[:, m:], scalar=hf[1][:, m:m + 1], in1=acc2[:, 0:n_],
                                                                              op0=ALU.mult, op1=ALU.add), reads=[vcur, hf[1], acc2], writes=[acc2])
                    fw.op("dve", lambda g: g.tensor_tensor(out=acc[:], in0=acc[:], in1=acc2[:], op=ALU.add), reads=[acc, acc2], writes=[acc])
                    fw.op("dve", lambda g, o=o, gi=gi: g.tensor_tensor(out=vcur[:], in0=acc[:], in1=XCc[:, o * 4 + gi, :], op=ALU.mult), reads=[acc, XCc], writes=[vcur])
                for tt in range(2):
                    p = PS.next()
                    fw.tr(p, p[:, 0:128], vcur, vcur[:, tt * 128:(tt + 1) * 128], ident)
                    fw.op("dve", lambda g, p=p, tt=tt, gi=gi: g.tensor_tensor(out=yo[tt][:, gi * 128:(gi + 1) * 128], in0=p[:, 0:128], in1=zt[tt][:, gi * 128:(gi + 1) * 128], op=ALU.mult),
                          reads=[p, zt[tt]], writes=[yo[tt]])
            for tt in range(2):
                fw.dma("act", Y[tt * 128:(tt + 1) * 128, Y_H:Y_H + DHY], yo[tt][:], reads=[yo[tt]], writes=[b_Y], key=yo[tt])

    def phase_hy_full(l, **kw):
        phase_hy(l, **kw)
        if l != DEPTH - 1 and opts.get("hyctx", True):
            phase_hy_ctx(l)
    scr = {"UF": (UF, b_UF), "UT": (UT, b_UT), "Y": (Y, b_Y), "xcur": (xcur, b_xcur), "ctxcur": (ctxcur, b_ctxcur),
           "OF": (OFs[0], b_OF[0]), "OB": (OFs[1], b_OF[1])}
    for key, (name, sl) in dbg_in.items():
        dst, dbuf = scr[name]
        dst = dst[sl]
        src = din("dbgin_" + key, list(dst.shape))
        fw.dma("sp", dst, src, writes=[dbuf], key=dbuf)
    PH = {"mod": phase_mod, "proj": phase_proj, "out": phase_out, "gdn": phase_gdn_full, "ml": phase_ml, "hy": phase_hy_full}
    for l in layers:
        for ph in ("mod", "proj", "gdn", "hy", "ml", "out"):
            if ph in phases and ph in PH:
                PH[ph](l, **opts.get(ph, {}))
    bd = Buf("dbg", multi=True)
    for name, sl in dbg.items():
        src, sbuf = scr[name]
        if sl != 1:
            src = src[sl]
        dt_ = dbg_tensor(name, src.shape)
        fw.dma("sp", dt_, src, reads=[sbuf], writes=[bd], key=bd)
    fw.fence([b_out, bd], engines=("sp",))
    return nc, fw


_HYC = {}


def make_hy_consts():
    if _HYC:
        return _HYC
    import ml_dtypes
    bf = ml_dtypes.bfloat16
    f = np.float32
    N = 2 * SEQ

    def feats(L):
        pos = np.arange(L, dtype=f)
        t = pos / f(max(L - 1, 1))
        ang = (f(2.0 * math.pi) * pos / f(L)).astype(f)
        bands = np.linspace(1e-4, 15, 16, dtype=f)
        ft = np.concatenate([t[:, None], np.cos(ang[:, None] * bands), -np.sin(ang[:, None] * bands)], axis=-1).astype(f)
        return np.ascontiguousarray(ft.T), t
    featL, tL = feats(SEQ)
    featC, tC = feats(CTXL)
    mind, maxd = math.log(1e-2) / 1.5, math.log(1e-2) / 0.3
    deltas = np.abs(np.linspace(mind, maxd, DHY, dtype=f)).astype(f)
    p = np.arange(64, dtype=np.float64)[:, None]
    r = np.arange(128, dtype=np.float64)[None, :]
    ang = 2 * np.pi * p * r / 128
    F1 = np.stack([np.concatenate([np.cos(ang[:, h * 64:(h + 1) * 64]), -np.sin(ang[:, h * 64:(h + 1) * 64])], axis=1) for h in range(2)], axis=1)
    q = np.arange(64, dtype=np.float64)[:, None, None]
    rr = np.arange(128, dtype=np.float64)[None, :, None]
    s_ = np.arange(64, dtype=np.float64)[None, None, :]
    th = 2 * np.pi * (q * s_ / 64 + q * rr / N)
    c, d = np.cos(th), -np.sin(th)
    W2 = np.stack([np.concatenate([c, d], -1), np.concatenate([-d, c], -1),
                   np.concatenate([-d, c], -1), np.concatenate([-c, -d], -1)], axis=2)
    s2 = np.arange(64, dtype=np.float64)[:, None, None]
    q2 = np.arange(64, dtype=np.float64)[None, None, :]
    th2 = 2 * np.pi * (q2 * s2 / 64 + q2 * rr / N)
    c2, d2 = np.cos(th2), np.sin(th2)
    W3 = np.concatenate([np.concatenate([c2, d2], -1), np.concatenate([-d2, c2], -1)], axis=0)
    r4 = np.arange(128, dtype=np.float64)[:, None]
    p4 = np.arange(64, dtype=np.float64)[None, :]
    a4 = 2 * np.pi * r4 * p4 / 128
    F4 = np.stack([np.cos(a4) / N, -np.sin(a4) / N], axis=1)
    _HYC.update({
        "featL": featL, "featC": featC,
        "negt": np.ascontiguousarray(-tL.reshape(64, 64)), "negtc": np.ascontiguousarray(-tC.reshape(1, CTXL)),
        "drow": np.ascontiguousarray(deltas.reshape(1, DHY)), "dcol": np.ascontiguousarray(deltas.reshape(4, 128).T),
        "F1": np.ascontiguousarray(F1.astype(f)).astype(bf), "F4": np.ascontiguousarray(F4.astype(f)).astype(bf),
        "W2": np.ascontiguousarray(W2.astype(f)).astype(bf), "W3": np.ascontiguousarray(W3.astype(f)).astype(bf),
        "identb": np.eye(128, dtype=f).astype(bf),
    })
    return _HYC


def make_cmask():
    i = np.arange(128)
    P_, F_ = i[:, None], i[None, :]
    m = np.stack([P_ <= F_, P_ >= F_, F_ < P_, F_ > P_, F_ >= P_, F_ <= P_], axis=1)
    return np.ascontiguousarray(m.astype(np.float32))


def make_in_maps(inputs, cores=range(8)):
    f = np.float32
    g = lambda k: np.asarray(inputs[k], dtype=f)
    x, c, ctx, c_ctx = g("x"), g("c"), g("ctx"), g("c_ctx")
    norm_w, mod_w, mod_b, w_in, w_out = g("norm_w"), g("mod_w"), g("mod_b"), g("w_in"), g("w_out")

    def pk(a):
        L_, _, N = a.shape
        return np.ascontiguousarray(a.reshape(L_, KC, 128, N).transpose(0, 2, 1, 3))

    shared = {
        "nwT": np.ascontiguousarray(norm_w.reshape(DEPTH, KC, 128).transpose(2, 0, 1)),
        "fnw": np.ascontiguousarray(g("final_norm").reshape(1, D)),
        "modw": pk(mod_w),
        "modbT": np.ascontiguousarray(mod_b[:, :2 * D].reshape(DEPTH, 32, 128).transpose(2, 0, 1)),
        "modbg": np.ascontiguousarray(mod_b[:, 2 * D:]),
        "winF": pk(w_in[:, :, FCOLS]),
        "winT": pk(w_in[:, :, TCOLS]),
        "wout": pk(w_out),
        "ident": np.eye(128, dtype=f),
        "cmask": make_cmask(),
        **make_hy_consts(),
        "hyw1": g("hy_w1"), "hyw2": g("hy_w2"), "hyw3": g("hy_w3"),
        "hyp": np.ascontiguousarray(np.stack([g("hy_b1"), g("hy_b2"), g("hy_freq")], axis=-1)),
        "hyskip": np.ascontiguousarray(g("hy_skip").reshape(DEPTH, 1024)),
        "hyconv": g("hy_conv"),
        "hyskipT": np.ascontiguousarray(g("hy_skip").reshape(DEPTH, 2, 4, 128).transpose(0, 3, 1, 2)),
        "hyconvT": np.ascontiguousarray(g("hy_conv").transpose(0, 2, 1).reshape(DEPTH, 12, 128, 3).transpose(0, 2, 1, 3)),
        "gnorm": g("gdn_norm"), "mnorm": g("ml_norm"), "mgb": np.ascontiguousarray(g("ml_gate_bias").reshape(DEPTH, 24)),
        "gpar": np.ascontiguousarray(np.concatenate([g("gdn_a_log").reshape(DEPTH, 12), g("gdn_dt_bias").reshape(DEPTH, 12)], axis=1)),
        "gconv": np.ascontiguousarray(g("gdn_conv").transpose(0, 2, 1).reshape(DEPTH, 18, 128, 5).transpose(0, 2, 1, 3)),
    }
    maps = []
    for b in cores:
        cc = np.stack([c[b], c_ctx], axis=-1)
        m = dict(shared)
        m["x"] = np.ascontiguousarray(x[b])
        m["ctx"] = np.ascontiguousarray(ctx[b])
        m["cT"] = np.ascontiguousarray(cc.reshape(KC, 128, 2).transpose(1, 0, 2))
        maps.append(m)
    return maps


_CACHE = {}


def kernel(**inputs):
    if "nc" not in _CACHE:
        _CACHE["nc"] = build()[0]
    nc = _CACHE["nc"]
    maps = make_in_maps(inputs)
    res = run_bass_kernel_spmd(nc, maps, core_ids=list(range(8)))
    return np.stack([np.asarray(r["out"]) for r in res.results], axis=0).astype(np.float32)
```

```python
import contextlib
import math
import numpy as np
import concourse.bass as bass
import concourse.mybir as mybir
from concourse.bass_utils import run_bass_kernel_spmd

F32 = mybir.dt.float32
BF16 = mybir.dt.bfloat16
AF = mybir.ActivationFunctionType
ALU = mybir.AluOpType
AX = mybir.AxisListType

SEM_EPOCH = 24000
class Buf:
    __slots__ = ("name", "ws", "r", "dsem", "dcnt", "multi", "ssem", "scnt")

    def __init__(self, name, multi=False):
        self.name = name
        self.ws = {}
        self.r = {}
        self.dsem = None
        self.dcnt = 0
        self.ssem = None
        self.scnt = 0
        self.multi = multi


class T:
    def __init__(self, t, name, psum=False):
        self.t = t
        self.b = Buf(name)
        self.psum = psum

    def __getitem__(self, k):
        return self.t[k]


class FW:
    def __init__(self, nc):
        self.nc = nc
        self.es = contextlib.ExitStack()
        self.eng = {"pe": nc.tensor, "act": nc.scalar, "dve": nc.vector, "pool": nc.gpsimd, "sp": nc.sync}
        self.sem, self.cnt, self.seen = {}, {}, {}
        for e in self.eng:
            self.sem[e] = self.es.enter_context(nc.semaphore("c_" + e))
            self.cnt[e] = 0
            self.seen[e] = {}
        self.pe_sems = {self.sem["pe"].num}
        self.nsem = 5
        self.ninstr = 0
        self.free_dsems = []
        self.free_ssems = []

    def uname(self, name):
        self.nname = getattr(self, "nname", 0) + 1
        return "t%d_%s" % (self.nname, name)

    def sb(self, name, shape, dt=F32):
        return T(self.es.enter_context(self.nc.sbuf_tensor(self.uname(name), list(shape), dt)), name)

    def ps(self, name, shape, dt=F32):
        return T(self.es.enter_context(self.nc.psum_tensor(self.uname(name), list(shape), dt)), name, psum=True)

    def scope(self):
        return Scope(self)

    @staticmethod
    def _bufs(lst):
        out = []
        for x in lst:
            if x is None:
                continue
            out.append(x.b if isinstance(x, T) else x)
        return out

    def _wait(self, e, evs):
        eng = self.eng[e]
        seen = self.seen[e]
        best = {}
        for (s, v) in evs:
            k = s.num
            if best.get(k, (None, 0))[1] < v:
                best[k] = (s, v)
        for k, (s, v) in best.items():
            if e == "pe" and k in self.pe_sems:
                continue
            if seen.get(k, 0) < v:
                eng.wait_ge(s, v)
                seen[k] = v

    @staticmethod
    def _deps(reads, writes):
        evs = []
        for b in reads:
            evs.extend(b.ws.values())
        for b in writes:
            if not b.multi:
                evs.extend(b.ws.values())
            evs.extend(b.r.values())
        return evs

    @staticmethod
    def _put(d, ev):
        k = ev[0].num
        if k not in d or d[k][1] < ev[1]:
            d[k] = ev

    def _record(self, ev, reads, writes):
        for b in reads:
            self._put(b.r, ev)
        for b in writes:
            if b.multi:
                self._put(b.ws, ev)
            else:
                b.ws = {ev[0].num: ev}
                b.r = {}

    def op(self, e, fn, reads=(), writes=()):
        pr = [x for x in reads if isinstance(x, T) and x.psum]
        if pr:
            reads = [x for x in reads if not (isinstance(x, T) and x.psum)]
            writes = list(writes) + pr
        reads = self._bufs(reads)
        writes = self._bufs(writes)
        self._wait(e, self._deps(reads, writes))
        ins = fn(self.eng[e])
        if self.cnt[e] >= SEM_EPOCH:
            self.sem[e] = self.es.enter_context(self.nc.semaphore("c%d_%s" % (self.nsem, e)))
            self.nsem += 1
            self.cnt[e] = 0
            if e == "pe":
                self.pe_sems.add(self.sem[e].num)
        self.cnt[e] += 1
        self.ninstr += 1
        ins.then_inc(self.sem[e], 1)
        ev = (self.sem[e], self.cnt[e])
        self._record(ev, reads, writes)
        return ev

    def dma(self, e, out, in_, reads=(), writes=(), key=None, **kw):
        reads = self._bufs(reads)
        writes = self._bufs(writes)
        kb = key.b if isinstance(key, T) else key
        sw = (e == "pool")
        sem, cnt = (kb.ssem, kb.scnt) if sw else (kb.dsem, kb.dcnt)
        evs = self._deps(reads, writes)
        if kb.dsem is not None and kb.dcnt > 0:
            evs.append((kb.dsem, kb.dcnt))
        if kb.ssem is not None and kb.scnt > 0:
            evs.append((kb.ssem, kb.scnt))
        self._wait(e, evs)
        if sem is None or cnt >= SEM_EPOCH:
            pool_ = self.free_ssems if sw else self.free_dsems
            if sem is None and pool_:
                sem, cnt = pool_.pop()
            else:
                sem = self.es.enter_context(self.nc.semaphore("%s%d" % ("s" if sw else "d", self.nsem)))
                self.nsem += 1
                cnt = 0
        ins = self.eng[e].dma_start(out=out, in_=in_, **kw)
        cnt += 16
        self.ninstr += 1
        ins.then_inc(sem, 16)
        if sw:
            kb.ssem, kb.scnt = sem, cnt
        else:
            kb.dsem, kb.dcnt = sem, cnt
        ev = (sem, cnt)
        self._record(ev, reads, writes)
        return ev

    def fence(self, tiles, engines=("pe", "act", "dve", "pool", "sp")):
        evs = []
        for b in self._bufs(tiles):
            evs.extend(b.ws.values())
            evs.extend(b.r.values())
        for e in engines:
            self._wait(e, evs)

    def release(self, tiles):
        for t in tiles:
            b = t.b if isinstance(t, T) else t
            if b.dsem is not None:
                if b.dcnt < SEM_EPOCH // 2:
                    self.free_dsems.append((b.dsem, b.dcnt))
                b.dsem = None
            if b.ssem is not None:
                if b.scnt < SEM_EPOCH // 2:
                    self.free_ssems.append((b.ssem, b.scnt))
                b.ssem = None

    def mm(self, out_t, out_ap, lhsT_t, lhsT_ap, rhs_t, rhs_ap, start=True, stop=True):
        return self.op("pe", lambda g: g.matmul(out_ap, lhsT=lhsT_ap, rhs=rhs_ap, start=start, stop=stop),
                       reads=[lhsT_t, rhs_t], writes=[out_t])

    def tr(self, out_t, out_ap, in_t, in_ap, ident):
        n = in_ap.shape[0]
        return self.op("pe", lambda g: g.transpose(out_ap, in_ap, ident.t[0:n, 0:n]),
                       reads=[in_t, ident], writes=[out_t])


class Scope:
    def __init__(self, fw):
        self.fw = fw
        self.es = contextlib.ExitStack()
        self.tiles = []

    def __enter__(self):
        return self

    def sb(self, name, shape, dt=F32):
        t = T(self.es.enter_context(self.fw.nc.sbuf_tensor(self.fw.uname(name), list(shape), dt)), name)
        self.tiles.append(t)
        return t

    def ps(self, name, shape, dt=F32):
        t = T(self.es.enter_context(self.fw.nc.psum_tensor(self.fw.uname(name), list(shape), dt)), name, psum=True)
        self.tiles.append(t)
        return t

    def __exit__(self, *a):
        self.fw.fence(self.tiles)
        self.fw.release(self.tiles)
        self.es.close()
        return False


def run_rr(gens):
    gens = list(gens)
    while gens:
        for g_ in list(gens):
            try:
                next(g_)
            except StopIteration:
                gens.remove(g_)


class RR:
    def __init__(self, items):
        self.items = list(items)
        self.i = 0

    def next(self):
        x = self.items[self.i % len(self.items)]
        self.i += 1
        return x

D = 2048
SEQ = 4096
CTXL = 256
NTOK = SEQ + CTXL
NTILE = NTOK // 128
KC = D // 128
DEPTH = 2
NH = 6
HD = 128
DG = 768
DHY = 512
EPS = 1e-6
FC = 3840
TC = 5168
T_GZ, T_GAB, T_HP, T_HZ, T_MV, T_MO, T_MZ, T_MG = 0, 768, 792, 2328, 2840, 3608, 4376, 5144
FCOLS = np.concatenate([np.arange(0, 2304), np.arange(5144, 6680)])
TCOLS = np.concatenate([np.arange(2304, 5144), np.arange(6680, 9008)])
Y_G, Y_H, Y_M = 0, 768, 1280


def build(dbg=None, layers=(0, 1), phases=("mod", "proj", "gdn", "hy", "ml", "out"), dbg_in=None, opts=None):
    dbg = dbg or {}
    dbg_in = dbg_in or {}
    opts = opts or {}
    nc = bass.Bass("TRN2", target_bir_lowering=False)
    fw = FW(nc)

    def din(name, shape, dt=F32):
        return nc.dram_tensor(name, list(shape), dt, kind="ExternalInput").ap()

    def dscr(name, shape, dt=F32):
        return nc.dram_tensor(name, list(shape), dt, kind="Internal").ap()

    big = ("proj" in phases) or ("out" in phases)
    x_in = din("x", [SEQ, D]) if big else None
    ctx_in = din("ctx", [CTXL, D]) if big else None
    cT_in = din("cT", [128, KC, 2])
    nwT_in = din("nwT", [128, DEPTH, KC])
    fnw_in = din("fnw", [1, D])
    modw_in = din("modw", [DEPTH, 128, KC, 3 * D]) if "mod" in phases else None
    modbT_in = din("modbT", [128, DEPTH, 32])
    modbg_in = din("modbg", [DEPTH, D])
    winF_in = din("winF", [DEPTH, 128, KC, FC]) if "proj" in phases else None
    winT_in = din("winT", [DEPTH, 128, KC, TC]) if "proj" in phases else None
    wout_in = din("wout", [DEPTH, 128, KC, D]) if "out" in phases else None
    ident_in = din("ident", [128, 128])
    cmask_in = din("cmask", [128, 6, 128])
    gpar_in = din("gpar", [DEPTH, 24])
    gconv_in = din("gconv", [DEPTH, 128, 18, 5])
    gnorm_in = din("gnorm", [DEPTH, 128])
    mnorm_in = din("mnorm", [DEPTH, 128])
    mgb_in = din("mgb", [DEPTH, 24])
    featL_in = din("featL", [33, SEQ])
    featC_in = din("featC", [33, CTXL])
    hyw1_in = din("hyw1", [DEPTH, 33, 64])
    hyw2_in = din("hyw2", [DEPTH, 64, 64])
    hyw3_in = din("hyw3", [DEPTH, 64, 2048])
    hyp_in = din("hyp", [DEPTH, 64, 3])
    hyskip_in = din("hyskip", [DEPTH, 1024])
    hyconv_in = din("hyconv", [DEPTH, 3, 1536])
    hyconvT_in = din("hyconvT", [DEPTH, 128, 12, 3])
    hyskipT_in = din("hyskipT", [DEPTH, 128, 2, 4])
    negt_in = din("negt", [64, 64])
    negtc_in = din("negtc", [1, CTXL])
    drow_in = din("drow", [1, 512])
    dcol_in = din("dcol", [128, 4])
    F1_in = din("F1", [64, 2, 128], BF16)
    F4_in = din("F4", [128, 2, 64], BF16)
    W2_in = din("W2", [64, 128, 4, 128], BF16)
    W3_in = din("W3", [128, 128, 128], BF16)
    identb_in = din("identb", [128, 128], BF16)
    identb = fw.sb("identb", [128, 128], BF16)
    fw.dma("sp", identb[:], identb_in[:, :], writes=[identb], key=identb)
    out_d = nc.dram_tensor("out", [SEQ, D], F32, kind="ExternalOutput").ap()

    xcur = dscr("xcur", [SEQ, D])
    ctxcur = dscr("ctxcur", [CTXL, D])
    UF = dscr("UF", [FC, NTOK])
    UT = dscr("UT", [NTOK, TC])
    Y = dscr("Y", [NTOK, D])
    b_xcur, b_ctxcur, b_UF, b_UT, b_Y, b_out = (Buf(n, multi=True) for n in ("xcur", "ctxcur", "UF", "UT", "Y", "out"))

    dbg_out = {}

    def dbg_tensor(name, shape):
        t = nc.dram_tensor("dbg_" + name, list(shape), F32, kind="ExternalOutput").ap()
        dbg_out[name] = t
        return t

    ident = fw.sb("ident", [128, 128])
    fw.dma("sp", ident[:], ident_in[:, :], writes=[ident], key=ident)
    cT = fw.sb("cT", [128, KC, 2])
    fw.dma("sp", cT[:], cT_in[:, :, :], writes=[cT], key=cT)
    scT = fw.sb("scT", [128, KC, 2])
    fw.op("act", lambda g: g.activation(out=scT[:], in_=cT[:], func=AF.Silu), reads=[cT], writes=[scT])
    nwT = fw.sb("nwT", [128, DEPTH, KC])
    fw.dma("sp", nwT[:], nwT_in[:, :, :], writes=[nwT], key=nwT)
    modbT = fw.sb("modbT", [128, DEPTH, 32])
    fw.dma("sp", modbT[:], modbT_in[:, :, :], writes=[modbT], key=modbT)
    modT = fw.sb("modT", [128, 32, 2])
    Amod = fw.sb("Amod", [128, 2, KC])
    Bmod = fw.sb("Bmod", [128, 2, KC])
    gtbc = [fw.sb("gtbc%d" % i, [128, D]) for i in range(2)]

    def phase_mod(l):
        with fw.scope() as sc:
            wb = [sc.sb("modw%d" % i, [128, KC, 512]) for i in range(2)]
            pss = [sc.ps("modps%d" % i, [128, 512]) for i in range(2)]
            gb = sc.sb("gbias", [128, D])
            rep = [sc.sb("rep%d" % i, [128, KC, 128]) for i in range(2)]
            for i in range(2):
                fw.op("dve", lambda g, i=i: g.tensor_copy(out=rep[i][:], in_=scT[:, :, i:i + 1].to_broadcast([128, KC, 128])),
                      reads=[scT], writes=[rep[i]])
            fw.dma("sp", gb[:], modbg_in[l:l + 1, :].partition_broadcast(128)[:, 0, :], writes=[gb], key=gb)
            for blk in range(12):
                w = wb[blk % 2]
                fw.dma("sp" if blk % 2 == 0 else "act", w[:], modw_in[l, :, :, blk * 512:(blk + 1) * 512],
                       writes=[w], key=w)
                if blk < 8:
                    p = pss[blk % 2]
                    for sub in range(4):
                        for kc in range(KC):
                            fw.mm(p, p[:, sub * 2:sub * 2 + 2], w, w[:, kc, sub * 128:(sub + 1) * 128],
                                  scT, scT[:, kc, :], start=(kc == 0), stop=(kc == KC - 1))
                    fw.op("dve", lambda g, p=p, blk=blk: g.tensor_tensor(
                        out=modT[:, blk * 4:(blk + 1) * 4, :],
                        in0=p[:, 0:8].rearrange("p (a b) -> p a b", b=2),
                        in1=modbT[:, l, blk * 4:(blk + 1) * 4].unsqueeze(2).to_broadcast([128, 4, 2]),
                        op=ALU.add), reads=[p, modbT], writes=[modT])
                else:
                    for i in range(2):
                        p = pss[i]
                        for kc in range(KC):
                            fw.mm(p, p[:, :], rep[i], rep[i][:, kc, :], w, w[:, kc, :],
                                  start=(kc == 0), stop=(kc == KC - 1))
                        cs = slice((blk - 8) * 512, (blk - 7) * 512)
                        fw.op("dve", lambda g, p=p, i=i, cs=cs: g.tensor_tensor(
                            out=gtbc[i][:, cs], in0=p[:, :], in1=gb[:, cs], op=ALU.add),
                            reads=[p, gb], writes=[gtbc[i]])
            for i in range(2):
                fw.op("dve", lambda g, i=i: g.scalar_tensor_tensor(
                    out=Amod[:, i, :], in0=modT[:, 16:32, i], scalar=1.0, in1=nwT[:, l, :],
                    op0=ALU.add, op1=ALU.mult), reads=[modT, nwT], writes=[Amod])
                fw.op("dve", lambda g, i=i: g.tensor_copy(out=Bmod[:, i, :], in_=modT[:, 0:16, i]),
                      reads=[modT], writes=[Bmod])

    def load_norm_transpose(sc, src_ap, src_buf, xt, ss, junk, tps, dstT, dst_cols, A_ap_fn, B_ap_fn, evq):
        fw.dma("sp", xt[:], src_ap, reads=[src_buf], writes=[xt], key=xt)
        if ss is not None:
            fw.op("act", lambda g: g.activation(out=junk[:], in_=xt[:], func=AF.Square, accum_out=ss[:, 0:1]),
                  reads=[xt], writes=[junk, ss])
            fw.op("dve", lambda g: g.tensor_scalar(out=ss[:, 1:2], in0=ss[:, 0:1], scalar1=1.0 / D, scalar2=EPS,
                                                    op0=ALU.mult, op1=ALU.add), reads=[ss], writes=[ss])
            fw.op("act", lambda g: g.sqrt(out=ss[:, 3:4], in_=ss[:, 1:2]), reads=[ss], writes=[ss])
            fw.op("dve", lambda g: g.reciprocal(out=ss[:, 2:3], in_=ss[:, 3:4]), reads=[ss], writes=[ss])
            fw.op("dve", lambda g: g.tensor_scalar(out=xt[:], in0=xt[:], scalar1=ss[:, 2:3], scalar2=None,
                                                    op0=ALU.mult), reads=[xt, ss], writes=[xt])
        for q4 in range(KC // 4):
            p = tps.next()
            for j in range(4):
                kc = q4 * 4 + j
                fw.tr(p, p[:, j * 128:(j + 1) * 128], xt, xt[:, kc * 128:(kc + 1) * 128], ident)
            for j in range(4):
                kc = q4 * 4 + j
                e = evq.next()
                if A_ap_fn is None:
                    if e == "act":
                        fw.op("act", lambda g, p=p, j=j, kc=kc: g.copy(out=dstT[:, kc, dst_cols], in_=p[:, j * 128:(j + 1) * 128]),
                              reads=[p], writes=[dstT])
                    else:
                        fw.op("dve", lambda g, p=p, j=j, kc=kc: g.tensor_copy(out=dstT[:, kc, dst_cols], in_=p[:, j * 128:(j + 1) * 128]),
                              reads=[p], writes=[dstT])
                else:
                    if e == "act":
                        fw.op("act", lambda g, p=p, j=j, kc=kc: g.activation(
                            out=dstT[:, kc, dst_cols], in_=p[:, j * 128:(j + 1) * 128], func=AF.Identity,
                            scale=A_ap_fn(kc), bias=B_ap_fn(kc)), reads=[p, Amod, Bmod], writes=[dstT])
                    else:
                        fw.op("dve", lambda g, p=p, j=j, kc=kc: g.tensor_scalar(
                            out=dstT[:, kc, dst_cols], in0=p[:, j * 128:(j + 1) * 128],
                            scalar1=A_ap_fn(kc), scalar2=B_ap_fn(kc), op0=ALU.mult, op1=ALU.add),
                            reads=[p, Amod, Bmod], writes=[dstT])

    def phase_proj(l):
        GT = 17
        xsrc, xbuf = (x_in, None) if l == 0 else (xcur, b_xcur)
        csrc, cbuf = (ctx_in, None) if l == 0 else (ctxcur, b_ctxcur)
        with fw.scope() as sc:
            xnT = sc.sb("xnT", [128, KC, GT * 128], BF16)
            xts = RR([sc.sb("xt%d" % i, [128, D]) for i in range(2)])
            junk = sc.sb("junk", [128, D])
            sss = RR([sc.sb("ss%d" % i, [128, 4]) for i in range(2)])
            tps = RR([sc.ps("tps%d" % i, [128, 512]) for i in range(2)])
            mps = RR([sc.ps("mps%d" % i, [128, 512]) for i in range(4)])
            wbs = RR([sc.sb("wb%d" % i, [128, KC, 512], BF16) for i in range(2)])
            sts = RR([sc.sb("st%d" % i, [128, 512]) for i in range(4)])
            evq = RR(["act", "dve"])
            for grp in range(2):
                for ti in range(GT):
                    gt_ = grp * GT + ti
                    if gt_ < 2:
                        src, sbuf, mi = csrc[gt_ * 128:(gt_ + 1) * 128, :], cbuf, 1
                    else:
                        src, sbuf, mi = xsrc[(gt_ - 2) * 128:(gt_ - 1) * 128, :], xbuf, 0
                    load_norm_transpose(sc, src, sbuf, xts.next(), sss.next(), junk, tps, xnT,
                                        slice(ti * 128, (ti + 1) * 128),
                                        lambda kc, mi=mi: Amod[:, mi, kc:kc + 1],
                                        lambda kc, mi=mi: Bmod[:, mi, kc:kc + 1], evq)
                tok0 = grp * GT * 128
                for c0 in range(0, TC, 512):
                    ncol = min(512, TC - c0)
                    w = wbs.next()
                    fw.dma("pool", w[:, :, 0:ncol], winT_in[l, :, :, c0:c0 + ncol], writes=[w], key=w)
                    for ti in range(GT):
                        p = mps.next()
                        for kc in range(KC):
                            fw.mm(p, p[:, 0:ncol], xnT, xnT[:, kc, ti * 128:(ti + 1) * 128], w, w[:, kc, 0:ncol],
                                  start=(kc == 0), stop=(kc == KC - 1))
                        st = sts.next()
                        e = evq.next()
                        if e == "act":
                            fw.op("act", lambda g, p=p, st=st: g.copy(out=st[:, 0:ncol], in_=p[:, 0:ncol]), reads=[p], writes=[st])
                        else:
                            fw.op("dve", lambda g, p=p, st=st: g.tensor_copy(out=st[:, 0:ncol], in_=p[:, 0:ncol]), reads=[p], writes=[st])
                        r0 = tok0 + ti * 128
                        fw.dma(e if e == 'act' else 'pool', UT[r0:r0 + 128, c0:c0 + ncol], st[:, 0:ncol], reads=[st], writes=[b_UT], key=st)
                ntk = GT * 128
                for c0 in range(0, FC, 512):
                    w = wbs.next()
                    ncf = min(512, FC - c0)
                    fw.dma("pool", w[:, :, 0:ncf], winF_in[l, :, :, c0:c0 + ncf], writes=[w], key=w)
                    for ct in range(4):
                        if c0 + ct * 128 >= FC:
                            break
                        for t0 in range(0, ntk, 512):
                            nt = min(512, ntk - t0)
                            p = mps.next()
                            for kc in range(KC):
                                fw.mm(p, p[:, 0:nt], w, w[:, kc, ct * 128:(ct + 1) * 128], xnT, xnT[:, kc, t0:t0 + nt],
                                      start=(kc == 0), stop=(kc == KC - 1))
                            st = sts.next()
                            e = evq.next()
                            if e == "act":
                                fw.op("act", lambda g, p=p, st=st, nt=nt: g.copy(out=st[:, 0:nt], in_=p[:, 0:nt]), reads=[p], writes=[st])
                            else:
                                fw.op("dve", lambda g, p=p, st=st, nt=nt: g.tensor_copy(out=st[:, 0:nt], in_=p[:, 0:nt]), reads=[p], writes=[st])
                            r0 = c0 + ct * 128
                            fw.dma(e if e == 'act' else 'pool', UF[r0:r0 + 128, tok0 + t0:tok0 + t0 + nt], st[:, 0:nt], reads=[st], writes=[b_UF], key=st)

    def phase_out(l):
        last = (l == DEPTH - 1)
        xsrc, xbuf = (x_in, None) if l == 0 else (xcur, b_xcur)
        with fw.scope() as sc:
            wo = sc.sb("wo", [128, KC, D], BF16)
            for h in range(4):
                fw.dma("pool", wo[:, h * 4:(h + 1) * 4, :], wout_in[l, :, h * 4:(h + 1) * 4, :], writes=[wo], key=wo)
            yts = RR([sc.sb("yt%d" % i, [128, D]) for i in range(2)])
            xts = RR([sc.sb("xr%d" % i, [128, D]) for i in range(2)])
            yT = RR([sc.sb("yT%d" % i, [128, KC, 128], BF16) for i in range(2)])
            tps = RR([sc.ps("tps%d" % i, [128, 512]) for i in range(2)])
            mps = RR([sc.ps("mps%d" % i, [128, 512]) for i in range(4)])
            evq = RR(["act", "dve"])
            ss = sc.sb("ss", [128, 4])
            junk = sc.sb("junk", [128, D])
            fnw = None
            if last:
                fnw = sc.sb("fnw", [128, D])
                fw.dma("sp", fnw[:], fnw_in[0:1, :].partition_broadcast(128)[:, 0, :], writes=[fnw], key=fnw)
            tiles = range(2, NTILE) if last else range(NTILE)
            for gt_ in tiles:
                isctx = gt_ < 2
                yt = yts.next()
                yTt = yT.next()
                load_norm_transpose(sc, Y[gt_ * 128:(gt_ + 1) * 128, :], b_Y, yt, None, None, tps, yTt,
                                    slice(0, 128), None, None, evq)
                xr = xts.next()
                if isctx:
                    src = (ctx_in if l == 0 else ctxcur)[gt_ * 128:(gt_ + 1) * 128, :]
                    sb_ = None if l == 0 else b_ctxcur
                else:
                    src = xsrc[(gt_ - 2) * 128:(gt_ - 1) * 128, :]
                    sb_ = xbuf
                fw.dma("act", xr[:], src, reads=[sb_], writes=[xr], key=xr)
                g_ = gtbc[1 if isctx else 0]
                for fb in range(4):
                    p = mps.next()
                    fs = slice(fb * 512, (fb + 1) * 512)
                    for kc in range(KC):
                        fw.mm(p, p[:, :], yTt, yTt[:, kc, :], wo, wo[:, kc, fs], start=(kc == 0), stop=(kc == KC - 1))
                    fw.op("dve", lambda g, p=p, fs=fs, g_=g_, yt=yt: g.tensor_tensor(out=yt[:, fs], in0=p[:, :], in1=g_[:, fs], op=ALU.mult),
                          reads=[p, g_], writes=[yt])
                    fw.op("pool", lambda g, fs=fs, yt=yt, xr=xr: g.tensor_tensor(out=xr[:, fs], in0=xr[:, fs], in1=yt[:, fs], op=ALU.add),
                          reads=[yt, xr], writes=[xr])
                if not last:
                    if isctx:
                        fw.dma("pool", ctxcur[gt_ * 128:(gt_ + 1) * 128, :], xr[:], reads=[xr], writes=[b_ctxcur], key=xr)
                    else:
                        fw.dma("pool", xcur[(gt_ - 2) * 128:(gt_ - 1) * 128, :], xr[:], reads=[xr], writes=[b_xcur], key=xr)
                else:
                    fw.op("act", lambda g, xr=xr: g.activation(out=junk[:], in_=xr[:], func=AF.Square, accum_out=ss[:, 0:1]),
                          reads=[xr], writes=[junk, ss])
                    fw.op("dve", lambda g: g.tensor_scalar(out=ss[:, 1:2], in0=ss[:, 0:1], scalar1=1.0 / D, scalar2=EPS,
                                                            op0=ALU.mult, op1=ALU.add), reads=[ss], writes=[ss])
                    fw.op("act", lambda g: g.sqrt(out=ss[:, 3:4], in_=ss[:, 1:2]), reads=[ss], writes=[ss])
                    fw.op("dve", lambda g: g.reciprocal(out=ss[:, 2:3], in_=ss[:, 3:4]), reads=[ss], writes=[ss])
                    fw.op("dve", lambda g, xr=xr: g.scalar_tensor_tensor(out=xr[:], in0=xr[:], scalar=ss[:, 2:3], in1=fnw[:],
                                                                         op0=ALU.mult, op1=ALU.mult), reads=[xr, ss, fnw], writes=[xr])
                    fw.dma("pool", out_d[(gt_ - 2) * 128:(gt_ - 1) * 128, :], xr[:], reads=[xr], writes=[b_out], key=xr)


    cmask = fw.sb("cmask", [128, 6, 128])
    fw.dma("sp", cmask[:], cmask_in[:, :, :], writes=[cmask], key=cmask)
    TRI = [cmask[:, 0, :], cmask[:, 1, :]]
    LS = [cmask[:, 2, :], cmask[:, 3, :]]
    LIT = [cmask[:, 4, :], cmask[:, 5, :]]
    trione = [fw.sb("trione%d" % d, [128, 129]) for d in range(2)]
    for d in range(2):
        fw.op("dve", lambda g, d=d: g.tensor_copy(out=trione[d][:, 0:128], in_=TRI[d]), reads=[cmask], writes=[trione[d]])
        fw.op("dve", lambda g, d=d: g.memset(trione[d][:, 128:129], 1.0), writes=[trione[d]])
    ones = fw.sb("ones", [128, 128])
    fw.op("dve", lambda g: g.memset(ones[:], 1.0), writes=[ones])
    epsc = fw.sb("epsc", [128, 1])
    fw.op("dve", lambda g: g.memset(epsc[:], EPS), writes=[epsc])
    NCH = NTILE

    def chunk_order(d):
        return list(range(NCH)) if d == 0 else [1, 0] + list(range(NCH - 1, 1, -1))

    OFs = [dscr("OF", [NTOK, DG]), dscr("OB", [NTOK, DG])]
    b_OF = [Buf("OF", multi=True), Buf("OB", multi=True)]

    def decay_prep(ws, gcol_ap, gsrc, d, need_D):
        fw.op("dve", lambda g: g.tensor_scalar(out=ws["gtri"][:], in0=TRI[d], scalar1=gcol_ap, scalar2=None, op0=ALU.mult),
              reads=[cmask, gsrc], writes=[ws["gtri"]])
        fw.op("dve", lambda g: g.tensor_copy(out=ws["grep"][:], in_=gcol_ap.to_broadcast([128, 128])),
              reads=[gsrc], writes=[ws["grep"]])
        pDT = ws["ps"].next()
        fw.mm(pDT, pDT[:, 0:128], cmask, LS[d], ws["gtri"], ws["gtri"][:])
        fw.op("act", lambda g: g.activation(out=ws["eDT"][:], in_=pDT[:, 0:128], func=AF.Exp), reads=[pDT], writes=[ws["eDT"]])
        if need_D:
            pD = ws["ps"].next()
            fw.mm(pD, pD[:, 0:128], ws["gtri"], ws["gtri"][:], cmask, LS[d])
            fw.op("act", lambda g: g.activation(out=ws["eD"][:], in_=pD[:, 0:128], func=AF.Exp), reads=[pD], writes=[ws["eD"]])
        pb = ws["ps"].next()
        fw.mm(pb, pb[:, 0:129], ws["grep"], ws["grep"][:], trione[d], trione[d][:])
        fw.op("act", lambda g: g.activation(out=ws["EG"][:], in_=pb[:, 0:128], func=AF.Exp), reads=[pb], writes=[ws["EG"]])
        fw.op("act", lambda g: g.activation(out=ws["sm"][:, 0:1], in_=pb[:, 128:129], func=AF.Exp), reads=[pb], writes=[ws["sm"]])
        fw.op("dve", lambda g: g.tensor_copy(out=ws["sm"][:, 1:2], in_=pb[:, 128:129]), reads=[pb], writes=[ws["sm"]])
        pc = ws["ps"].next()
        fw.mm(pc, pc[:, 0:1], cmask, TRI[d], gsrc, gcol_ap)
        fw.op("act", lambda g: g.activation(out=ws["sm"][:, 2:3], in_=pc[:, 0:1], func=AF.Exp), reads=[pc], writes=[ws["sm"]])
        fw.op("act", lambda g: g.activation(out=ws["sm"][:, 3:4], in_=pc[:, 0:1], func=AF.Exp, scale=-1.0, bias=ws["sm"][:, 1:2]),
              reads=[pc, ws["sm"]], writes=[ws["sm"]])

    def make_ws(sc, tag, names):
        ws = {}
        for n in names:
            ws[n] = sc.sb(tag + n, [128, 128])
        ws["sm"] = sc.sb(tag + "sm", [128, 8])
        return ws

    def phase_gdn(l, heads=range(NH)):
        last = (l == DEPTH - 1)
        with fw.scope() as sc:
            AB = sc.sb("AB", [128, NCH, 24])
            fw.dma("sp", AB[:], UT[:, T_GAB:T_GAB + 24].rearrange("(n p) c -> p n c", p=128), reads=[b_UT], writes=[AB], key=AB)
            gpar = sc.sb("gpar", [128, 24])
            fw.dma("sp", gpar[:], gpar_in[l:l + 1, :].partition_broadcast(128)[:, 0, :], writes=[gpar], key=gpar)
            G = sc.sb("G", [128, NCH, 12])
            NB = sc.sb("NB", [128, NCH, 12])
            BETA = sc.sb("BETA", [128, NCH, 12])
            fw.op("dve", lambda g: g.tensor_tensor(out=G[:], in0=AB[:, :, 0:12], in1=gpar[:, 12:24].unsqueeze(1).to_broadcast([128, NCH, 12]), op=ALU.add),
                  reads=[AB, gpar], writes=[G])
            fw.op("act", lambda g: g.activation(out=G[:], in_=G[:], func=AF.Exp), reads=[G], writes=[G])
            fw.op("act", lambda g: g.activation(out=G[:], in_=G[:], func=AF.Ln, bias=ones[:, 0:1]), reads=[G, ones], writes=[G])
            fw.op("act", lambda g: g.activation(out=gpar[:, 0:12], in_=gpar[:, 0:12], func=AF.Exp), reads=[gpar], writes=[gpar])
            fw.op("dve", lambda g: g.scalar_tensor_tensor(out=G[:], in0=G[:], scalar=-1.0, in1=gpar[:, 0:12].unsqueeze(1).to_broadcast([128, NCH, 12]),
                                                         op0=ALU.mult, op1=ALU.mult), reads=[G, gpar], writes=[G])
            fw.op("act", lambda g: g.activation(out=BETA[:], in_=AB[:, :, 12:24], func=AF.Sigmoid), reads=[AB], writes=[BETA])
            fw.op("dve", lambda g: g.tensor_scalar(out=NB[:], in0=BETA[:], scalar1=-1.0, scalar2=None, op0=ALU.mult), reads=[BETA], writes=[NB])
            cw = sc.sb("cw", [128, 18, 5])
            fw.dma("sp", cw[:], gconv_in[l, :, :, :], writes=[cw], key=cw)

            qkv_raw = [sc.sb("raw%d" % i, [128, NTOK]) for i in range(3)]
            qkv = [sc.sb("qkv%d" % i, [128, NTOK]) for i in range(3)]
            psl = [sc.ps("gps%d" % i, [128, 512]) for i in range(8)]
            PS = RR(psl)
            S = [sc.sb("S%d" % d, [128, 128]) for d in range(2)]
            names = ["gtri", "grep", "eDT", "eD", "EG", "Ktok", "Vtok", "kkm", "qkTm", "P", "PT", "P2", "P2T", "TT",
                     "AqkT", "QsT", "Ke", "bV", "R", "U", "O"]
            WS = []
            for i in range(4):
                w_ = make_ws(sc, "w%d" % i, names)
                w_["ps"] = PS
                WS.append(w_)
            cengs = RR(["dve"])
            for h in heads:
                for i in range(3):
                    r0 = i * DG + h * 128
                    fw.dma("sp" if i != 1 else "act", qkv_raw[i][:], UF[r0:r0 + 128, :], reads=[b_UF], writes=[qkv_raw[i]], key=qkv_raw[i])
                for i in range(3):
                    e = cengs.next()
                    src, dst = qkv_raw[i], qkv[i]
                    wcol = lambda j, i=i: cw[:, i * 6 + h, j:j + 1]
                    fw.op(e, lambda g, src=src, dst=dst: g.tensor_scalar(out=dst[:], in0=src[:], scalar1=wcol(2), scalar2=None, op0=ALU.mult),
                          reads=[src, cw], writes=[dst])
                    for j in (0, 1, 3, 4):
                        off = j - 2
                        a, b = max(0, -off), CTXL - max(0, off)
                        fw.op(e, lambda g, src=src, dst=dst, a=a, b=b, off=off, j=j: g.scalar_tensor_tensor(
                            out=dst[:, a:b], in0=src[:, a + off:b + off], scalar=wcol(j), in1=dst[:, a:b], op0=ALU.mult, op1=ALU.add),
                            reads=[src, cw, dst], writes=[dst])
                        a, b = max(0, -off), 64 - max(0, off)
                        sv = src[:, CTXL:].rearrange("p (r c) -> p r c", c=64)
                        dv = dst[:, CTXL:].rearrange("p (r c) -> p r c", c=64)
                        fw.op(e, lambda g, sv=sv, dv=dv, a=a, b=b, off=off, j=j, src=src, dst=dst: g.scalar_tensor_tensor(
                            out=dv[:, :, a:b], in0=sv[:, :, a + off:b + off], scalar=wcol(j), in1=dv[:, :, a:b], op0=ALU.mult, op1=ALU.add),
                            reads=[src, cw, dst], writes=[dst])
                    fw.op("act", lambda g, dst=dst: g.activation(out=dst[:], in_=dst[:], func=AF.Silu), reads=[dst], writes=[dst])
                for i in range(2):
                    x_, sq = qkv[i], qkv_raw[i]
                    fw.op("act", lambda g, x_=x_, sq=sq: g.activation(out=sq[:], in_=x_[:], func=AF.Square), reads=[x_], writes=[sq])
                    for t0 in range(0, NTOK, 512):
                        nt = min(512, NTOK - t0)
                        p = PS.next()
                        fw.mm(p, p[:, 0:nt], ones, ones[:], sq, sq[:, t0:t0 + nt])
                        fw.op("act", lambda g, p=p, sq=sq, t0=t0, nt=nt: g.activation(out=sq[:, t0:t0 + nt], in_=p[:, 0:nt], func=AF.Sqrt, bias=epsc[:, 0:1]),
                              reads=[p, epsc, sq], writes=[sq])
                    fw.op("dve", lambda g, sq=sq: g.reciprocal(out=sq[:], in_=sq[:]), reads=[sq], writes=[sq])
                    scl = HD ** -0.5 if i == 0 else 1.0
                    fw.op("dve", lambda g, x_=x_, sq=sq, scl=scl: g.scalar_tensor_tensor(out=x_[:], in0=x_[:], scalar=scl, in1=sq[:], op0=ALU.mult, op1=ALU.mult),
                          reads=[x_, sq], writes=[x_])
                qT, kT, vT = qkv
                for d in range(2):
                    fw.op("dve", lambda g, d=d: g.memset(S[d][:], 0.0), writes=[S[d]])
                orders = [chunk_order(0), chunk_order(1)]

                def unit_vars(step, d):
                    n = orders[d][step]
                    return n, WS[(step % 2) * 2 + d], d * 6 + h, slice(n * 128, (n + 1) * 128)

                def g_prep(step, d):
                    n, ws, u, cs = unit_vars(step, d)
                    gcol = G[:, n, u:u + 1]
                    u = d * 6 + h
                    cs = slice(n * 128, (n + 1) * 128)
                    gcol = G[:, n, u:u + 1]
                    decay_prep(ws, gcol, G, d, True)
                    yield
                    p = PS.next()
                    fw.tr(p, p[:, 0:128], kT, kT[:, cs], ident)
                    fw.tr(p, p[:, 128:256], vT, vT[:, cs], ident)
                    fw.op("act", lambda g, p=p, ws=ws: g.activation(out=ws["Ke"][:], in_=p[:, 0:128], func=AF.Copy, scale=ws["sm"][:, 3:4]),
                          reads=[p, ws["sm"]], writes=[ws["Ke"]])
                    fw.op("dve", lambda g, p=p, ws=ws, n=n, u=u: g.tensor_scalar(out=ws["bV"][:], in0=p[:, 128:256], scalar1=BETA[:, n, u:u + 1], scalar2=None, op0=ALU.mult),
                          reads=[p, BETA], writes=[ws["bV"]])
                    fw.op("dve", lambda g, ws=ws, n=n, u=u: g.tensor_tensor(out=ws["sm"][:, 4:5], in0=ws["sm"][:, 2:3], in1=NB[:, n, u:u + 1], op=ALU.mult),
                          reads=[ws["sm"], NB], writes=[ws["sm"]])
                    yield
                    p = PS.next()
                    fw.mm(p, p[:, 0:128], kT, kT[:, cs], kT, kT[:, cs])
                    fw.mm(p, p[:, 128:256], kT, kT[:, cs], qT, qT[:, cs])
                    fw.op("dve", lambda g, p=p, ws=ws, d=d: g.tensor_tensor(out=ws["kkm"][:], in0=p[:, 0:128], in1=LS[d], op=ALU.mult),
                          reads=[p, cmask], writes=[ws["kkm"]])
                    fw.op("dve", lambda g, p=p, ws=ws, d=d: g.tensor_tensor(out=ws["qkTm"][:], in0=p[:, 128:256], in1=LIT[d], op=ALU.mult),
                          reads=[p, cmask], writes=[ws["qkTm"]])
                    yield
                    fw.op("dve", lambda g, ws=ws, n=n, u=u: g.scalar_tensor_tensor(out=ws["P"][:], in0=ws["eD"][:], scalar=NB[:, n, u:u + 1], in1=ws["kkm"][:],
                                                                                 op0=ALU.mult, op1=ALU.mult), reads=[ws["eD"], NB, ws["kkm"]], writes=[ws["P"]])
                    fw.op("dve", lambda g, ws=ws: g.tensor_tensor(out=ws["AqkT"][:], in0=ws["eDT"][:], in1=ws["qkTm"][:], op=ALU.mult),
                          reads=[ws["eDT"], ws["qkTm"]], writes=[ws["AqkT"]])
                    fw.op("dve", lambda g, ws=ws, cs=cs: g.tensor_tensor(out=ws["QsT"][:], in0=qT[:, cs], in1=ws["EG"][:], op=ALU.mult),
                          reads=[qT, ws["EG"]], writes=[ws["QsT"]])
                    yield
                    p = PS.next()
                    fw.tr(p, p[:, 0:128], ws["P"], ws["P"][:], ident)
                    fw.op("act", lambda g, p=p, ws=ws: g.copy(out=ws["PT"][:], in_=p[:, 0:128]), reads=[p], writes=[ws["PT"]])
                    fw.op("dve", lambda g, p=p, ws=ws: g.tensor_tensor(out=ws["TT"][:], in0=p[:, 0:128], in1=ident[:], op=ALU.add),
                          reads=[p, ident], writes=[ws["TT"]])
                    Pc, PTc, Pn, PTn = "P", "PT", "P2", "P2T"
                    yield
                    for lev in range(1, 7):
                        p = PS.next()
                        fw.mm(p, p[:, 0:128], ws[PTc], ws[PTc][:], ws[Pc], ws[Pc][:])
                        if lev < 6:
                            fw.mm(p, p[:, 128:256], ws[Pc], ws[Pc][:], ws[PTc], ws[PTc][:])
                        fw.op("act", lambda g, p=p, ws=ws, Pn=Pn: g.copy(out=ws[Pn][:], in_=p[:, 0:128]), reads=[p], writes=[ws[Pn]])
                        if lev < 6:
                            fw.op("dve", lambda g, p=p, ws=ws, PTn=PTn: g.tensor_copy(out=ws[PTn][:], in_=p[:, 128:256]), reads=[p], writes=[ws[PTn]])
                        yield
                        p2 = PS.next()
                        fw.mm(p2, p2[:, 0:128], ws[Pn], ws[Pn][:], ws["TT"], ws["TT"][:])
                        fw.op("dve", lambda g, p2=p2, ws=ws: g.tensor_tensor(out=ws["TT"][:], in0=p2[:, 0:128], in1=ws["TT"][:], op=ALU.add),
                              reads=[p2, ws["TT"]], writes=[ws["TT"]])
                        Pc, PTc, Pn, PTn = Pn, PTn, Pc, PTc

                def g_seq(step, d):
                    n, ws, u, cs = unit_vars(step, d)
                    p = PS.next()
                    fw.mm(p, p[:, 0:128], kT, kT[:, cs], S[d], S[d][:])
                    fw.op("dve", lambda g, p=p, ws=ws: g.scalar_tensor_tensor(out=ws["R"][:], in0=p[:, 0:128], scalar=ws["sm"][:, 4:5], in1=ws["bV"][:],
                                                                           op0=ALU.mult, op1=ALU.add), reads=[p, ws["sm"], ws["bV"]], writes=[ws["R"]])
                    yield
                    fw.mm(p, p[:, 128:256], ws["TT"], ws["TT"][:], ws["R"], ws["R"][:])
                    fw.op("act", lambda g, p=p, ws=ws: g.copy(out=ws["U"][:], in_=p[:, 128:256]), reads=[p], writes=[ws["U"]])
                    yield
                    fw.mm(p, p[:, 256:384], ws["QsT"], ws["QsT"][:], S[d], S[d][:], start=True, stop=False)
                    fw.mm(p, p[:, 256:384], ws["AqkT"], ws["AqkT"][:], ws["U"], ws["U"][:], start=False, stop=True)
                    fw.mm(p, p[:, 384:512], ws["Ke"], ws["Ke"][:], ws["U"], ws["U"][:])
                    fw.op("dve", lambda g, p=p, ws=ws, d=d: g.scalar_tensor_tensor(out=S[d][:], in0=S[d][:], scalar=ws["sm"][:, 0:1], in1=p[:, 384:512],
                                                                                op0=ALU.mult, op1=ALU.add), reads=[p, ws["sm"], S[d]], writes=[S[d]])
                    if not (last and n < 2):
                        fw.op("act", lambda g, p=p, ws=ws: g.copy(out=ws["O"][:], in_=p[:, 256:384]), reads=[p], writes=[ws["O"]])
                        fw.dma("act", OFs[d][n * 128:(n + 1) * 128, h * 128:(h + 1) * 128], ws["O"][:], reads=[ws["O"]], writes=[b_OF[d]], key=ws["O"])


                    yield

                run_rr([g_prep(0, 0), g_prep(0, 1)])
                for step in range(NCH):
                    gens = [g_seq(step, 0), g_seq(step, 1)]
                    if step + 1 < NCH:
                        gens += [g_prep(step + 1, 0), g_prep(step + 1, 1)]
                    run_rr(gens)

    def finalize(l, kind):
        last = (l == DEPTH - 1)
        zcol = T_GZ if kind == "gdn" else T_MZ
        ycol = Y_G if kind == "gdn" else Y_M
        nw_in = gnorm_in if kind == "gdn" else mnorm_in
        with fw.scope() as sc:
            nwb = sc.sb("nwb", [128, 128])
            fw.dma("sp", nwb[:], nw_in[l:l + 1, :].partition_broadcast(128)[:, 0, :], writes=[nwb], key=nwb)
            A = RR([sc.sb("fa%d" % i, [128, DG]) for i in range(2)])
            B = RR([sc.sb("fb%d" % i, [128, DG]) for i in range(2)])
            Z = RR([sc.sb("fz%d" % i, [128, DG]) for i in range(2)])
            OGt = RR([sc.sb("fo%d" % i, [128, DG]) for i in range(2)])
            SQ = sc.sb("fsq", [128, DG])
            ssm = RR([sc.sb("fss%d" % i, [128, 12]) for i in range(2)])
            for n in (range(2, NCH) if last else range(NCH)):
                a, b_, z, ss = A.next(), B.next(), Z.next(), ssm.next()
                rs = slice(n * 128, (n + 1) * 128)
                fw.dma("sp", a[:], OFs[0][rs, :], reads=[b_OF[0]], writes=[a], key=a)
                fw.dma("sp", b_[:], OFs[1][rs, :], reads=[b_OF[1]], writes=[b_], key=b_)
                fw.dma("sp", z[:], UT[rs, zcol:zcol + DG], reads=[b_UT], writes=[z], key=z)
                fw.op("pool", lambda g, a=a, b_=b_: g.tensor_tensor(out=a[:], in0=a[:], in1=b_[:], op=ALU.add), reads=[a, b_], writes=[a])
                if kind == "ml":
                    og = OGt.next()
                    fw.dma("sp", og[:], UT[rs, T_MO:T_MO + DG], reads=[b_UT], writes=[og], key=og)
                    fw.op("act", lambda g, og=og: g.activation(out=og[:], in_=og[:], func=AF.Sigmoid), reads=[og], writes=[og])
                    fw.op("pool", lambda g, a=a, og=og: g.tensor_tensor(out=a[:], in0=a[:], in1=og[:], op=ALU.mult), reads=[a, og], writes=[a])
                fw.op("act", lambda g, a=a: g.activation(out=SQ[:], in_=a[:], func=AF.Square), reads=[a], writes=[SQ])
                fw.op("dve", lambda g, ss=ss: g.tensor_reduce(out=ss[:, 0:6], in_=SQ[:].rearrange("p (h c) -> p h c", c=128), axis=AX.X, op=ALU.add),
                      reads=[SQ], writes=[ss])
                fw.op("dve", lambda g, ss=ss: g.tensor_scalar(out=ss[:, 0:6], in0=ss[:, 0:6], scalar1=1.0 / HD, scalar2=EPS, op0=ALU.mult, op1=ALU.add),
                      reads=[ss], writes=[ss])
                fw.op("act", lambda g, ss=ss: g.sqrt(out=ss[:, 0:6], in_=ss[:, 0:6]), reads=[ss], writes=[ss])
                fw.op("dve", lambda g, ss=ss: g.reciprocal(out=ss[:, 6:12], in_=ss[:, 0:6]), reads=[ss], writes=[ss])
                a3 = a[:].rearrange("p (h c) -> p h c", c=128)
                fw.op("dve", lambda g, a3=a3, ss=ss, a=a: g.tensor_tensor(out=a3, in0=a3, in1=ss[:, 6:12].unsqueeze(2).to_broadcast([128, 6, 128]), op=ALU.mult),
                      reads=[a, ss], writes=[a])
                fw.op("pool", lambda g, a3=a3, a=a: g.tensor_tensor(out=a3, in0=a3, in1=nwb[:].unsqueeze(1).to_broadcast([128, 6, 128]), op=ALU.mult),
                      reads=[a, nwb], writes=[a])
                fw.op("act", lambda g, z=z: g.activation(out=z[:], in_=z[:], func=AF.Silu), reads=[z], writes=[z])
                fw.op("dve", lambda g, a=a, z=z: g.tensor_tensor(out=a[:], in0=a[:], in1=z[:], op=ALU.mult), reads=[a, z], writes=[a])
                fw.dma("act", Y[rs, ycol:ycol + DG], a[:], reads=[a], writes=[b_Y], key=a)

    def phase_ml(l, heads=range(NH)):
        last = (l == DEPTH - 1)
        with fw.scope() as sc:
            MG = sc.sb("MG", [128, NCH, 24])
            fw.dma("sp", MG[:], UT[:, T_MG:T_MG + 24].rearrange("(n p) c -> p n c", p=128), reads=[b_UT], writes=[MG], key=MG)
            mgb = sc.sb("mgb", [128, 24])
            fw.dma("sp", mgb[:], mgb_in[l:l + 1, :].partition_broadcast(128)[:, 0, :], writes=[mgb], key=mgb)
            fw.op("dve", lambda g: g.tensor_tensor(out=MG[:], in0=MG[:], in1=mgb[:].unsqueeze(1).to_broadcast([128, NCH, 24]), op=ALU.add),
                  reads=[MG, mgb], writes=[MG])
            ELI = sc.sb("ELI", [128, NCH, 12])
            LF = sc.sb("LF", [128, NCH, 12])
            for d in range(2):
                fw.op("act", lambda g, d=d: g.activation(out=ELI[:, :, d * 6:(d + 1) * 6], in_=MG[:, :, d * 12:d * 12 + 6], func=AF.Exp),
                      reads=[MG], writes=[ELI])
                fw.op("act", lambda g, d=d: g.activation(out=LF[:, :, d * 6:(d + 1) * 6], in_=MG[:, :, d * 12 + 6:d * 12 + 12], func=AF.Exp, scale=-1.0),
                      reads=[MG], writes=[LF])
            fw.op("act", lambda g: g.activation(out=LF[:], in_=LF[:], func=AF.Ln, bias=ones[:, 0:1]), reads=[LF, ones], writes=[LF])
            fw.op("dve", lambda g: g.tensor_scalar(out=LF[:], in0=LF[:], scalar1=-1.0, scalar2=None, op0=ALU.mult), reads=[LF], writes=[LF])
            fw.op("dve", lambda g: g.tensor_scalar(out=ELI[:], in0=ELI[:], scalar1=HD ** -0.5, scalar2=None, op0=ALU.mult), reads=[ELI], writes=[ELI])
            qT = sc.sb("mq", [128, NTOK])
            kT = sc.sb("mk", [128, NTOK])
            Vt = sc.sb("mv", [128, NCH, 129])
            fw.op("dve", lambda g: g.memset(Vt[:, :, 128:129], 1.0), writes=[Vt])
            PS = RR([sc.ps("mps%d" % i, [128, 512]) for i in range(8)])
            Cst = [sc.sb("C%d" % d, [128, 129]) for d in range(2)]
            names = ["gtri", "grep", "eDT", "EG", "Ke", "kqTm", "PT", "QsT", "H"]
            WS = []
            for i in range(4):
                w_ = make_ws(sc, "m%d" % i, names)
                w_["ps"] = PS
                WS.append(w_)
            wsi = 0
            for h in heads:
                fw.dma("sp", qT[:], UF[2304 + h * 128:2304 + (h + 1) * 128, :], reads=[b_UF], writes=[qT], key=qT)
                fw.dma("act", kT[:], UF[3072 + h * 128:3072 + (h + 1) * 128, :], reads=[b_UF], writes=[kT], key=kT)
                fw.dma("sp", Vt[:, :, 0:128], UT[:, T_MV + h * 128:T_MV + (h + 1) * 128].rearrange("(n p) c -> p n c", p=128),
                       reads=[b_UT], writes=[Vt], key=Vt)
                for d in range(2):
                    fw.op("dve", lambda g, d=d: g.memset(Cst[d][:], 0.0), writes=[Cst[d]])
                orders = [chunk_order(0), chunk_order(1)]

                def unit_vars(step, d):
                    n = orders[d][step]
                    return n, WS[(step % 2) * 2 + d], d * 6 + h, slice(n * 128, (n + 1) * 128)

                def m_prep(step, d):
                    n, ws, u, cs = unit_vars(step, d)
                    decay_prep(ws, LF[:, n, u:u + 1], LF, d, False)
                    fw.op("dve", lambda g, ws=ws, n=n, u=u: g.tensor_tensor(out=ws["sm"][:, 4:5], in0=ws["sm"][:, 3:4], in1=ELI[:, n, u:u + 1], op=ALU.mult),
                          reads=[ws["sm"], ELI], writes=[ws["sm"]])
                    yield
                    p = PS.next()
                    fw.tr(p, p[:, 0:128], kT, kT[:, cs], ident)
                    fw.mm(p, p[:, 128:256], kT, kT[:, cs], qT, qT[:, cs])
                    fw.op("act", lambda g, p=p, ws=ws: g.activation(out=ws["Ke"][:], in_=p[:, 0:128], func=AF.Copy, scale=ws["sm"][:, 4:5]),
                          reads=[p, ws["sm"]], writes=[ws["Ke"]])
                    yield
                    fw.op("dve", lambda g, p=p, ws=ws, d=d: g.tensor_tensor(out=ws["kqTm"][:], in0=p[:, 128:256], in1=LIT[d], op=ALU.mult),
                          reads=[p, cmask], writes=[ws["kqTm"]])
                    fw.op("dve", lambda g, ws=ws, n=n, u=u: g.scalar_tensor_tensor(out=ws["PT"][:], in0=ws["eDT"][:], scalar=ELI[:, n, u:u + 1], in1=ws["kqTm"][:],
                                                                                 op0=ALU.mult, op1=ALU.mult), reads=[ws["eDT"], ELI, ws["kqTm"]], writes=[ws["PT"]])
                    fw.op("dve", lambda g, ws=ws, cs=cs: g.tensor_tensor(out=ws["QsT"][:], in0=qT[:, cs], in1=ws["EG"][:], op=ALU.mult),
                          reads=[qT, ws["EG"]], writes=[ws["QsT"]])
                    yield

                def m_seq(step, d):
                    n, ws, u, cs = unit_vars(step, d)
                    p = PS.next()
                    fw.mm(p, p[:, 0:129], ws["QsT"], ws["QsT"][:], Cst[d], Cst[d][:], start=True, stop=False)
                    fw.mm(p, p[:, 0:129], ws["PT"], ws["PT"][:], Vt, Vt[:, n, :], start=False, stop=True)
                    yield
                    fw.mm(p, p[:, 256:385], ws["Ke"], ws["Ke"][:], Vt, Vt[:, n, :])
                    fw.op("dve", lambda g, p=p, ws=ws, d=d: g.scalar_tensor_tensor(out=Cst[d][:], in0=Cst[d][:], scalar=ws["sm"][:, 0:1], in1=p[:, 256:385],
                                                                                op0=ALU.mult, op1=ALU.add), reads=[p, ws["sm"], Cst[d]], writes=[Cst[d]])
                    if not (last and n < 2):
                        fw.op("act", lambda g, p=p, ws=ws: g.activation(out=ws["sm"][:, 7:8], in_=p[:, 128:129], func=AF.Abs), reads=[p], writes=[ws["sm"]])
                        fw.op("dve", lambda g, ws=ws: g.tensor_scalar(out=ws["sm"][:, 5:6], in0=ws["sm"][:, 7:8], scalar1=1.0, scalar2=None, op0=ALU.max),
                              reads=[ws["sm"]], writes=[ws["sm"]])
                        fw.op("dve", lambda g, ws=ws: g.reciprocal(out=ws["sm"][:, 6:7], in_=ws["sm"][:, 5:6]), reads=[ws["sm"]], writes=[ws["sm"]])
                        fw.op("act", lambda g, p=p, ws=ws: g.activation(out=ws["H"][:], in_=p[:, 0:128], func=AF.Copy, scale=ws["sm"][:, 6:7]),
                              reads=[p, ws["sm"]], writes=[ws["H"]])
                        fw.dma("act", OFs[d][n * 128:(n + 1) * 128, h * 128:(h + 1) * 128], ws["H"][:], reads=[ws["H"]], writes=[b_OF[d]], key=ws["H"])

                    yield

                run_rr([m_prep(0, 0), m_prep(0, 1)])
                for step in range(NCH):
                    gens = [m_seq(step, 0), m_seq(step, 1)]
                    if step + 1 < NCH:
                        gens += [m_prep(step + 1, 0), m_prep(step + 1, 1)]
                    run_rr(gens)
        if opts.get("finalize", True):
            finalize(l, "ml")

    def phase_gdn_full(l, **kw):
        phase_gdn(l, **kw)
        if opts.get("finalize", True):
            finalize(l, "gdn")

    CGW = 64
    NCG = DHY // CGW
    S3 = 3 * CGW
    QG = 512 // CGW
    XC = dscr("XC", [NCG, 3, 64, 64 * CGW])
    b_XC = Buf("XC", multi=True)
    RG = 8

    def hy_filter_mlp(sc, l, featT_in, Lf, name):
        featT = sc.sb(name + "featT", [33, Lf])
        fw.dma("sp", featT[:], featT_in[:, :], writes=[featT], key=featT)
        w1 = sc.sb(name + "w1", [33, 64])
        fw.dma("sp", w1[:], hyw1_in[l, :, :], writes=[w1], key=w1)
        w2 = sc.sb(name + "w2", [64, 64])
        fw.dma("sp", w2[:], hyw2_in[l, :, :], writes=[w2], key=w2)
        hp = sc.sb(name + "hp", [64, 8])
        fw.dma("sp", hp[:, 0:3], hyp_in[l, :, :], writes=[hp], key=hp)
        fw.op("dve", lambda g: g.tensor_tensor(out=hp[:, 3:4], in0=hp[:, 0:1], in1=hp[:, 2:3], op=ALU.mult), reads=[hp], writes=[hp])
        fw.op("dve", lambda g: g.tensor_tensor(out=hp[:, 4:5], in0=hp[:, 1:2], in1=hp[:, 2:3], op=ALU.mult), reads=[hp], writes=[hp])
        fw.op("dve", lambda g: g.memset(hp[:, 5:6], -math.pi), writes=[hp])
        h1T = sc.sb(name + "h1T", [64, Lf])
        kt = sc.sb(name + "kt", [64, 512])
        h2T = sc.sb(name + "h2T", [64, Lf])
        pss = RR([sc.ps(name + "fps%d" % i, [64, 512]) for i in range(2)])
        for (wt, kdim, src, dst, bcol) in ((w1, 33, featT, h1T, 3), (w2, 64, h1T, h2T, 4)):
            for t0 in range(0, Lf, 512):
                nt = min(512, Lf - t0)
                p = pss.next()
                fw.mm(p, p[:, 0:nt], wt, wt[0:kdim, :], src, src[0:kdim, t0:t0 + nt])
                fw.op("dve", lambda g, p=p, dst=dst, t0=t0, nt=nt, bcol=bcol: g.tensor_scalar(
                    out=dst[:, t0:t0 + nt], in0=p[:, 0:nt], scalar1=hp[:, 2:3], scalar2=hp[:, bcol:bcol + 1], op0=ALU.mult, op1=ALU.add),
                    reads=[p, hp], writes=[dst])
                fw.op("dve", lambda g, dst=dst, t0=t0, nt=nt: g.tensor_scalar(
                    out=kt[:, 0:nt], in0=dst[:, t0:t0 + nt], scalar1=1.0 / (2.0 * math.pi), scalar2=12582912.0, op0=ALU.mult, op1=ALU.add),
                    reads=[dst], writes=[kt])
                fw.op("dve", lambda g, nt=nt: g.tensor_scalar(out=kt[:, 0:nt], in0=kt[:, 0:nt], scalar1=12582912.0, scalar2=None, op0=ALU.subtract),
                      reads=[kt], writes=[kt])
                fw.op("dve", lambda g, dst=dst, t0=t0, nt=nt: g.scalar_tensor_tensor(
                    out=dst[:, t0:t0 + nt], in0=dst[:, t0:t0 + nt], scalar=1.0 / (2.0 * math.pi), in1=kt[:, 0:nt], op0=ALU.mult, op1=ALU.subtract),
                    reads=[dst, kt], writes=[dst])
                fw.op("act", lambda g, dst=dst, t0=t0, nt=nt: g.activation(out=dst[:, t0:t0 + nt], in_=dst[:, t0:t0 + nt], func=AF.Sin, scale=2.0 * math.pi),
                      reads=[dst], writes=[dst])
        return h2T

    def phase_hy(l, cgs=None):
        cgs = range(NCG) if cgs is None else cgs
        last = (l == DEPTH - 1)
        with fw.scope() as sc:
            h2T = sc.sb("h2Tp", [64, SEQ])
            with fw.scope() as sm_:
                h2tmp = hy_filter_mlp(sm_, l, featL_in, SEQ, "L")
                fw.op("pool", lambda g: g.tensor_copy(out=h2T[:], in_=h2tmp[:]), reads=[h2tmp], writes=[h2T])
            w3t = sc.sb("w3t", [64, 2048])
            fw.dma("sp", w3t[:], hyw3_in[l, :, :], writes=[w3t], key=w3t)
            F1 = sc.sb("F1", [64, 2, 128], BF16)
            fw.dma("sp", F1[:], F1_in[:, :, :], writes=[F1], key=F1)
            F4 = sc.sb("F4", [128, 2, 64], BF16)
            fw.dma("sp", F4[:], F4_in[:, :, :], writes=[F4], key=F4)
            negt = sc.sb("negt", [64, 64])
            fw.dma("sp", negt[:], negt_in[:, :], writes=[negt], key=negt)
            drow = sc.sb("drow", [64, 512])
            fw.dma("sp", drow[:], drow_in[0:1, :].partition_broadcast(64)[:, 0, :], writes=[drow], key=drow)
            skipb = sc.sb("skipb", [64, 2, 512])
            fw.dma("sp", skipb[:], hyskip_in[l:l + 1, :].partition_broadcast(64)[:, 0, :].rearrange("p (o c) -> p o c", o=2), writes=[skipb], key=skipb)
            PS = RR([sc.ps("hps%d" % i, [128, 512]) for i in range(5)])
            PSB = RR([sc.ps("hpb%d" % i, [128, 1024], BF16) for i in range(2)])
            VH = sc.sb("VH", [64, S3, 64], BF16)
            vf = sc.sb("vf", [64, CGW, 64])
            evq = RR(["act", "dve"])

            def evac(p_ap, out_ap, ptile, otile, eng=None):
                e = eng or evq.next()
                if e == "act":
                    fw.op("act", lambda g: g.copy(out=out_ap, in_=p_ap), reads=[ptile], writes=[otile])
                else:
                    fw.op(e, lambda g: g.tensor_copy(out=out_ap, in_=p_ap), reads=[ptile], writes=[otile])

            for cg in cgs:
                c0 = cg * CGW
                with fw.scope() as s0:
                    raws = RR([s0.sb("raw%d" % i, [64, 64, CGW]) for i in range(2)])
                    cvs = RR([s0.sb("cv%d" % i, [64, CGW, 64]) for i in range(2)])
                    tmp = s0.sb("ctmp", [64, 64, CGW])
                    cwb = s0.sb("cwb", [64, 3, 3, CGW])
                    fw.dma("sp", cwb[:], hyconv_in[l, :, :].rearrange("j (a c) -> j a c", a=3)[:, :, c0:c0 + CGW].partition_broadcast(64),
                           writes=[cwb], key=cwb)
                    for a in (2, 0, 1, 3):
                        col = (T_HP + a * 512 + c0) if a < 3 else (T_HZ + c0)
                        raw = raws.next()
                        fw.dma("sp" if a % 2 == 0 else "act", raw[:],
                               UT[CTXL:, col:col + CGW].rearrange("(p q) c -> p q c", q=64), reads=[b_UT], writes=[raw], key=raw)
                        if a == 3:
                            cvt = cvs.next()
                            fw.op("act", lambda g, cvt=cvt, raw=raw: g.activation(out=cvt[:].rearrange("p c q -> p q c"), in_=raw[:], func=AF.Silu), reads=[raw], writes=[cvt])
                            fw.dma("act", XC[cg, 2, :, :], cvt[:].rearrange("p c q -> p (c q)"), reads=[cvt], writes=[b_XC], key=cvt)
                            continue
                        dst = vf if a == 2 else cvs.next()
                        dv = dst[:].rearrange("p c q -> p q c")
                        fw.op("dve", lambda g, a=a, dv=dv, dst=dst, raw=raw: g.tensor_tensor(out=dv, in0=raw[:], in1=cwb[:, 1, a, :].unsqueeze(1).to_broadcast([64, 64, CGW]), op=ALU.mult),
                              reads=[raw, cwb], writes=[dst])
                        fw.op("pool", lambda g, a=a, raw=raw: g.tensor_tensor(out=tmp[:, 0:63, :], in0=raw[:, 0:63, :], in1=cwb[:, 0, a, :].unsqueeze(1).to_broadcast([64, 63, CGW]), op=ALU.mult),
                              reads=[raw, cwb], writes=[tmp])
                        fw.op("dve", lambda g, dv=dv, dst=dst: g.tensor_tensor(out=dv[:, 1:64, :], in0=dv[:, 1:64, :], in1=tmp[:, 0:63, :], op=ALU.add),
                              reads=[dst, tmp], writes=[dst])
                        fw.op("pool", lambda g, a=a, raw=raw: g.tensor_tensor(out=tmp[:, 0:63, :], in0=raw[:, 1:64, :], in1=cwb[:, 2, a, :].unsqueeze(1).to_broadcast([64, 63, CGW]), op=ALU.mult),
                              reads=[raw, cwb], writes=[tmp])
                        fw.op("dve", lambda g, dv=dv, dst=dst: g.tensor_tensor(out=dv[:, 0:63, :], in0=dv[:, 0:63, :], in1=tmp[:, 0:63, :], op=ALU.add),
                              reads=[dst, tmp], writes=[dst])
                        if a < 2:
                            fw.dma("act", XC[cg, a, :, :], dst[:].rearrange("p c q -> p (c q)"), reads=[dst], writes=[b_XC], key=dst)
                    fw.op("act", lambda g: g.copy(out=VH[:, 0:CGW, :], in_=vf[:]), reads=[vf], writes=[VH])
                for o in range(2):
                    with fw.scope() as sa:
                        Wt = sa.sb("Wt", [64, CGW, 64])
                        for q in range(64):
                            fw.op("act", lambda g, q=q: g.activation(out=Wt[:, :, q], in_=drow[:, c0:c0 + CGW], func=AF.Exp, scale=negt[:, q:q + 1]),
                                  reads=[drow, negt], writes=[Wt])
                        Hraw = sa.sb("Hraw", [64, 64, 2 * CGW])
                        Hw = sa.sb("Hw", [64, 2 * CGW, 64])
                        part = sa.sb("part", [64, 2 * CGW])
                        tot = sa.sb("tot", [64, 2 * CGW])
                        h2v = h2T[:, :].rearrange("k (p q) -> k q p", q=64)
                        w3v = w3t[:, o * 1024:(o + 1) * 1024].rearrange("k (d c) -> k d c", d=2)[:, :, c0:c0 + CGW]
                        NQ = 512 // (2 * CGW)
                        for q0 in range(0, 64, NQ):
                            p = PS.next()
                            for j in range(NQ):
                                fw.mm(p, p[0:64, j * 2 * CGW:(j + 1) * 2 * CGW], h2T, h2v[:, q0 + j, :], w3t, w3v)
                            evac(p[0:64, :], Hraw[:, q0:q0 + NQ, :].rearrange("p q c -> p (q c)"), p, Hraw)
                        for d in range(2):
                            fw.op("dve", lambda g, d=d: g.tensor_tensor(out=Hw[:, d * CGW:(d + 1) * CGW, :], in0=Hraw[:, :, d * CGW:(d + 1) * CGW].rearrange("p q c -> p c q"),
                                                                       in1=Wt[:], op=ALU.mult), reads=[Hraw, Wt], writes=[Hw])
                        fw.op("act", lambda g: g.activation(out=Hraw[:].rearrange("p q c -> p (q c)"), in_=Hw[:].rearrange("p c q -> p (c q)"), func=AF.Abs),
                              reads=[Hw], writes=[Hraw])
                        fw.op("dve", lambda g: g.tensor_reduce(out=part[:], in_=Hraw[:].rearrange("p q c -> p (q c)").rearrange("p (c q) -> p c q", q=64), axis=AX.X, op=ALU.add),
                              reads=[Hraw], writes=[part])
                        p = PS.next()
                        fw.mm(p, p[0:64, 0:2 * CGW], ones, ones[0:64, 0:64], part, part[:])
                        fw.op("dve", lambda g, p=p: g.tensor_scalar(out=tot[:], in0=p[0:64, 0:2 * CGW], scalar1=EPS, scalar2=None, op0=ALU.add), reads=[p], writes=[tot])
                        fw.op("dve", lambda g: g.reciprocal(out=tot[:], in_=tot[:]), reads=[tot], writes=[tot])
                        fw.op("dve", lambda g: g.tensor_tensor(out=VH[:, CGW:S3, :], in0=Hw[:], in1=tot[:].unsqueeze(2).to_broadcast([64, 2 * CGW, 64]), op=ALU.mult),
                              reads=[Hw, tot], writes=[VH])
                    with fw.scope() as sbc:
                     Zp = sbc.sb("Zp", [128, CGW, 128], BF16)
                     with fw.scope() as sb_:
                         Bt = sb_.sb("Bt", [64, S3, 2, 64], BF16)
                         w2s = RR([sb_.sb("w2s%d" % i, [64, RG, 4, 128], BF16) for i in range(2)])
                         w3s = RR([sb_.sb("w3s%d" % i, [128, RG, 128], BF16) for i in range(2)])
                         XS = RR([sb_.sb("XS%d" % i, [128, RG, 2, S3], BF16) for i in range(2)])
                         KA = sb_.sb("KA", [128, RG, CGW])
                         KB = sb_.sb("KB", [128, RG, CGW])
                         Yt = RR([sb_.sb("Yt%d" % i, [128, RG, CGW], BF16) for i in range(2)])
                         for rh in range(2):
                             for s4 in range(0, S3, 4):
                                 p = PS.next()
                                 for j in range(4):
                                     fw.mm(p, p[0:64, j * 128:(j + 1) * 128], VH, VH[:, s4 + j, :], F1, F1[:, rh, :])
                                 evac(p[0:64, :], Bt[:, s4:s4 + 4, :, :].rearrange("q s i r -> q (s i r)"), p, Bt)
                             def st2(rg):
                                 r0 = rh * 64 + rg * RG
                                 w2 = w2s.next()
                                 fw.dma("sp", w2[:], W2_in[:, r0:r0 + RG, :, :], writes=[w2], key=w2)
                                 w3_ = w3s.next()
                                 fw.dma("pool", w3_[:], W3_in[:, r0:r0 + RG, :], writes=[w3_], key=w3_)
                                 xs = XS.next()
                                 for rl in range(RG):
                                     rloc = rg * RG + rl
                                     p = PS.next()
                                     for v in range(2):
                                         fw.mm(p, p[:, v * 256:v * 256 + S3], w2, w2[:, rl, 2 * v, :], Bt, Bt[:, :, 0, rloc], start=True, stop=False)
                                         fw.mm(p, p[:, v * 256:v * 256 + S3], w2, w2[:, rl, 2 * v + 1, :], Bt, Bt[:, :, 1, rloc], start=False, stop=True)
                                     evac(p[:, :].rearrange("p (v c) -> p v c", v=2)[:, :, 0:S3], xs[:, rl, :, :], p, xs)
                                 return r0, xs, w3_

                             def spec_inv(r0, xs, w3_):
                                 fw.op("pool", lambda g, xs=xs: g.tensor_tensor(out=KA[0:64], in0=xs[0:64, :, 0, CGW:2 * CGW], in1=xs[0:64, :, 0, 2 * CGW:S3], op=ALU.add), reads=[xs], writes=[KA])
                                 fw.op("pool", lambda g, xs=xs: g.tensor_tensor(out=KA[64:128], in0=xs[64:128, :, 1, CGW:2 * CGW], in1=xs[64:128, :, 1, 2 * CGW:S3], op=ALU.add), reads=[xs], writes=[KA])
                                 fw.op("pool", lambda g, xs=xs: g.tensor_tensor(out=KB[0:64], in0=xs[0:64, :, 1, 2 * CGW:S3], in1=xs[0:64, :, 1, CGW:2 * CGW], op=ALU.subtract), reads=[xs], writes=[KB])
                                 fw.op("pool", lambda g, xs=xs: g.tensor_tensor(out=KB[64:128], in0=xs[64:128, :, 0, CGW:2 * CGW], in1=xs[64:128, :, 0, 2 * CGW:S3], op=ALU.subtract), reads=[xs], writes=[KB])
                                 fw.op("dve", lambda g, xs=xs: g.tensor_tensor(out=KA[:], in0=KA[:], in1=xs[:, :, 0, 0:CGW], op=ALU.mult), reads=[KA, xs], writes=[KA])
                                 fw.op("dve", lambda g, xs=xs: g.tensor_tensor(out=KB[:], in0=KB[:], in1=xs[:, :, 1, 0:CGW], op=ALU.mult), reads=[KB, xs], writes=[KB])
                                 yt = Yt.next()
                                 fw.op("dve", lambda g, yt=yt: g.tensor_tensor(out=yt[:], in0=KA[:], in1=KB[:], op=ALU.add), reads=[KA, KB], writes=[yt])
                                 p = PS.next()
                                 for rl in range(RG):
                                     fw.mm(p, p[:, rl * CGW:(rl + 1) * CGW], w3_, w3_[:, rl, :], yt, yt[:, rl, :])
                                 evac(p[:, 0:RG * CGW].rearrange("p (r c) -> p r c", c=CGW), Zp[:, :, r0:r0 + RG].rearrange("p c r -> p r c"), p, Zp)

                             pend = None
                             for rg in range(64 // RG):
                                 cur = st2(rg)
                                 if pend is not None:
                                     spec_inv(*pend)
                                 pend = cur
                             spec_inv(*pend)
                     with fw.scope() as sc_:
                         ZT = sc_.sb("ZT", [128, CGW, 2, 64], BF16)
                         xo = sc_.sb("xo", [64, CGW, 64])
                         sv = sc_.sb("sv", [64, CGW, 64])
                         fw.dma("sp", xo[:].rearrange("p c q -> p (c q)"), XC[cg, o, :, :], reads=[b_XC], writes=[xo], key=xo)
                         fw.op("pool", lambda g: g.tensor_tensor(out=sv[:], in0=vf[:], in1=skipb[:, o, c0:c0 + CGW].unsqueeze(2).to_broadcast([64, CGW, 64]), op=ALU.mult),
                               reads=[vf, skipb], writes=[sv])
                         if o == 1:
                             sz = sc_.sb("sz", [64, CGW, 64])
                             yo = sc_.sb("yo", [64, 64, CGW])
                             fw.dma("act", sz[:].rearrange("p c q -> p (c q)"), XC[cg, 2, :, :], reads=[b_XC], writes=[sz], key=sz)
                             fw.op("pool", lambda g: g.tensor_tensor(out=xo[:], in0=xo[:], in1=sz[:], op=ALU.mult), reads=[xo, sz], writes=[xo])
                         for c8 in range(0, CGW, 8):
                             pb = PSB.next()
                             for j in range(8):
                                 fw.tr(pb, pb[:, j * 128:(j + 1) * 128], Zp, Zp[:, c8 + j, :], identb)
                             evac(pb[:, :], ZT[:, c8:c8 + 8, :, :].rearrange("r c i q -> r (c i q)"), pb, ZT)
                         for q0 in range(0, 64, QG):
                             p = PS.next()
                             fw.mm(p, p[0:64, :], F4, F4[:, 0, :], ZT, ZT[:, :, 0, q0:q0 + QG], start=True, stop=False)
                             fw.mm(p, p[0:64, :], F4, F4[:, 1, :], ZT, ZT[:, :, 1, q0:q0 + QG], start=False, stop=True)
                             pv = p[0:64, :].rearrange("p (c q) -> p c q", q=QG)
                             fw.op("dve", lambda g, pv=pv, q0=q0: g.tensor_tensor(out=sv[:, :, q0:q0 + QG], in0=pv, in1=sv[:, :, q0:q0 + QG], op=ALU.add), reads=[p, sv], writes=[sv])
                             if o == 0:
                                 fw.op("pool", lambda g, q0=q0: g.tensor_tensor(out=vf[:, :, q0:q0 + QG], in0=sv[:, :, q0:q0 + QG], in1=xo[:, :, q0:q0 + QG], op=ALU.mult),
                                       reads=[sv, xo], writes=[vf])
                             else:
                                 fw.op("pool", lambda g, q0=q0: g.tensor_tensor(out=yo[:, q0:q0 + QG, :].rearrange("p q c -> p c q"), in0=sv[:, :, q0:q0 + QG], in1=xo[:, :, q0:q0 + QG], op=ALU.mult),
                                       reads=[sv, xo], writes=[yo])
                         if o == 0:
                             fw.op("act", lambda g: g.copy(out=VH[:, 0:CGW, :], in_=vf[:]), reads=[vf], writes=[VH])
                         else:
                             fw.dma("act", Y[CTXL:, Y_H + c0:Y_H + c0 + CGW].rearrange("(p q) c -> p q c", q=64), yo[:], reads=[yo], writes=[b_Y], key=yo)

    def phase_hy_ctx(l):
        with fw.scope() as sc:
            h2Tc = hy_filter_mlp(sc, l, featC_in, CTXL, "C")
            w3c = sc.sb("w3c", [64, 2048])
            fw.dma("sp", w3c[:], hyw3_in[l, :, :], writes=[w3c], key=w3c)
            negtc = sc.sb("negtc", [128, CTXL])
            fw.dma("sp", negtc[:], negtc_in[0:1, :].partition_broadcast(128)[:, 0, :], writes=[negtc], key=negtc)
            dcol = sc.sb("dcol", [128, 4])
            fw.dma("sp", dcol[:], dcol_in[:, :], writes=[dcol], key=dcol)
            cwT = sc.sb("cwT", [128, 12, 3])
            fw.dma("sp", cwT[:], hyconvT_in[l, :, :, :], writes=[cwT], key=cwT)
            skc = sc.sb("skc", [128, 2, 4])
            fw.dma("sp", skc[:], hyskipT_in[l, :, :, :], writes=[skc], key=skc)
            PS = RR([sc.ps("cps%d" % i, [128, 512]) for i in range(4)])
            XT = sc.sb("XT", [128, 12, CTXL])
            XCc = sc.sb("XCc", [128, 12, CTXL])
            zt = [sc.sb("zt%d" % i, [128, 512]) for i in range(2)]
            yo = [sc.sb("yoc%d" % i, [128, 512]) for i in range(2)]
            tl = sc.sb("ctile", [128, 2048])
            for tt in range(2):
                fw.dma("sp", tl[:], UT[tt * 128:(tt + 1) * 128, T_HP:T_HP + 2048], reads=[b_UT], writes=[tl], key=tl)
                for g4 in range(3):
                    p = PS.next()
                    for j in range(4):
                        gi = g4 * 4 + j
                        fw.tr(p, p[:, j * 128:(j + 1) * 128], tl, tl[:, gi * 128:(gi + 1) * 128], ident)
                    fw.op("act", lambda g, p=p, g4=g4, tt=tt: g.copy(out=XT[:, g4 * 4:(g4 + 1) * 4, tt * 128:(tt + 1) * 128],
                                                                   in_=p[:, :].rearrange("p (g t) -> p g t", g=4)), reads=[p], writes=[XT])
                fw.op("act", lambda g, tt=tt: g.activation(out=zt[tt][:], in_=tl[:, 1536:2048], func=AF.Silu), reads=[tl], writes=[zt[tt]])
            for gi in range(12):
                fw.op("dve", lambda g, gi=gi: g.tensor_scalar(out=XCc[:, gi, :], in0=XT[:, gi, :], scalar1=cwT[:, gi, 1:2], scalar2=None, op0=ALU.mult),
                      reads=[XT, cwT], writes=[XCc])
                fw.op("dve", lambda g, gi=gi: g.scalar_tensor_tensor(out=XCc[:, gi, 1:CTXL], in0=XT[:, gi, 0:CTXL - 1], scalar=cwT[:, gi, 0:1], in1=XCc[:, gi, 1:CTXL],
                                                                    op0=ALU.mult, op1=ALU.add), reads=[XT, cwT, XCc], writes=[XCc])
                fw.op("dve", lambda g, gi=gi: g.scalar_tensor_tensor(out=XCc[:, gi, 0:CTXL - 1], in0=XT[:, gi, 1:CTXL], scalar=cwT[:, gi, 2:3], in1=XCc[:, gi, 0:CTXL - 1],
                                                                    op0=ALU.mult, op1=ALU.add), reads=[XT, cwT, XCc], writes=[XCc])
            Wc = sc.sb("Wc", [128, CTXL])
            hf = [sc.sb("hf%d" % i, [128, CTXL]) for i in range(2)]
            habs = sc.sb("habs", [128, CTXL])
            hs = sc.sb("hs", [128, 4])
            acc = sc.sb("acc", [128, CTXL])
            acc2 = sc.sb("acc2", [128, CTXL])
            tmpc = sc.sb("tmpc", [128, CTXL])
            vcur = sc.sb("vcur", [128, CTXL])
            for gi in range(4):
                fw.op("act", lambda g, gi=gi: g.activation(out=Wc[:], in_=negtc[:], func=AF.Exp, scale=dcol[:, gi:gi + 1]), reads=[negtc, dcol], writes=[Wc])
                fw.op("pool", lambda g, gi=gi: g.tensor_copy(out=vcur[:], in_=XCc[:, 8 + gi, :]), reads=[XCc], writes=[vcur])
                for o in range(2):
                    for dr in range(2):
                        col0 = o * 1024 + dr * 512 + gi * 128
                        p = PS.next()
                        fw.mm(p, p[:, 0:CTXL], w3c, w3c[:, col0:col0 + 128], h2Tc, h2Tc[:, :])
                        fw.op("dve", lambda g, p=p, dr=dr: g.tensor_tensor(out=hf[dr][:], in0=p[:, 0:CTXL], in1=Wc[:], op=ALU.mult), reads=[p, Wc], writes=[hf[dr]])
                        fw.op("act", lambda g, dr=dr: g.activation(out=habs[:], in_=hf[dr][:], func=AF.Abs), reads=[hf[dr]], writes=[habs])
                        fw.op("dve", lambda g: g.tensor_reduce(out=hs[:, 0:1], in_=habs[:], axis=AX.X, op=ALU.add), reads=[habs], writes=[hs])
                        fw.op("dve", lambda g: g.tensor_scalar(out=hs[:, 1:2], in0=hs[:, 0:1], scalar1=EPS, scalar2=None, op0=ALU.add), reads=[hs], writes=[hs])
                        fw.op("dve", lambda g: g.reciprocal(out=hs[:, 2:3], in_=hs[:, 1:2]), reads=[hs], writes=[hs])
                        fw.op("dve", lambda g, dr=dr: g.tensor_scalar(out=hf[dr][:], in0=hf[dr][:], scalar1=hs[:, 2:3], scalar2=None, op0=ALU.mult), reads=[hf[dr], hs], writes=[hf[dr]])
                    fw.op("dve", lambda g, o=o, gi=gi: g.tensor_scalar(out=acc[:], in0=vcur[:], scalar1=skc[:, o, gi:gi + 1], scalar2=None, op0=ALU.mult),
                          reads=[vcur, skc], writes=[acc])
                    fw.op("pool", lambda g: g.memset(acc2[:], 0.0), writes=[acc2])
                    for m in range(CTXL):
                        n_ = CTXL - m
                        fw.op("dve", lambda g, m=m, n_=n_: g.scalar_tensor_tensor(out=acc[:, m:], in0=vcur[:, 0:n_], scalar=hf[0][:, m:m + 1], in1=acc[:, m:],
                                                                              op0=ALU.mult, op1=ALU.add), reads=[vcur, hf[0], acc], writes=[acc])
                        fw.op("dve", lambda g, m=m, n_=n_: g.scalar_tensor_tensor(out=acc2[:, 0:n_], in0=vcur[:, m:], scalar=hf[1][:, m:m + 1], in1=acc2[:, 0:n_],
                                                                              op0=ALU.mult, op1=ALU.add), reads=[vcur, hf[1], acc2], writes=[acc2])
                    fw.op("dve", lambda g: g.tensor_tensor(out=acc[:], in0=acc[:], in1=acc2[:], op=ALU.add), reads=[acc, acc2], writes=[acc])
                    fw.op("dve", lambda g, o=o, gi=gi: g.tensor_tensor(out=vcur[:], in0=acc[:], in1=XCc[:, o * 4 + gi, :], op=ALU.mult), reads=[acc, XCc], writes=[vcur])
                for tt in range(2):
                    p = PS.next()
                    fw.tr(p, p[:, 0:128], vcur, vcur[:, tt * 128:(tt + 1) * 128], ident)
                    fw.op("dve", lambda g, p=p, tt=tt, gi=gi: g.tensor_tensor(out=yo[tt][:, gi * 128:(gi + 1) * 128], in0=p[:, 0:128], in1=zt[tt][:, gi * 128:(gi + 1) * 128], op=ALU.mult),
                          reads=[p, zt[tt]], writes=[yo[tt]])
            for tt in range(2):
                fw.dma("act", Y[tt * 128:(tt + 1) * 128, Y_H:Y_H + DHY], yo[tt][:], reads=[yo[tt]], writes=[b_Y], key=yo[tt])

    def phase_hy_full(l, **kw):
        phase_hy(l, **kw)
        if l != DEPTH - 1 and opts.get("hyctx", True):
            phase_hy_ctx(l)
    scr = {"UF": (UF, b_UF), "UT": (UT, b_UT), "Y": (Y, b_Y), "xcur": (xcur, b_xcur), "ctxcur": (ctxcur, b_ctxcur),
           "OF": (OFs[0], b_OF[0]), "OB": (OFs[1], b_OF[1])}
    for key, (name, sl) in dbg_in.items():
        dst, dbuf = scr[name]
        dst = dst[sl]
        src = din("dbgin_" + key, list(dst.shape))
        fw.dma("sp", dst, src, writes=[dbuf], key=dbuf)
    PH = {"mod": phase_mod, "proj": phase_proj, "out": phase_out, "gdn": phase_gdn_full, "ml": phase_ml, "hy": phase_hy_full}
    for l in layers:
        for ph in ("mod", "proj", "gdn", "hy", "ml", "out"):
            if ph in phases and ph in PH:
                PH[ph](l, **opts.get(ph, {}))
    bd = Buf("dbg", multi=True)
    for name, sl in dbg.items():
        src, sbuf = scr[name]
        if sl != 1:
            src = src[sl]
        dt_ = dbg_tensor(name, src.shape)
        fw.dma("sp", dt_, src, reads=[sbuf], writes=[bd], key=bd)
    fw.fence([b_out, bd], engines=("sp",))
    return nc, fw


_HYC = {}


def make_hy_consts():
    if _HYC:
        return _HYC
    import ml_dtypes
    bf = ml_dtypes.bfloat16
    f = np.float32
    N = 2 * SEQ

    def feats(L):
        pos = np.arange(L, dtype=f)
        t = pos / f(max(L - 1, 1))
        ang = (f(2.0 * math.pi) * pos / f(L)).astype(f)
        bands = np.linspace(1e-4, 15, 16, dtype=f)
        ft = np.concatenate([t[:, None], np.cos(ang[:, None] * bands), -np.sin(ang[:, None] * bands)], axis=-1).astype(f)
        return np.ascontiguousarray(ft.T), t
    featL, tL = feats(SEQ)
    featC, tC = feats(CTXL)
    mind, maxd = math.log(1e-2) / 1.5, math.log(1e-2) / 0.3
    deltas = np.abs(np.linspace(mind, maxd, DHY, dtype=f)).astype(f)
    p = np.arange(64, dtype=np.float64)[:, None]
    r = np.arange(128, dtype=np.float64)[None, :]
    ang = 2 * np.pi * p * r / 128
    F1 = np.stack([np.concatenate([np.cos(ang[:, h * 64:(h + 1) * 64]), -np.sin(ang[:, h * 64:(h + 1) * 64])], axis=1) for h in range(2)], axis=1)
    q = np.arange(64, dtype=np.float64)[:, None, None]
    rr = np.arange(128, dtype=np.float64)[None, :, None]
    s_ = np.arange(64, dtype=np.float64)[None, None, :]
    th = 2 * np.pi * (q * s_ / 64 + q * rr / N)
    c, d = np.cos(th), -np.sin(th)
    W2 = np.stack([np.concatenate([c, d], -1), np.concatenate([-d, c], -1),
                   np.concatenate([-d, c], -1), np.concatenate([-c, -d], -1)], axis=2)
    s2 = np.arange(64, dtype=np.float64)[:, None, None]
    q2 = np.arange(64, dtype=np.float64)[None, None, :]
    th2 = 2 * np.pi * (q2 * s2 / 64 + q2 * rr / N)
    c2, d2 = np.cos(th2), np.sin(th2)
    W3 = np.concatenate([np.concatenate([c2, d2], -1), np.concatenate([-d2, c2], -1)], axis=0)
    r4 = np.arange(128, dtype=np.float64)[:, None]
    p4 = np.arange(64, dtype=np.float64)[None, :]
    a4 = 2 * np.pi * r4 * p4 / 128
    F4 = np.stack([np.cos(a4) / N, -np.sin(a4) / N], axis=1)
    _HYC.update({
        "featL": featL, "featC": featC,
        "negt": np.ascontiguousarray(-tL.reshape(64, 64)), "negtc": np.ascontiguousarray(-tC.reshape(1, CTXL)),
        "drow": np.ascontiguousarray(deltas.reshape(1, DHY)), "dcol": np.ascontiguousarray(deltas.reshape(4, 128).T),
        "F1": np.ascontiguousarray(F1.astype(f)).astype(bf), "F4": np.ascontiguousarray(F4.astype(f)).astype(bf),
        "W2": np.ascontiguousarray(W2.astype(f)).astype(bf), "W3": np.ascontiguousarray(W3.astype(f)).astype(bf),
        "identb": np.eye(128, dtype=f).astype(bf),
    })
    return _HYC


def make_cmask():
    i = np.arange(128)
    P_, F_ = i[:, None], i[None, :]
    m = np.stack([P_ <= F_, P_ >= F_, F_ < P_, F_ > P_, F_ >= P_, F_ <= P_], axis=1)
    return np.ascontiguousarray(m.astype(np.float32))


def make_in_maps(inputs, cores=range(8)):
    f = np.float32
    g = lambda k: np.asarray(inputs[k], dtype=f)
    x, c, ctx, c_ctx = g("x"), g("c"), g("ctx"), g("c_ctx")
    norm_w, mod_w, mod_b, w_in, w_out = g("norm_w"), g("mod_w"), g("mod_b"), g("w_in"), g("w_out")

    def pk(a):
        L_, _, N = a.shape
        return np.ascontiguousarray(a.reshape(L_, KC, 128, N).transpose(0, 2, 1, 3))

    shared = {
        "nwT": np.ascontiguousarray(norm_w.reshape(DEPTH, KC, 128).transpose(2, 0, 1)),
        "fnw": np.ascontiguousarray(g("final_norm").reshape(1, D)),
        "modw": pk(mod_w),
        "modbT": np.ascontiguousarray(mod_b[:, :2 * D].reshape(DEPTH, 32, 128).transpose(2, 0, 1)),
        "modbg": np.ascontiguousarray(mod_b[:, 2 * D:]),
        "winF": pk(w_in[:, :, FCOLS]),
        "winT": pk(w_in[:, :, TCOLS]),
        "wout": pk(w_out),
        "ident": np.eye(128, dtype=f),
        "cmask": make_cmask(),
        **make_hy_consts(),
        "hyw1": g("hy_w1"), "hyw2": g("hy_w2"), "hyw3": g("hy_w3"),
        "hyp": np.ascontiguousarray(np.stack([g("hy_b1"), g("hy_b2"), g("hy_freq")], axis=-1)),
        "hyskip": np.ascontiguousarray(g("hy_skip").reshape(DEPTH, 1024)),
        "hyconv": g("hy_conv"),
        "hyskipT": np.ascontiguousarray(g("hy_skip").reshape(DEPTH, 2, 4, 128).transpose(0, 3, 1, 2)),
        "hyconvT": np.ascontiguousarray(g("hy_conv").transpose(0, 2, 1).reshape(DEPTH, 12, 128, 3).transpose(0, 2, 1, 3)),
        "gnorm": g("gdn_norm"), "mnorm": g("ml_norm"), "mgb": np.ascontiguousarray(g("ml_gate_bias").reshape(DEPTH, 24)),
        "gpar": np.ascontiguousarray(np.concatenate([g("gdn_a_log").reshape(DEPTH, 12), g("gdn_dt_bias").reshape(DEPTH, 12)], axis=1)),
        "gconv": np.ascontiguousarray(g("gdn_conv").transpose(0, 2, 1).reshape(DEPTH, 18, 128, 5).transpose(0, 2, 1, 3)),
    }
    maps = []
    for b in cores:
        cc = np.stack([c[b], c_ctx], axis=-1)
        m = dict(shared)
        m["x"] = np.ascontiguousarray(x[b])
        m["ctx"] = np.ascontiguousarray(ctx[b])
        m["cT"] = np.ascontiguousarray(cc.reshape(KC, 128, 2).transpose(1, 0, 2))
        maps.append(m)
    return maps


_CACHE = {}


def kernel(**inputs):
    if "nc" not in _CACHE:
        _CACHE["nc"] = build()[0]
    nc = _CACHE["nc"]
    maps = make_in_maps(inputs)
    res = run_bass_kernel_spmd(nc, maps, core_ids=list(range(8)))
    return np.stack([np.asarray(r["out"]) for r in res.results], axis=0).astype(np.float32)
```
